# Optimizing a Trainium2 kernel written in Bass

```python
import math
import jax, jax.numpy as jnp
from jax import lax
import numpy as np

D_MODEL = 1024
BATCH = 8
SEQ = 4096
DEPTH = 1

RNN_WIDTH = D_MODEL
RNN_BLOCKS = 8
RNN_BLOCK = RNN_WIDTH // RNN_BLOCKS
CONV_WIDTH = 4
LRU_C = 8.0
LRU_A_MIN, LRU_A_MAX = 0.9, 0.999
S5_WIDTH = D_MODEL // 2
S5_GROUP = 16
S5_GROUPS = S5_WIDTH // S5_GROUP
S5_STATE = 64
DT_MIN, DT_MAX = 1e-3, 1e-1
N_BRANCHES = 2
IN_COLS = 2 * RNN_WIDTH + S5_WIDTH + N_BRANCHES * D_MODEL
N_EXPERTS = 32
TOP_K = 4
D_FF = D_MODEL
SWIGLU_LIMIT = 7.0
SWIGLU_ALPHA = 1.702
ROUTE_BLOCK = 128
LN_EPS = 1e-5
DEEPNORM_ALPHA = (2.0 * DEPTH) ** 0.25
DEEPNORM_BETA = (8.0 * DEPTH) ** -0.25
N_MOD = 6

kernel_name = "hybrid_rglru_s5_moe_deepnorm_adaln"


def layer_norm(x, gain, bias):
    xf = x.astype(jnp.float32)
    mu = jnp.mean(xf, axis=-1, keepdims=True)
    var = jnp.mean(jnp.square(xf - mu), axis=-1, keepdims=True)
    y = (xf - mu) * lax.rsqrt(var + LN_EPS)
    return (y * gain.astype(jnp.float32) + bias.astype(jnp.float32)).astype(x.dtype)


def causal_depthwise_conv(x, w, b):
    C = x.shape[-1]
    y = lax.conv_general_dilated(
        x, w[:, None, :].astype(x.dtype), window_strides=(1,),
        padding=[(CONV_WIDTH - 1, 0)],
        dimension_numbers=("NWC", "WIO", "NWC"), feature_group_count=C)
    return y + b


def rg_lru(x, w_a, b_a, w_x, b_x, lam):
    Bn, S, C = x.shape
    xf = x.astype(jnp.float32)
    xb = xf.reshape(Bn, S, RNN_BLOCKS, RNN_BLOCK)
    r = jax.nn.sigmoid(jnp.einsum("bshi,hij->bshj", xb, w_a.astype(jnp.float32)).reshape(Bn, S, C) + b_a.astype(jnp.float32))
    i = jax.nn.sigmoid(jnp.einsum("bshi,hij->bshj", xb, w_x.astype(jnp.float32)).reshape(Bn, S, C) + b_x.astype(jnp.float32))
    log_a = -LRU_C * r * jax.nn.softplus(-lam.astype(jnp.float32))
    a = jnp.exp(log_a)
    u = jnp.sqrt(-jnp.expm1(2.0 * log_a)) * (i * xf)

    def step(h, au):
        a_t, u_t = au
        h = a_t * h + u_t
        return h, h

    h0 = jnp.zeros((Bn, C), jnp.float32)
    _, hs = lax.scan(step, h0, (jnp.swapaxes(a, 0, 1), jnp.swapaxes(u, 0, 1)))
    return jnp.swapaxes(hs, 0, 1).astype(x.dtype)


def s5_ssm(u, lam_re, lam_im, log_dt, b_re, b_im, c_re, c_im, d_skip):
    Bn, S, _ = u.shape
    f32 = jnp.float32
    ug = u.reshape(Bn, S, S5_GROUPS, S5_GROUP).astype(f32)
    lr, li = lam_re.astype(f32), lam_im.astype(f32)
    dt = jnp.exp(log_dt.astype(f32))[:, None]
    mag = jnp.exp(lr * dt)
    ab_re, ab_im = mag * jnp.cos(li * dt), mag * jnp.sin(li * dt)
    den = lr * lr + li * li
    q_re = ((ab_re - 1.0) * lr + ab_im * li) / den
    q_im = (ab_im * lr - (ab_re - 1.0) * li) / den
    br, bi = b_re.astype(f32), b_im.astype(f32)
    bb_re = q_re[..., None] * br - q_im[..., None] * bi
    bb_im = q_re[..., None] * bi + q_im[..., None] * br
    bu_re = jnp.einsum("bsgc,gpc->bsgp", ug, bb_re)
    bu_im = jnp.einsum("bsgc,gpc->bsgp", ug, bb_im)
    a_re = jnp.broadcast_to(ab_re, bu_re.shape)
    a_im = jnp.broadcast_to(ab_im, bu_im.shape)

    def combine(left, right):
        a1r, a1i, b1r, b1i = left
        a2r, a2i, b2r, b2i = right
        return (a2r * a1r - a2i * a1i,
                a2r * a1i + a2i * a1r,
                a2r * b1r - a2i * b1i + b2r,
                a2r * b1i + a2i * b1r + b2i)

    _, _, st_re, st_im = lax.associative_scan(combine, (a_re, a_im, bu_re, bu_im), axis=1)
    y = (jnp.einsum("bsgp,gcp->bsgc", st_re, c_re.astype(f32))
         - jnp.einsum("bsgp,gcp->bsgc", st_im, c_im.astype(f32))
         + d_skip.astype(f32) * ug)
    return y.reshape(Bn, S, S5_WIDTH).astype(u.dtype)


def mixer_sublayer(h, w_in, b_in, conv_w, conv_b, w_rg_a, b_rg_a, w_rg_x, b_rg_x, lru_lambda,
                   w_rnn_out, s5_lambda_re, s5_lambda_im, s5_log_dt, s5_b_re, s5_b_im,
                   s5_c_re, s5_c_im, s5_d, w_glu, w_out):
    proj = h @ w_in + b_in
    x_rnn, y_rnn, u_s5, g_logits = jnp.split(
        proj, [RNN_WIDTH, 2 * RNN_WIDTH, 2 * RNN_WIDTH + S5_WIDTH], axis=-1)
    xr = causal_depthwise_conv(x_rnn, conv_w, conv_b)
    hr = rg_lru(xr, w_rg_a, b_rg_a, w_rg_x, b_rg_x, lru_lambda)
    branch_a = (jax.nn.gelu(y_rnn) * hr) @ w_rnn_out
    ys = jax.nn.gelu(s5_ssm(u_s5, s5_lambda_re, s5_lambda_im, s5_log_dt, s5_b_re, s5_b_im,
                            s5_c_re, s5_c_im, s5_d))
    glu = ys @ w_glu
    branch_b = glu[..., :D_MODEL] * jax.nn.sigmoid(glu[..., D_MODEL:])
    gates = jax.nn.sigmoid(g_logits).reshape(*g_logits.shape[:-1], N_BRANCHES, D_MODEL)
    merged = gates[..., 0, :] * branch_a + gates[..., 1, :] * branch_b
    return merged @ w_out


def moe_sublayer(h, w_router, b_router, w_gu, b_gu, w_down, b_down):
    Bn, S, D = h.shape
    T = Bn * S
    xt = h.reshape(T, D)
    logits = (xt @ w_router + b_router).astype(jnp.float32)
    top_val, top_idx = lax.top_k(logits, TOP_K)
    probs = jax.nn.softmax(top_val, axis=-1)
    N = T * TOP_K
    flat_e = top_idx.reshape(N).astype(jnp.int32)
    flat_tok = jnp.repeat(jnp.arange(T, dtype=jnp.int32), TOP_K)
    flat_w = probs.reshape(N)
    order = jnp.argsort(flat_e)
    sorted_e = flat_e[order]
    counts = jnp.bincount(flat_e, length=N_EXPERTS).astype(jnp.int32)
    padded = (counts + ROUTE_BLOCK - 1) // ROUTE_BLOCK * ROUTE_BLOCK
    pad_end = jnp.cumsum(padded)
    pad_start = pad_end - padded
    start = jnp.cumsum(counts) - counts
    dest = pad_start[sorted_e] + jnp.arange(N, dtype=jnp.int32) - start[sorted_e]
    n_blocks = -(-(N + N_EXPERTS * (ROUTE_BLOCK - 1)) // ROUTE_BLOCK)
    n_rows = n_blocks * ROUTE_BLOCK
    row_tok = jnp.full((n_rows,), T, jnp.int32).at[dest].set(flat_tok[order])
    row_w = jnp.zeros((n_rows,), jnp.float32).at[dest].set(flat_w[order])
    block_e = jnp.minimum(
        jnp.searchsorted(pad_end, jnp.arange(n_blocks, dtype=jnp.int32) * ROUTE_BLOCK, side="right"),
        N_EXPERTS - 1)
    x_pad = jnp.concatenate([xt, jnp.zeros((1, D), xt.dtype)], axis=0)
    xb = x_pad[row_tok].reshape(n_blocks, ROUTE_BLOCK, D)

    def expert_block(args):
        xblk, e = args
        gu = xblk @ w_gu[e] + b_gu[e]
        gate = jnp.minimum(gu[:, :D_FF], SWIGLU_LIMIT)
        up = jnp.clip(gu[:, D_FF:], -SWIGLU_LIMIT, SWIGLU_LIMIT)
        act = gate * jax.nn.sigmoid(SWIGLU_ALPHA * gate) * (up + 1.0)
        return act @ w_down[e] + b_down[e]

    yb = lax.map(expert_block, (xb, block_e))
    y = jnp.zeros((T + 1, D), jnp.float32).at[row_tok].add(
        yb.reshape(n_rows, D).astype(jnp.float32) * row_w[:, None])
    return y[:T].reshape(Bn, S, D).astype(h.dtype)


def setup_inputs(seed: int = 0) -> dict:
    key = jax.random.key(seed)
    ks = iter(jax.random.split(key, 40))
    nrm = lambda shape, s: jax.random.normal(next(ks), shape, jnp.float32) * s
    L, D = DEPTH, D_MODEL
    a0 = jax.random.uniform(next(ks), (L, RNN_WIDTH), jnp.float32, LRU_A_MIN, LRU_A_MAX)
    return {
        "x": nrm((BATCH, SEQ, D), 1.0),
        "c": nrm((BATCH, D), 1.0),
        "w_ada": nrm((L, D, N_MOD * D), 0.2 * D ** -0.5),
        "b_ada": nrm((L, N_MOD * D), 0.01),
        "w_in": nrm((L, D, IN_COLS), D ** -0.5),
        "b_in": nrm((L, IN_COLS), 0.01),
        "conv_w": nrm((L, CONV_WIDTH, RNN_WIDTH), CONV_WIDTH ** -0.5),
        "conv_b": nrm((L, RNN_WIDTH), 0.01),
        "w_rg_a": nrm((L, RNN_BLOCKS, RNN_BLOCK, RNN_BLOCK), RNN_BLOCK ** -0.5),
        "b_rg_a": nrm((L, RNN_WIDTH), 0.01),
        "w_rg_x": nrm((L, RNN_BLOCKS, RNN_BLOCK, RNN_BLOCK), RNN_BLOCK ** -0.5),
        "b_rg_x": nrm((L, RNN_WIDTH), 0.01),
        "lru_lambda": jnp.log(a0) - jnp.log1p(-a0),
        "w_rnn_out": nrm((L, RNN_WIDTH, D), RNN_WIDTH ** -0.5),
        "s5_lambda_re": -0.5 + nrm((L, S5_GROUPS, S5_STATE), 0.01),
        "s5_lambda_im": jnp.pi * jnp.arange(S5_STATE, dtype=jnp.float32) + nrm((L, S5_GROUPS, S5_STATE), 0.01),
        "s5_log_dt": jax.random.uniform(next(ks), (L, S5_GROUPS), jnp.float32, math.log(DT_MIN), math.log(DT_MAX)),
        "s5_b_re": nrm((L, S5_GROUPS, S5_STATE, S5_GROUP), (2 * S5_GROUP) ** -0.5),
        "s5_b_im": nrm((L, S5_GROUPS, S5_STATE, S5_GROUP), (2 * S5_GROUP) ** -0.5),
        "s5_c_re": nrm((L, S5_GROUPS, S5_GROUP, S5_STATE), S5_STATE ** -0.5),
        "s5_c_im": nrm((L, S5_GROUPS, S5_GROUP, S5_STATE), S5_STATE ** -0.5),
        "s5_d": nrm((L, S5_GROUPS, S5_GROUP), 1.0),
        "w_glu": nrm((L, S5_WIDTH, 2 * D), S5_WIDTH ** -0.5),
        "w_out": nrm((L, D, D), DEEPNORM_BETA * D ** -0.5),
        "ln1_g": 1.0 + nrm((L, D), 0.01),
        "ln1_b": nrm((L, D), 0.01),
        "w_router": nrm((L, D, N_EXPERTS), D ** -0.5),
        "b_router": nrm((L, N_EXPERTS), 0.01),
        "w_gu": nrm((L, N_EXPERTS, D, 2 * D_FF), D ** -0.5),
        "b_gu": nrm((L, N_EXPERTS, 2 * D_FF), 0.01),
        "w_down": nrm((L, N_EXPERTS, D_FF, D), DEEPNORM_BETA * D_FF ** -0.5),
        "b_down": nrm((L, N_EXPERTS, D), 0.01),
        "ln2_g": 1.0 + nrm((L, D), 0.01),
        "ln2_b": nrm((L, D), 0.01),
    }


def reference(x, c, w_ada, b_ada, w_in, b_in, conv_w, conv_b, w_rg_a, b_rg_a, w_rg_x, b_rg_x,
              lru_lambda, w_rnn_out, s5_lambda_re, s5_lambda_im, s5_log_dt, s5_b_re, s5_b_im,
              s5_c_re, s5_c_im, s5_d, w_glu, w_out, ln1_g, ln1_b, w_router, b_router,
              w_gu, b_gu, w_down, b_down, ln2_g, ln2_b):
    c_act = jax.nn.silu(c)
    for l in range(DEPTH):
        mod = (c_act @ w_ada[l] + b_ada[l])[:, None, :]
        sh1, sc1, g1, sh2, sc2, g2 = jnp.split(mod, N_MOD, axis=-1)
        h = x * (1.0 + sc1) + sh1
        mix = mixer_sublayer(h, w_in[l], b_in[l], conv_w[l], conv_b[l], w_rg_a[l], b_rg_a[l],
                             w_rg_x[l], b_rg_x[l], lru_lambda[l], w_rnn_out[l],
                             s5_lambda_re[l], s5_lambda_im[l], s5_log_dt[l], s5_b_re[l], s5_b_im[l],
                             s5_c_re[l], s5_c_im[l], s5_d[l], w_glu[l], w_out[l])
        x = layer_norm(DEEPNORM_ALPHA * x + (1.0 + g1) * mix, ln1_g[l], ln1_b[l])
        h = x * (1.0 + sc2) + sh2
        ffn = moe_sublayer(h, w_router[l], b_router[l], w_gu[l], b_gu[l], w_down[l], b_down[l])
        x = layer_norm(DEEPNORM_ALPHA * x + (1.0 + g2) * ffn, ln2_g[l], ln2_b[l])
    return x
```

```python
import contextlib
import types
import numpy as np
import ml_dtypes
import concourse.bass as bass
import concourse.mybir as mybir
from concourse.bass_utils import run_bass_kernel_spmd

F32 = mybir.dt.float32
BF16 = mybir.dt.bfloat16
I32 = mybir.dt.int32
U8 = mybir.dt.uint8
ALU = mybir.AluOpType
AF = mybir.ActivationFunctionType
AX = mybir.AxisListType

D = 1024
S = 4096
NT = S // 128
NE = 32
CAP = 1024
NB = CAP // 128
ALPHA = 2.0 ** 0.25
EPS = 1e-5
ENGS = ("pe", "act", "dve", "pool", "sp")
EPOCH = 12000


class Prog:
    def __init__(self, nc):
        self.nc = nc
        self.ops = {e: [] for e in ENGS}
        self.cnt = {e: 0 for e in ENGS}
        self.res = {}
        self.dmacnt = {}
        self.waited = {e: {} for e in ENGS}
        self.semnames = []

    def _sem(self, name):
        if name not in self.semnames:
            self.semnames.append(name)
        return name

    def _tok_engine(self, eng):
        self.cnt[eng] += 1
        c = self.cnt[eng]
        ep = (c - 1) // EPOCH
        return (self._sem(f"E{eng}{ep}"), c - ep * EPOCH, eng)

    def op(self, eng, fn, reads=(), writes=(), dma=None):
        waits = []
        for k in reads:
            st = self.res.get(k)
            if st and st["w"] is not None:
                waits.append(st["w"])
        for k in writes:
            st = self.res.get(k)
            if st:
                if st["w"] is not None:
                    waits.append(st["w"])
                waits.extend(st["r"])
        if dma is not None:
            self.dmacnt[dma] = self.dmacnt.get(dma, 0) + 16
            tok = (self._sem(dma), self.dmacnt[dma], "dma")
        else:
            tok = self._tok_engine(eng)
        need = []
        for (s, v, e) in waits:
            if e == eng and dma is None and eng == "pe":
                continue
            if self.waited[eng].get(s, 0) >= v:
                continue
            self.waited[eng][s] = v
            need.append((s, v))
        for k in reads:
            st = self.res.setdefault(k, {"w": None, "r": []})
            st["r"].append(tok)
        for k in writes:
            self.res[k] = {"w": tok, "r": []}
        self.ops[eng].append((need, fn, (tok[0], 16 if dma is not None else 1)))
        return tok

    def barrier(self):
        toks = []
        for e in ENGS:
            if self.cnt[e] > 0:
                c = self.cnt[e]
                ep = (c - 1) // EPOCH
                toks.append((f"E{e}{ep}", c - ep * EPOCH))
        for s, v in self.dmacnt.items():
            toks.append((s, v))
        for e in ENGS:
            need = []
            for (s, v) in toks:
                if s.startswith(f"E{e}"):
                    continue
                if self.waited[e].get(s, 0) >= v:
                    continue
                self.waited[e][s] = v
                need.append((s, v))
            if need:
                self.ops[e].append((need, None, None))

    def emit(self):
        nc = self.nc
        with contextlib.ExitStack() as es:
            sems = {n: es.enter_context(nc.semaphore(n)) for n in self.semnames}
            block = es.enter_context(nc.Block())

            def run(engname, eng):
                for (need, fn, inc) in self.ops[engname]:
                    for (s, v) in need:
                        eng.wait_ge(sems[s], v)
                    if fn is not None:
                        fn(eng).then_inc(sems[inc[0]], inc[1])

            @block.sync
            def _(e):
                run("sp", e)

            @block.scalar
            def _(e):
                run("act", e)

            @block.vector
            def _(e):
                run("dve", e)

            @block.gpsimd
            def _(e):
                run("pool", e)

            @block.tensor
            def _(e):
                run("pe", e)


def interleave(P, builders):
    chains = []
    prev = P.__dict__.get("op")
    for b in builders:
        lst = []
        P.op = lambda *a, _l=lst, **k: _l.append((a, k))
        try:
            b()
        finally:
            if prev is None:
                del P.op
            else:
                P.op = prev
        chains.append(lst)
    n = max(len(l) for l in chains)
    for i in range(n):
        for l in chains:
            if i < len(l):
                P.op(*l[i][0], **l[i][1])


class Arena:
    def __init__(self, t, size):
        self.t = t
        self.size = size
        self.off = 0

    def mark(self):
        return self.off

    def reset(self, m):
        self.off = m

    def alloc(self, shape, dt):
        esz = {F32: 4, BF16: 2, I32: 4, U8: 1}[dt]
        n = int(np.prod(shape))
        nb = (n * esz + 31) // 32 * 32
        assert self.off + nb <= self.size, f"arena overflow {self.off + nb} > {self.size}"
        v = self.t[:, self.off:self.off + n * esz].bitcast(dt)
        self.off += nb
        if len(shape) == 2:
            v = v.rearrange("p (a b) -> p a b", a=shape[0])
        elif len(shape) == 3:
            v = v.rearrange("p (a b c) -> p a b c", a=shape[0], b=shape[1])
        return v


def build_program(dbg=None):
    dbg = dbg or {}
    stop = dbg.get("stop", "end")
    nc = bass.Bass("TRN2", target_bir_lowering=False)
    P = Prog(nc)
    outs_dbg = {}

    def dram_in(name, shape, dt=F32):
        return nc.dram_tensor(name, list(shape), dt, kind="ExternalInput").ap()

    def dram_scr(name, shape, dt, inject=False):
        if inject and name in dbg.get("inject", ()):
            kind = "ExternalInput"
        elif name in dbg.get("dump", ()):
            kind = "ExternalOutput"
        else:
            kind = "Internal"
        return nc.dram_tensor(name, list(shape), dt, kind=kind).ap()

    x_d = dram_in("x", [S, D])
    ccol_d = dram_in("ccol", [128, 8])
    w_ada_d = dram_in("w_ada", [D, 6 * D])
    b_ada_d = dram_in("b_ada", [1, 6 * D])
    w_in_d = dram_in("w_in", [D, 4608])
    smallv_d = dram_in("smallv", [128, 100])
    w_rg_d = dram_in("w_rg", [2, 8, 128, 128])
    w_rnn_out_d = dram_in("w_rnn_out", [D, D])
    w_glu_d = dram_in("w_glu", [512, 2 * D])
    w_out_d = dram_in("w_out", [D, D])
    lnrows_d = dram_in("lnrows", [4, D])
    w_router_d = dram_in("w_router", [D, NE])
    b_router_d = dram_in("b_router", [1, NE])
    w_gu_d = dram_in("w_gu", [NE, D, 2 * D])
    b_gu_d = dram_in("b_gu", [128, NE, 16])
    w_down_d = dram_in("w_down", [NE, D, D])
    b_down_d = dram_in("b_down", [NE, D])
    s5lam_d = dram_in("s5lam", [128, 3, 16])
    s5b_d = dram_in("s5b", [128, 2, 16, 16])
    s5c_d = dram_in("s5c", [128, 2, 16, 16])
    s5d_d = dram_in("s5d", [128, 4])
    cst_d = dram_in("cst", [128, 640])
    out_d = nc.dram_tensor("out", [S, D], F32, kind="ExternalOutput").ap()

    ys_d = dram_scr("ys", [4, 128, S], BF16, inject=True)
    x1_d = dram_scr("x1", [S, D], F32, inject=True)
    h2_d = dram_scr("h2", [S + 128, D], BF16, inject=True)
    lg_d = dram_scr("lg", [128, NT, NE], F32, inject=True)
    tab_d = dram_scr("tab", [NE * CAP, 2], F32)
    yb_d = dram_scr("yb", [NE * CAP if "yb" in dbg.get("dump", ()) else 128, D], F32)
    acc_d = dram_scr("acc", [S + 128, D], F32)
    tabinit_d = dram_in("tabinit", [NE * CAP, 2])
    mod_d = dram_scr("modr", [1, 6 * D], F32)

    ARENA = 197 * 1024
    with contextlib.ExitStack() as es:
        arena_t = es.enter_context(nc.sbuf_tensor("arena", [128, ARENA], U8))
        pers_t = es.enter_context(nc.sbuf_tensor("pers", [128, 10 * 1024], U8))
        psum_t = es.enter_context(nc.psum_tensor("ps", [128, 8, 512], F32))
        A = Arena(arena_t, ARENA)
        PA = Arena(pers_t, 10 * 1024)

        cst = PA.alloc([640], F32)
        ident = cst[:, 0:128]
        smallv = PA.alloc([100], F32)
        modcol = PA.alloc([16], F32)
        cf = PA.alloc([8], F32)
        L = PA.alloc([NT, NE], F32)
        identb = PA.alloc([128], BF16)
        onesb = PA.alloc([128], BF16)
        ones_f = cst[:, 384:512]
        epsc = PA.alloc([1], F32)

        b_in_c = smallv[:, 0:36]
        conv_w_c = smallv[:, 36:68]
        conv_b_c = smallv[:, 68:76]
        b_rga_c = smallv[:, 76:84]
        b_rgx_c = smallv[:, 84:92]
        lam_c = smallv[:, 92:100]

        def PS(bank, lo=0, hi=512):
            return psum_t[:, bank, lo:hi]

        P.op("sp", lambda e: e.dma_start(out=cst, in_=cst_d), writes=["cst"], dma="d_cst")
        P.op("sp", lambda e: e.dma_start(out=smallv, in_=smallv_d), writes=["smallv"], dma="d_smallv")
        P.op("dve", lambda e: e.tensor_copy(identb, ident), reads=["cst"], writes=["identb"])
        P.op("dve", lambda e: e.tensor_copy(onesb, ones_f), reads=["cst"], writes=["onesb"])
        P.op("dve", lambda e: e.memset(epsc, EPS), writes=["epsc"])
        P.op("dve", lambda e: e.memset(L, 0.0), writes=["L"])

        mark0 = A.mark()
        K5 = None
        if "ys" not in dbg.get("inject", ()):
            K5 = s5_prepare(types.SimpleNamespace(**locals()))
        mA = A.mark()
        ccol = A.alloc([8], F32)
        cact = A.alloc([8], F32)
        modrow = A.alloc([6 * D], F32)
        wada = [A.alloc([8, 512], F32) for _ in range(2)]
        P.op("sp", lambda e: e.dma_start(out=ccol, in_=ccol_d), writes=["ccol"], dma="d_ccol")
        P.op("sp", lambda e: e.dma_start(out=modrow[0:1, :], in_=b_ada_d), writes=["modrow"], dma="d_bada")
        P.op("act", lambda e: e.activation(out=cact, in_=ccol, func=AF.Silu), reads=["ccol"], writes=["cact"])
        wada_v = w_ada_d.rearrange("(k p) n -> p k n", p=128)
        for j in range(12):
            buf = wada[j % 2]
            P.op("sp", lambda e, buf=buf, j=j: e.dma_start(out=buf, in_=wada_v[:, :, j * 512:(j + 1) * 512]),
                 writes=[f"wada{j % 2}"], dma=f"d_wada{j % 2}")
            bank = j % 2
            for k in range(8):
                P.op("pe", lambda e, buf=buf, k=k, bank=bank: e.matmul(PS(bank)[0:1, :], cact[:, k:k + 1], buf[:, k, :],
                                                                      start=(k == 0), stop=(k == 7)),
                     reads=[f"wada{j % 2}", "cact"], writes=[f"ps{bank}"])
            P.op("dve", lambda e, j=j, bank=bank: e.tensor_tensor(modrow[0:1, j * 512:(j + 1) * 512], PS(bank)[0:1, :],
                                                                 modrow[0:1, j * 512:(j + 1) * 512], op=ALU.add),
                 reads=[f"ps{bank}", "modrow"], writes=["modrow"])
        P.op("act", lambda e: e.activation(out=cf, in_=lam_c, func=AF.Exp, scale=-1.0), reads=["smallv"], writes=["cf"])
        P.op("act", lambda e: e.activation(out=cf, in_=cf, func=AF.Ln, bias=1.0), reads=["cf"], writes=["cf"])
        P.op("dve", lambda e: e.tensor_scalar(cf, cf, -8.0, None, op0=ALU.mult), reads=["cf"], writes=["cf"])

        P.op("sp", lambda e: e.dma_start(out=mod_d, in_=modrow[0:1, :]), reads=["modrow"], writes=["mod_d"], dma="d_modst")
        P.op("sp", lambda e: e.dma_start(out=modcol[:, 0:8], in_=mod_d[0:1, D:2 * D].rearrange("o (k p) -> p (o k)", p=128), allow_slow_non_contiguous=True),
             reads=["mod_d"], writes=["modcol"], dma="d_mc0")
        P.op("sp", lambda e: e.dma_start(out=modcol[:, 8:16], in_=mod_d[0:1, 0:D].rearrange("o (k p) -> p (o k)", p=128), allow_slow_non_contiguous=True),
             reads=["mod_d"], writes=["modcol2"], dma="d_mc1")
        P.op("dve", lambda e: e.tensor_scalar(modcol[:, 0:8], modcol[:, 0:8], 1.0, None, op0=ALU.add), reads=["modcol"], writes=["modcol"])
        P.barrier()
        A.reset(K5.mark if K5 is not None else mark0)

        if stop == "phaseA":
            return finish(nc, P, out_d, dbg)

        if "ys" not in dbg.get("inject", ()):
            s5_pass(types.SimpleNamespace(**locals()))
            P.barrier()
        A.reset(mark0)
        if stop == "s5":
            return finish(nc, P, out_d, dbg)

        if "x1" not in dbg.get("inject", ()):
            (mixer_pass2 if dbg.get('newmixer') else mixer_pass)(types.SimpleNamespace(**locals()))
            P.barrier()
            A.reset(mark0)
        if stop == "mixer":
            return finish(nc, P, out_d, dbg)

        moe_phase(types.SimpleNamespace(**locals()))
        return finish(nc, P, out_d, dbg)


def finish(nc, P, out_d, dbg):
    P.barrier()
    P.emit()
    return nc


def mixer_pass(c):
    P, A, psum_t = c.P, c.A, c.psum_t
    ident, smallv, modcol, cf, L = c.ident, c.smallv, c.modcol, c.cf, c.L
    TT = 256
    NST = S // TT
    NX = 2
    WIN = A.alloc([8, 4096], BF16)
    WRO = A.alloc([8, D], BF16)
    WGL = A.alloc([4, 2 * D], BF16)
    WOU = A.alloc([8, D], BF16)
    WRG = A.alloc([2, 8, 128], BF16)
    WR = A.alloc([8, NE], F32)
    LN1G = A.alloc([D], F32)
    LN1B = A.alloc([D], F32)
    P1 = A.alloc([D], F32)
    P2 = A.alloc([D], F32)
    BRB = A.alloc([NE], F32)
    XIN = [A.alloc([D], F32) for _ in range(NX)]
    hTs = [A.alloc([8, TT], BF16) for _ in range(2)]
    YSTs = [A.alloc([4, TT], BF16) for _ in range(2)]
    zAs = [A.alloc([8, TT], BF16) for _ in range(2)]
    XRES = A.alloc([D], F32)
    mT = A.alloc([8, TT], BF16)
    TS = []
    for i in range(2):
        TS.append(dict(xc=A.alloc([TT + 8], F32), xr=A.alloc([TT], F32), xrb=A.alloc([TT], BF16), thr=A.alloc([TT], F32),
                       thi=A.alloc([TT], F32), a2=A.alloc([TT], F32), ix=A.alloc([TT], F32)))
        TS[-1]["gy"] = TS[-1]["xc"][:, 0:TT]
        TS[-1]["hs"] = TS[-1]["thi"]
    MTS = [dict(t0=A.alloc([TT], F32), t1=A.alloc([TT], F32), tb=A.alloc([TT], F32)) for _ in range(2)]
    for M__ in MTS:
        M__['ta'] = M__['t0']
        M__['tbb'] = M__['tb']
    V = A.alloc([D], F32)
    H2 = A.alloc([D], F32)
    H2T = V.rearrange("p (k t) -> p k t", k=8)
    HALO = A.alloc([8, 4], F32)
    CARRY = A.alloc([8], F32)
    hbias = A.alloc([36], F32)
    hbrg = A.alloc([16], F32)
    cfh = A.alloc([8], F32)
    STATS = A.alloc([2, 6], F32)
    MV = A.alloc([2], F32)
    RSTD = A.alloc([1], F32)
    MHALF = A.alloc([1], F32)

    b_in_c = smallv[:, 0:36]
    conv_w_c = smallv[:, 36:68]
    conv_b_c = smallv[:, 68:76]

    w_in_v = c.w_in_d.rearrange("(k p) n -> p k n", p=128)
    for k in range(8):
        P.op("pool", lambda e, k=k: e.dma_start(out=WIN[:, k, 0:2048], in_=w_in_v[:, k, 0:2048]), writes=[f"WIN{k}a"], dma=f"d_win{k}a")
    P.op("pool", lambda e: e.dma_start(out=WRG, in_=c.w_rg_d.rearrange("a h i j -> i a h j")), writes=["WRG"], dma="d_wrg")
    P.op("pool", lambda e: e.dma_start(out=WRO, in_=c.w_rnn_out_d.rearrange("(k p) n -> p k n", p=128)), writes=["WRO"], dma="d_wro")
    P.op("pool", lambda e: e.dma_start(out=WGL, in_=c.w_glu_d.rearrange("(k p) n -> p k n", p=128)), writes=["WGL"], dma="d_wgl")
    for k in range(8):
        P.op("pool", lambda e, k=k: e.dma_start(out=WIN[:, k, 2048:4096], in_=w_in_v[:, k, 2560:4608]), writes=[f"WIN{k}b"], dma=f"d_win{k}b")
    P.op("sp", lambda e: e.dma_start(out=WR, in_=c.w_router_d.rearrange("(k p) n -> p k n", p=128)), writes=["WR"], dma="d_wr")
    P.op("sp", lambda e: e.dma_start(out=BRB, in_=c.b_router_d.partition_broadcast(128)), writes=["BRB"], dma="d_brb")
    P.op("sp", lambda e: e.dma_start(out=LN1G, in_=c.lnrows_d[0:1, :].partition_broadcast(128)), writes=["LN1G"], dma="d_ln1g")
    P.op("sp", lambda e: e.dma_start(out=LN1B, in_=c.lnrows_d[1:2, :].partition_broadcast(128)), writes=["LN1B"], dma="d_ln1b")
    P.op("sp", lambda e: e.dma_start(out=P1, in_=c.mod_d[0:1, 4 * D:5 * D].partition_broadcast(128)), reads=["mod_d"], writes=["P1"], dma="d_p1")
    P.op("sp", lambda e: e.dma_start(out=H2, in_=c.mod_d[0:1, 3 * D:4 * D].partition_broadcast(128)), reads=["mod_d"], writes=["H2"], dma="d_h2")
    P.op("sp", lambda e: e.dma_start(out=V, in_=c.mod_d[0:1, 2 * D:3 * D].partition_broadcast(128)), reads=["mod_d"], writes=["V"], dma="d_v")
    P.op("dve", lambda e: e.tensor_scalar(P1, P1, 1.0, None, op0=ALU.add), reads=["P1"], writes=["P1"])
    P.op("dve", lambda e: e.tensor_tensor(P2, LN1B, P1, op=ALU.mult), reads=["LN1B", "P1"], writes=["P2"])
    P.op("dve", lambda e: e.tensor_tensor(P2, P2, H2, op=ALU.add), reads=["P2", "H2"], writes=["P2"])
    P.op("dve", lambda e: e.tensor_tensor(P1, P1, LN1G, op=ALU.mult), reads=["P1", "LN1G"], writes=["P1"])
    P.op("dve", lambda e: e.tensor_scalar(V, V, 1.0, 0.5, op0=ALU.add, op1=ALU.mult), reads=["V"], writes=["V"])
    w_out_v = c.w_out_d.rearrange("(k p) n -> p k n", p=128)
    for k in range(8):
        sl = k % NX
        P.op("sp", lambda e, k=k, sl=sl: e.dma_start(out=XIN[sl], in_=w_out_v[:, k, :]), writes=[f"xin{sl}"], dma=f"d_xin{sl}")
        P.op("dve", lambda e, k=k, sl=sl: e.tensor_tensor(WOU[:, k, :], XIN[sl], V, op=ALU.mult), reads=[f"xin{sl}", "V"], writes=[f"WOU{k}"])
    P.op("dve", lambda e: e.tensor_scalar(hbias, b_in_c, 0.5, None, op0=ALU.mult), reads=["smallv"], writes=["hbias"])
    P.op("dve", lambda e: e.tensor_scalar(hbrg, smallv[:, 76:92], 0.5, None, op0=ALU.mult), reads=["smallv"], writes=["hbrg"])
    P.op("dve", lambda e: e.tensor_scalar(cfh, cf, 0.5, None, op0=ALU.mult), reads=["cf"], writes=["cfh"])
    P.op("dve", lambda e: e.memset(HALO, 0.0), writes=["halo"])
    P.op("dve", lambda e: e.memset(CARRY, 0.0), writes=["carry"])
    P.op("pool", lambda e: e.memset(MHALF, -0.5), writes=["mhalf"])

    hbn = [0]

    pools = {"A": [0, 1, 2, 3], "B4": [4, 5, 6, 7], "B2": [4, 5]}
    pcnt = {"A": 0, "B4": 0, "B2": 0}
    cur_pool = ["A"]

    def _nb():
        pl = cur_pool[0]
        i = pools[pl][pcnt[pl] % len(pools[pl])]
        pcnt[pl] += 1
        return i

    def hb():
        i = _nb()
        return psum_t[:, i, 0:256], f"bank{i}"

    def hb2():
        i = _nb()
        return psum_t[:, i, 0:256], psum_t[:, i, 256:512], f"bank{i}"

    def load_x(g):
        sl = g % NX
        P.op("sp", lambda e, g=g, sl=sl: e.dma_start(out=XIN[sl], in_=c.x_d[g * 128:(g + 1) * 128, :]), writes=[f"xin{sl}"], dma=f"d_xin{sl}")

    load_x(0)
    load_x(1)

    def sec_front(s):
        cur_pool[0] = "A"
        t0 = s * TT
        hT = hTs[s % 2]
        YST = YSTs[s % 2]
        hp_ = s % 2
        P.op("sp", lambda e, t0=t0, YST=YST: e.dma_start(out=YST, in_=c.ys_d[:, :, t0:t0 + TT].rearrange("q p t -> p q t")), writes=[f"yst{hp_}"], dma=f"d_yst{hp_}")
        for k in range(8):
            ps, key = hb()
            for tt in range(2):
                sl = (2 * s + tt) % NX
                P.op("pe", lambda e, ps=ps, tt=tt, sl=sl, k=k: e.transpose(ps[:, tt * 128:(tt + 1) * 128], XIN[sl][:, k * 128:(k + 1) * 128], ident),
                     reads=[f"xin{sl}", "cst"], writes=[key])
            P.op("act", lambda e, ps=ps, k=k, hT=hT: e.activation(out=hT[:, k, :], in_=ps, func=AF.Identity, scale=modcol[:, k:k + 1], bias=modcol[:, 8 + k:9 + k]),
                 reads=[key, "modcol", "modcol2"], writes=[f"hT{hp_}_{k}"])
        if 2 * s + 2 < NT:
            load_x(2 * s + 2)
        if 2 * s + 3 < NT:
            load_x(2 * s + 3)

    def sec_rg(s, hps):
        cur_pool[0] = "A"
        hT = hTs[s % 2]
        zA = zAs[s % 2]
        hp_ = s % 2
        for hp in hps:
            st = {}
            def front_body(j):
                h = 2 * hp + j
                T_ = TS[j]
                psx, kx = hb()
                for k in range(8):
                    P.op("pe", lambda e, psx=psx, k=k, h=h: e.matmul(psx, WIN[:, k, h * 128:(h + 1) * 128], hT[:, k, :], start=(k == 0), stop=(k == 7)),
                         reads=[f"WIN{k}a", f"hT{hp_}_{k}"], writes=[kx])
                P.op("pool", lambda e, T_=T_, h=h: e.tensor_copy(T_["xc"][:, 0:3], HALO[:, h, 0:3]), reads=["halo"], writes=[f"xc{j}"])
                P.op("act", lambda e, T_=T_, psx=psx, h=h: e.activation(out=T_["xc"][:, 3:3 + TT], in_=psx, func=AF.Identity, bias=b_in_c[:, h:h + 1]),
                     reads=[kx, "smallv"], writes=[f"xc{j}"])
                P.op("pool", lambda e, T_=T_, h=h: e.tensor_copy(HALO[:, h, 0:3], T_["xc"][:, TT:TT + 3]), reads=[f"xc{j}"], writes=["halo"])
                P.op("dve", lambda e, T_=T_, h=h: e.tensor_scalar(T_["xr"], T_["xc"][:, 0:TT], conv_w_c[:, 4 * h:4 * h + 1], conv_b_c[:, h:h + 1], op0=ALU.mult, op1=ALU.add),
                     reads=[f"xc{j}", "smallv"], writes=[f"xr{j}"])
                for q in range(1, 4):
                    P.op("dve", lambda e, T_=T_, h=h, q=q: e.scalar_tensor_tensor(T_["xr"], T_["xc"][:, q:q + TT], conv_w_c[:, 4 * h + q:4 * h + q + 1], T_["xr"], op0=ALU.mult, op1=ALU.add),
                         reads=[f"xc{j}", f"xr{j}"], writes=[f"xr{j}"])
                P.op("pool", lambda e, T_=T_: e.tensor_copy(T_["xrb"], T_["xr"]), reads=[f"xr{j}"], writes=[f"xrb{j}"])
                psr, psi, kr = hb2()
                ki = kr
                P.op("pe", lambda e, psr=psr, T_=T_, h=h: e.matmul(psr, WRG[:, 0, h, :], T_["xrb"], start=True, stop=True), reads=["WRG", f"xrb{j}"], writes=[kr])
                P.op("pe", lambda e, psi=psi, T_=T_, h=h: e.matmul(psi, WRG[:, 1, h, :], T_["xrb"], start=True, stop=True), reads=["WRG", f"xrb{j}"], writes=[ki])
                P.op("act", lambda e, T_=T_, psr=psr, h=h: e.activation(out=T_["thr"], in_=psr, func=AF.Tanh, scale=0.5, bias=hbrg[:, h:h + 1]),
                     reads=[kr, "hbrg"], writes=[f"thr{j}"])
                P.op("act", lambda e, T_=T_, psi=psi, h=h: e.activation(out=T_["thi"], in_=psi, func=AF.Tanh, scale=0.5, bias=hbrg[:, 8 + h:9 + h]),
                     reads=[ki, "hbrg"], writes=[f"thi{j}"])
                P.op("act", lambda e, T_=T_, h=h: e.activation(out=T_["thr"], in_=T_["thr"], func=AF.Exp, scale=cfh[:, h:h + 1], bias=cfh[:, h:h + 1]),
                     reads=[f"thr{j}", "cfh"], writes=[f"thr{j}"])
                P.op("pool", lambda e, T_=T_: e.tensor_tensor(T_["a2"], T_["thr"], T_["thr"], op=ALU.mult), reads=[f"thr{j}"], writes=[f"a2{j}"])
                P.op("dve", lambda e, T_=T_: e.scalar_tensor_tensor(T_["ix"], T_["thi"], 1.0, T_["xr"], op0=ALU.add, op1=ALU.mult),
                     reads=[f"thi{j}", f"xr{j}"], writes=[f"ix{j}"])
            interleave(P, [lambda j=j: front_body(j) for j in range(2)])
            for j in range(2):
                T_ = TS[j]
                P.op("act", lambda e, T_=T_: e.activation(out=T_["a2"], in_=T_["a2"], func=AF.Sqrt, scale=-1.0, bias=1.0), reads=[f"a2{j}"], writes=[f"a2{j}"])
            def back_body(j):
                h = 2 * hp + j
                T_ = TS[j]
                psy, ky = hb()
                for k in range(8):
                    P.op("pe", lambda e, psy=psy, k=k, h=h: e.matmul(psy, WIN[:, k, 1024 + h * 128:1024 + (h + 1) * 128], hT[:, k, :], start=(k == 0), stop=(k == 7)),
                         reads=[f"WIN{k}a", f"hT{hp_}_{k}"], writes=[ky])
                P.op("act", lambda e, T_=T_, psy=psy, h=h: e.activation(out=T_["gy"], in_=psy, func=AF.Gelu_apprx_tanh, bias=b_in_c[:, 8 + h:9 + h]),
                     reads=[ky, "smallv"], writes=[f"xc{j}"])
                P.op("dve", lambda e, T_=T_: e.scalar_tensor_tensor(T_["ix"], T_["a2"], 0.5, T_["ix"], op0=ALU.mult, op1=ALU.mult),
                     reads=[f"a2{j}", f"ix{j}"], writes=[f"ix{j}"])
                P.op("dve", lambda e, T_=T_, h=h: e.tensor_tensor_scan(T_["hs"], T_["thr"], T_["ix"], CARRY[:, h:h + 1], op0=ALU.mult, op1=ALU.add),
                     reads=[f"thr{j}", f"ix{j}", "carry"], writes=[f"thi{j}"])
                P.op("dve", lambda e, T_=T_, h=h: e.tensor_copy(CARRY[:, h:h + 1], T_["hs"][:, TT - 1:TT]), reads=[f"thi{j}"], writes=["carry"])
                P.op("pool", lambda e, T_=T_, h=h, zA=zA: e.tensor_tensor(zA[:, h, :], T_["gy"], T_["hs"], op=ALU.mult), reads=[f"xc{j}", f"thi{j}"], writes=[f"zA{hp_}_{h}"])
            interleave(P, [lambda j=j: back_body(j) for j in range(2)])

    def sec_merge(s, ccs):
        cur_pool[0] = "B4"
        hT = hTs[s % 2]
        zA = zAs[s % 2]
        YST = YSTs[s % 2]
        hp_ = s % 2
        def merge_body(cc):
            psA, kA = hb()
            for h in range(8):
                P.op("pe", lambda e, psA=psA, h=h, cc=cc, zA=zA: e.matmul(psA, WRO[:, h, cc * 128:(cc + 1) * 128], zA[:, h, :], start=(h == 0), stop=(h == 7)),
                     reads=["WRO", f"zA{hp_}_{h}"], writes=[kA])
            psB1, psB2, kB1 = hb2()
            kB2 = kB1
            for q in range(4):
                P.op("pe", lambda e, psB1=psB1, q=q, cc=cc, YST=YST: e.matmul(psB1, WGL[:, q, cc * 128:(cc + 1) * 128], YST[:, q, :], start=(q == 0), stop=(q == 3)),
                     reads=["WGL", f"yst{hp_}"], writes=[kB1])
            for q in range(4):
                P.op("pe", lambda e, psB2=psB2, q=q, cc=cc, YST=YST: e.matmul(psB2, WGL[:, q, D + cc * 128:D + (cc + 1) * 128], YST[:, q, :], start=(q == 0), stop=(q == 3)),
                     reads=["WGL", f"yst{hp_}"], writes=[kB2])
            psG0, psG1, kG0 = hb2()
            kG1 = kG0
            for k in range(8):
                P.op("pe", lambda e, psG0=psG0, k=k, cc=cc: e.matmul(psG0, WIN[:, k, 2048 + cc * 128:2048 + (cc + 1) * 128], hT[:, k, :], start=(k == 0), stop=(k == 7)),
                     reads=[f"WIN{k}b", f"hT{hp_}_{k}"], writes=[kG0])
            for k in range(8):
                P.op("pe", lambda e, psG1=psG1, k=k, cc=cc: e.matmul(psG1, WIN[:, k, 3072 + cc * 128:3072 + (cc + 1) * 128], hT[:, k, :], start=(k == 0), stop=(k == 7)),
                     reads=[f"WIN{k}b", f"hT{hp_}_{k}"], writes=[kG1])
            M_ = MTS[cc % 2]
            mk = cc % 2
            P.op("act", lambda e, psG0=psG0, cc=cc, M_=M_: e.activation(out=M_["t0"], in_=psG0, func=AF.Tanh, scale=0.5, bias=hbias[:, 20 + cc:21 + cc]), reads=[kG0, "hbias"], writes=[f"m_t0{mk}"])
            P.op("act", lambda e, psG1=psG1, cc=cc, M_=M_: e.activation(out=M_["t1"], in_=psG1, func=AF.Tanh, scale=0.5, bias=hbias[:, 28 + cc:29 + cc]), reads=[kG1, "hbias"], writes=[f"m_t1{mk}"])
            P.op("act", lambda e, psB2=psB2, M_=M_: e.activation(out=M_["tb"], in_=psB2, func=AF.Tanh, scale=0.5), reads=[kB2], writes=[f"m_tb{mk}"])
            P.op("dve", lambda e, psA=psA, M_=M_: e.scalar_tensor_tensor(M_["ta"], M_["t0"], 1.0, psA, op0=ALU.add, op1=ALU.mult), reads=[f"m_t0{mk}", kA], writes=[f"m_t0{mk}"])
            P.op("dve", lambda e, psB1=psB1, M_=M_: e.scalar_tensor_tensor(M_["tbb"], M_["tb"], 1.0, psB1, op0=ALU.add, op1=ALU.mult), reads=[f"m_tb{mk}", kB1], writes=[f"m_tb{mk}"])
            P.op("dve", lambda e, M_=M_: e.scalar_tensor_tensor(M_["tbb"], M_["t1"], 1.0, M_["tbb"], op0=ALU.add, op1=ALU.mult), reads=[f"m_t1{mk}", f"m_tb{mk}"], writes=[f"m_tb{mk}"])
            P.op("dve", lambda e, cc=cc, M_=M_: e.scalar_tensor_tensor(mT[:, cc, :], M_["tbb"], 0.5, M_["ta"], op0=ALU.mult, op1=ALU.add), reads=[f"m_tb{mk}", f"m_t0{mk}"], writes=[f"mT{cc}"])
        for cc in ccs:
            merge_body(cc)

    def sec_out(s, tts):
        cur_pool[0] = "B2"
        for tt in tts:
            g = 2 * s + tt
            sl = 0
            P.op("sp", lambda e, g=g: e.dma_start(out=XRES, in_=c.x_d[g * 128:(g + 1) * 128, :]), writes=["xres"], dma="d_xres")
            for hlf in range(2):
                for cc in range(8):
                    P.op("pe", lambda e, hlf=hlf, cc=cc, tt=tt: e.matmul(psum_t[:, 6 + hlf, :], mT[:, cc, tt * 128:(tt + 1) * 128], WOU[:, cc, hlf * 512:(hlf + 1) * 512],
                                                                       start=(cc == 0), stop=(cc == 7)),
                         reads=[f"mT{cc}", f"WOU{cc}"], writes=[f"bank{6 + hlf}"])
                P.op("dve", lambda e, hlf=hlf, sl=sl: e.scalar_tensor_tensor(V[:, hlf * 512:(hlf + 1) * 512], XRES[:, hlf * 512:(hlf + 1) * 512], ALPHA,
                                                                            psum_t[:, 6 + hlf, :], op0=ALU.mult, op1=ALU.add),
                     reads=["xres", f"bank{6 + hlf}"], writes=["V"])
                P.op("dve", lambda e, hlf=hlf: e.bn_stats(STATS[:, hlf, :], V[:, hlf * 512:(hlf + 1) * 512]), reads=["V"], writes=["stats"])
            P.op("dve", lambda e: e.bn_aggr(MV, STATS.rearrange("p a b -> p (a b)")), reads=["stats"], writes=["mv"])
            P.op("pool", lambda e: e.tensor_scalar(RSTD, MV[:, 1:2], EPS, None, op0=ALU.add), reads=["mv"], writes=["rstd"])
            P.op("pool", lambda e: e.tensor_tensor(RSTD, RSTD, MHALF, op=ALU.pow), reads=["rstd", "mhalf"], writes=["rstd"])
            P.op("dve", lambda e: e.tensor_scalar(V, V, MV[:, 0:1], RSTD, op0=ALU.subtract, op1=ALU.mult), reads=["V", "mv", "rstd"], writes=["V"])
            P.op("pool", lambda e, sl=sl: e.tensor_tensor(XRES, V, LN1G, op=ALU.mult), reads=["V", "LN1G"], writes=["xres"])
            P.op("pool", lambda e, sl=sl: e.tensor_tensor(XRES, XRES, LN1B, op=ALU.add), reads=["xres", "LN1B"], writes=["xres"])
            P.op("dve", lambda e: e.tensor_tensor(H2, V, P1, op=ALU.mult), reads=["V", "P1"], writes=["H2"])
            P.op("dve", lambda e: e.tensor_tensor(H2, H2, P2, op=ALU.add), reads=["H2", "P2"], writes=["H2"])
            P.op("sp", lambda e, g=g, sl=sl: e.dma_start(out=c.x1_d[g * 128:(g + 1) * 128, :], in_=XRES), reads=["xres"], writes=["x1_d"], dma="d_x1st")
            P.op("pool", lambda e, g=g: e.dma_start(out=c.h2_d[g * 128:(g + 1) * 128, :], in_=H2), reads=["H2"], writes=["h2_d"], dma="d_h2st")
            for kp in range(4):
                ps, key = hb()
                for j in range(2):
                    k = 2 * kp + j
                    P.op("pe", lambda e, ps=ps, j=j, k=k: e.transpose(ps[:, j * 128:(j + 1) * 128], H2[:, k * 128:(k + 1) * 128], ident), reads=["H2", "cst"], writes=[key])
                P.op("act", lambda e, ps=ps, kp=kp: e.activation(out=H2T[:, 2 * kp:2 * kp + 2, :], in_=ps.rearrange("p (a b) -> p a b", a=2), func=AF.Identity),
                     reads=[key], writes=["V"])
            psl, kl = hb()
            for k in range(8):
                P.op("pe", lambda e, psl=psl, k=k: e.matmul(psl[:, 0:NE], H2T[:, k, :], WR[:, k, :], start=(k == 0), stop=(k == 7)), reads=["V", "WR"], writes=[kl])
            P.op("dve", lambda e, psl=psl, g=g: e.tensor_tensor(L[:, g, :], psl[:, 0:NE], BRB, op=ALU.add), reads=[kl, "BRB"], writes=["L"])
    nst_ = c.dbg.get('nst', NST)
    sec_front(0)
    for s in range(nst_ + 1):
        for hp in range(4):
            bl_ = []
            if s < nst_:
                bl_.append(lambda s=s, hp=hp: sec_rg(s, [hp]))
            if s >= 1:
                bl_.append(lambda s=s, hp=hp: sec_merge(s - 1, [2 * hp, 2 * hp + 1]))
            interleave(P, bl_)
        bl_ = []
        if s >= 1:
            bl_.append(lambda s=s: sec_out(s - 1, [0, 1]))
        if s + 1 < nst_:
            bl_.append(lambda s=s: sec_front(s + 1))
        if bl_:
            interleave(P, bl_)
    P.op("sp", lambda e: e.dma_start(out=c.lg_d, in_=L), reads=["L"], writes=["lg_d"], dma="d_lgst")


def mixer_pass2(c):
    P, A, psum_t = c.P, c.A, c.psum_t
    ident, smallv, modcol, cf, L = c.ident, c.smallv, c.modcol, c.cf, c.L
    TT = 128
    NST = c.dbg.get("nst", S // TT)
    WIN = A.alloc([8, 4096], BF16)
    WRO = A.alloc([8, D], BF16)
    WGL = A.alloc([4, 2 * D], BF16)
    WOU = A.alloc([8, D], BF16)
    WRG = A.alloc([2, 8, 128], BF16)
    WR = A.alloc([8, NE], F32)
    LN1G = A.alloc([D], F32)
    LN1B = A.alloc([D], F32)
    P1 = A.alloc([D], F32)
    P2 = A.alloc([D], F32)
    BRB = A.alloc([NE], F32)
    XIN = [A.alloc([D], F32) for _ in range(3)]
    hTs = [A.alloc([8, TT], BF16) for _ in range(2)]
    YSTs = [A.alloc([4, TT], BF16)]
    zA = A.alloc([8, TT], BF16)
    mT = A.alloc([8, TT], BF16)
    XC = A.alloc([8, TT + 8], F32)
    XR = A.alloc([8, TT], F32)
    XRB = A.alloc([8, TT], BF16)
    THR = A.alloc([8, TT], F32)
    THI = A.alloc([8, TT], F32)
    A2 = A.alloc([8, TT], F32)
    IX = A.alloc([8, TT], F32)
    GY = XC[:, :, 0:TT]
    HS = THI
    MTS = [dict(t0=A.alloc([4, TT], F32), t1=A.alloc([4, TT], F32), tb=A.alloc([4, TT], F32)) for _ in range(1)]
    MTS[0]['ta'] = MTS[0]['t0']
    MTS[0]['tbb'] = MTS[0]['tb']
    V = A.alloc([D], F32)
    H2 = A.alloc([D], F32)
    H2T = V.rearrange("p (k t) -> p k t", k=8)
    HALO = A.alloc([8, 4], F32)
    CARRY = A.alloc([8], F32)
    hbias = A.alloc([36], F32)
    hbrg = A.alloc([16], F32)
    cfh = A.alloc([8], F32)
    cf1 = A.alloc([8], F32)
    STATS = A.alloc([2, 6], F32)
    MV = A.alloc([2], F32)
    RSTD = A.alloc([1], F32)
    MHALF = A.alloc([1], F32)
    b_in_c = smallv[:, 0:36]
    conv_w_c = smallv[:, 36:68]
    conv_b_c = smallv[:, 68:76]
    MUL, ADD = ALU.mult, ALU.add

    w_in_v = c.w_in_d.rearrange("(k p) n -> p k n", p=128)
    for k in range(8):
        P.op("pool", lambda e, k=k: e.dma_start(out=WIN[:, k, 0:2048], in_=w_in_v[:, k, 0:2048]), writes=[f"WIN{k}a"], dma=f"d_win{k}a")
    P.op("pool", lambda e: e.dma_start(out=WRG, in_=c.w_rg_d.rearrange("a h i j -> i a h j")), writes=["WRG"], dma="d_wrg")
    P.op("pool", lambda e: e.dma_start(out=WRO, in_=c.w_rnn_out_d.rearrange("(k p) n -> p k n", p=128)), writes=["WRO"], dma="d_wro")
    P.op("pool", lambda e: e.dma_start(out=WGL, in_=c.w_glu_d.rearrange("(k p) n -> p k n", p=128)), writes=["WGL"], dma="d_wgl")
    for k in range(8):
        P.op("pool", lambda e, k=k: e.dma_start(out=WIN[:, k, 2048:4096], in_=w_in_v[:, k, 2560:4608]), writes=[f"WIN{k}b"], dma=f"d_win{k}b")
    P.op("sp", lambda e: e.dma_start(out=WR, in_=c.w_router_d.rearrange("(k p) n -> p k n", p=128)), writes=["WR"], dma="d_wr")
    P.op("sp", lambda e: e.dma_start(out=BRB, in_=c.b_router_d.partition_broadcast(128)), writes=["BRB"], dma="d_brb")
    P.op("sp", lambda e: e.dma_start(out=LN1G, in_=c.lnrows_d[0:1, :].partition_broadcast(128)), writes=["LN1G"], dma="d_ln1g")
    P.op("sp", lambda e: e.dma_start(out=LN1B, in_=c.lnrows_d[1:2, :].partition_broadcast(128)), writes=["LN1B"], dma="d_ln1b")
    P.op("sp", lambda e: e.dma_start(out=P1, in_=c.mod_d[0:1, 4 * D:5 * D].partition_broadcast(128)), reads=["mod_d"], writes=["P1"], dma="d_p1")
    P.op("sp", lambda e: e.dma_start(out=H2, in_=c.mod_d[0:1, 3 * D:4 * D].partition_broadcast(128)), reads=["mod_d"], writes=["H2"], dma="d_h2")
    P.op("sp", lambda e: e.dma_start(out=V, in_=c.mod_d[0:1, 2 * D:3 * D].partition_broadcast(128)), reads=["mod_d"], writes=["V"], dma="d_v")
    P.op("dve", lambda e: e.tensor_scalar(P1, P1, 1.0, None, op0=ADD), reads=["P1"], writes=["P1"])
    P.op("dve", lambda e: e.tensor_tensor(P2, LN1B, P1, op=MUL), reads=["LN1B", "P1"], writes=["P2"])
    P.op("dve", lambda e: e.tensor_tensor(P2, P2, H2, op=ADD), reads=["P2", "H2"], writes=["P2"])
    P.op("dve", lambda e: e.tensor_tensor(P1, P1, LN1G, op=MUL), reads=["P1", "LN1G"], writes=["P1"])
    P.op("dve", lambda e: e.tensor_scalar(V, V, 1.0, 0.5, op0=ADD, op1=MUL), reads=["V"], writes=["V"])
    w_out_v = c.w_out_d.rearrange("(k p) n -> p k n", p=128)
    for k in range(8):
        sl = k % 3
        P.op("sp", lambda e, k=k, sl=sl: e.dma_start(out=XIN[sl], in_=w_out_v[:, k, :]), writes=[f"xin{sl}"], dma=f"d_xin{sl}")
        P.op("dve", lambda e, k=k, sl=sl: e.tensor_tensor(WOU[:, k, :], XIN[sl], V, op=MUL), reads=[f"xin{sl}", "V"], writes=[f"WOU{k}"])
    P.op("dve", lambda e: e.tensor_scalar(hbias, b_in_c, 0.5, None, op0=MUL), reads=["smallv"], writes=["hbias"])
    P.op("dve", lambda e: e.tensor_scalar(hbrg, smallv[:, 76:92], 0.5, None, op0=MUL), reads=["smallv"], writes=["hbrg"])
    P.op("dve", lambda e: e.tensor_scalar(cfh, cf, 0.5, None, op0=MUL), reads=["cf"], writes=["cfh"])
    P.op("dve", lambda e: e.tensor_copy(cf1, cf), reads=["cf"], writes=["cf1"])
    P.op("dve", lambda e: e.memset(HALO, 0.0), writes=["halo"])
    P.op("dve", lambda e: e.memset(CARRY, 0.0), writes=["carry"])
    P.op("pool", lambda e: e.memset(MHALF, -0.5), writes=["mhalf"])

    bn = [0]

    def nb():
        i = bn[0] % 8
        bn[0] += 1
        return psum_t[:, i, :], f"bank{i}"

    def load_x(g):
        sl = g % 3
        P.op("sp", lambda e, g=g, sl=sl: e.dma_start(out=XIN[sl], in_=c.x_d[g * 128:(g + 1) * 128, :]), writes=[f"xin{sl}"], dma=f"d_xin{sl}")

    def load_ys(g):
        P.op("sp", lambda e, g=g: e.dma_start(out=YSTs[0], in_=c.ys_d[:, :, g * TT:(g + 1) * TT].rearrange("q p t -> p q t")), writes=["yst0"], dma="d_yst0")

    load_x(0)
    load_x(1)
    load_ys(0)
    for s in range(NST):
        g = s
        sl = g % 3
        hT = hTs[s % 2]
        YST = YSTs[0]
        hp_ = s % 2
        if s + 2 < NT:
            load_x(s + 2)
        for kh in range(2):
            ps, key = nb()
            for kk in range(4):
                k = kh * 4 + kk
                P.op("pe", lambda e, ps=ps, kk=kk, k=k, sl=sl: e.transpose(ps[:, kk * 128:(kk + 1) * 128], XIN[sl][:, k * 128:(k + 1) * 128], ident), reads=[f"xin{sl}", "cst"], writes=[key])
            for kk in range(4):
                k = kh * 4 + kk
                P.op("act", lambda e, ps=ps, kk=kk, k=k, hT=hT: e.activation(out=hT[:, k, :], in_=ps[:, kk * 128:(kk + 1) * 128], func=AF.Identity, scale=modcol[:, k:k + 1], bias=modcol[:, 8 + k:9 + k]),
                     reads=[key, "modcol", "modcol2"], writes=[f"hT{hp_}"])
        hk = f"hT{hp_}"
        P.op("pool", lambda e: e.tensor_copy(XC[:, :, 0:3], HALO[:, :, 0:3]), reads=["halo"], writes=[f"XC{h_}" for h_ in range(8)])
        for hh in range(2):
            ps, key = nb()
            for hq in range(4):
                h = hh * 4 + hq
                for k in range(8):
                    P.op("pe", lambda e, ps=ps, hq=hq, h=h, k=k, hT=hT: e.matmul(ps[:, hq * 128:(hq + 1) * 128], WIN[:, k, h * 128:(h + 1) * 128], hT[:, k, :], start=(k == 0), stop=(k == 7)),
                         reads=[f"WIN{k}a", hk], writes=[key])
            for hq in range(4):
                h = hh * 4 + hq
                P.op("act", lambda e, ps=ps, hq=hq, h=h: e.activation(out=XC[:, h, 3:3 + TT], in_=ps[:, hq * 128:(hq + 1) * 128], func=AF.Identity, bias=b_in_c[:, h:h + 1]),
                     reads=[key, "smallv"], writes=[f"XC{h}"])
        P.op("pool", lambda e: e.tensor_copy(HALO[:, :, 0:3], XC[:, :, TT:TT + 3]), reads=[f"XC{h_}" for h_ in range(8)], writes=["halo"])
        for h in range(8):
            P.op("dve", lambda e, h=h: e.tensor_scalar(XR[:, h, :], XC[:, h, 0:TT], conv_w_c[:, 4 * h:4 * h + 1], conv_b_c[:, h:h + 1], op0=MUL, op1=ADD), reads=[f"XC{h}", "smallv"], writes=[f"XR{h}"])
        for q in range(1, 4):
            for h in range(8):
                P.op("dve", lambda e, h=h, q=q: e.scalar_tensor_tensor(XR[:, h, :], XC[:, h, q:q + TT], conv_w_c[:, 4 * h + q:4 * h + q + 1], XR[:, h, :], op0=MUL, op1=ADD),
                     reads=[f"XC{h}", f"XR{h}"], writes=[f"XR{h}"])
        for hh in range(2):
            P.op("dve", lambda e, hh=hh: e.tensor_copy(XRB[:, hh * 4:(hh + 1) * 4, :], XR[:, hh * 4:(hh + 1) * 4, :]), reads=[f"XR{h_}" for h_ in range(hh * 4, hh * 4 + 4)], writes=[f"XRB{hh}"])
        gbanks = []
        for a_ in range(2):
            for hh in range(2):
                ps, key = nb()
                gbanks.append((ps, key))
                for hq in range(4):
                    h = hh * 4 + hq
                    P.op("pe", lambda e, ps=ps, hq=hq, h=h, a_=a_: e.matmul(ps[:, hq * 128:(hq + 1) * 128], WRG[:, a_, h, :], XRB[:, h, :], start=True, stop=True), reads=["WRG", f"XRB{hh}"], writes=[key])
        for a_ in range(2):
            for hh in range(2):
                ps, key = gbanks[a_ * 2 + hh]
                for hq in range(4):
                    h = hh * 4 + hq
                    dst = THR if a_ == 0 else THI
                    P.op("act", lambda e, ps=ps, hq=hq, h=h, a_=a_, dst=dst: e.activation(out=dst[:, h, :], in_=ps[:, hq * 128:(hq + 1) * 128], func=AF.Tanh, scale=0.5, bias=hbrg[:, a_ * 8 + h:a_ * 8 + h + 1]),
                         reads=[key, "hbrg"], writes=[(f"THR{h}" if a_ == 0 else f"THI{h}")])
        for h in range(8):
            P.op("act", lambda e, h=h: e.activation(out=A2[:, h, :], in_=THR[:, h, :], func=AF.Exp, scale=cf1[:, h:h + 1], bias=cf1[:, h:h + 1]), reads=[f"THR{h}", "cf1"], writes=[f"A2{h}"])
        for h in range(8):
            P.op("act", lambda e, h=h: e.activation(out=THR[:, h, :], in_=THR[:, h, :], func=AF.Exp, scale=cfh[:, h:h + 1], bias=cfh[:, h:h + 1]), reads=[f"THR{h}", "cfh"], writes=[f"THR{h}"])
        def hv(t, hh):
            return t[:, hh * 4:(hh + 1) * 4, :].rearrange("p a b -> p (a b)")
        for hh in range(2):
            P.op("dve", lambda e, hh=hh: e.scalar_tensor_tensor(hv(IX, hh), hv(THI, hh), 1.0, hv(XR, hh), op0=ADD, op1=MUL),
                 reads=[f"THI{h_}" for h_ in range(hh * 4, hh * 4 + 4)] + [f"XR{h_}" for h_ in range(hh * 4, hh * 4 + 4)], writes=[f"IX{hh}"])
        for hh in range(2):
            P.op("act", lambda e, hh=hh: e.activation(out=hv(A2, hh), in_=hv(A2, hh), func=AF.Sqrt, scale=-1.0, bias=1.0), reads=[f"A2{h_}" for h_ in range(hh * 4, hh * 4 + 4)], writes=[f"A2s{hh}"])
        for hh in range(2):
            ps, key = nb()
            for hq in range(4):
                h = hh * 4 + hq
                for k in range(8):
                    P.op("pe", lambda e, ps=ps, hq=hq, h=h, k=k, hT=hT: e.matmul(ps[:, hq * 128:(hq + 1) * 128], WIN[:, k, 1024 + h * 128:1024 + (h + 1) * 128], hT[:, k, :], start=(k == 0), stop=(k == 7)),
                         reads=[f"WIN{k}a", hk], writes=[key])
            for hq in range(4):
                h = hh * 4 + hq
                P.op("act", lambda e, ps=ps, hq=hq, h=h: e.activation(out=GY[:, h, :], in_=ps[:, hq * 128:(hq + 1) * 128], func=AF.Gelu_apprx_tanh, bias=b_in_c[:, 8 + h:9 + h]),
                     reads=[key, "smallv"], writes=[f"XC{h}"])
        for hh in range(2):
            P.op("dve", lambda e, hh=hh: e.scalar_tensor_tensor(hv(IX, hh), hv(A2, hh), 0.5, hv(IX, hh), op0=MUL, op1=MUL), reads=[f"A2s{hh}", f"IX{hh}"], writes=[f"IX{hh}"])
        for h in range(8):
            P.op("dve", lambda e, h=h: e.tensor_tensor_scan(HS[:, h, :], THR[:, h, :], IX[:, h, :], CARRY[:, h:h + 1], op0=MUL, op1=ADD), reads=[f"THR{h}", f"IX{h // 4}", "carry", f"THI{h}"], writes=[f"THI{h}"])
        P.op("dve", lambda e: e.tensor_copy(CARRY, HS[:, :, TT - 1]), reads=[f"THI{h_}" for h_ in range(8)], writes=["carry"])
        for hh in range(2):
            P.op("dve", lambda e, hh=hh: e.tensor_tensor(hv(zA, hh), GY[:, hh * 4:(hh + 1) * 4, :], HS[:, hh * 4:(hh + 1) * 4, :], op=MUL),
                 reads=[f"XC{h_}" for h_ in range(hh * 4, hh * 4 + 4)] + [f"THI{h_}" for h_ in range(hh * 4, hh * 4 + 4)], writes=[f"zA{hh}"])
        for ch in range(2):
            M_ = MTS[0]
            psA, kA = nb()
            for cq in range(4):
                cc = ch * 4 + cq
                for h in range(8):
                    P.op("pe", lambda e, psA=psA, cq=cq, cc=cc, h=h: e.matmul(psA[:, cq * 128:(cq + 1) * 128], WRO[:, h, cc * 128:(cc + 1) * 128], zA[:, h, :], start=(h == 0), stop=(h == 7)),
                         reads=["WRO", f"zA{h // 4}"], writes=[kA])
            psB1, kB1 = nb()
            for cq in range(4):
                cc = ch * 4 + cq
                for q in range(4):
                    P.op("pe", lambda e, psB1=psB1, cq=cq, cc=cc, q=q, YST=YST: e.matmul(psB1[:, cq * 128:(cq + 1) * 128], WGL[:, q, cc * 128:(cc + 1) * 128], YST[:, q, :], start=(q == 0), stop=(q == 3)),
                         reads=["WGL", "yst0"], writes=[kB1])
            psB2, kB2 = nb()
            for cq in range(4):
                cc = ch * 4 + cq
                for q in range(4):
                    P.op("pe", lambda e, psB2=psB2, cq=cq, cc=cc, q=q, YST=YST: e.matmul(psB2[:, cq * 128:(cq + 1) * 128], WGL[:, q, D + cc * 128:D + (cc + 1) * 128], YST[:, q, :], start=(q == 0), stop=(q == 3)),
                         reads=["WGL", "yst0"], writes=[kB2])
            psG = []
            for gi_ in range(2):
                psg, kg = nb()
                psG.append((psg, kg))
                for cq in range(4):
                    cc = ch * 4 + cq
                    for k in range(8):
                        P.op("pe", lambda e, psg=psg, cq=cq, cc=cc, k=k, gi_=gi_, hT=hT: e.matmul(psg[:, cq * 128:(cq + 1) * 128], WIN[:, k, 2048 + gi_ * 1024 + cc * 128:2048 + gi_ * 1024 + (cc + 1) * 128], hT[:, k, :],
                                                                                                 start=(k == 0), stop=(k == 7)),
                             reads=[f"WIN{k}b", hk], writes=[kg])
            for gi_ in range(2):
                psg, kg = psG[gi_]
                dst = M_["t0"] if gi_ == 0 else M_["t1"]
                for cq in range(4):
                    cc = ch * 4 + cq
                    P.op("act", lambda e, psg=psg, cq=cq, cc=cc, gi_=gi_, dst=dst: e.activation(out=dst[:, cq, :], in_=psg[:, cq * 128:(cq + 1) * 128], func=AF.Tanh, scale=0.5, bias=hbias[:, 20 + gi_ * 8 + cc:21 + gi_ * 8 + cc]),
                         reads=[kg, "hbias"], writes=[f"m_t{gi_}"])
            f2 = lambda t: t.rearrange("p a b -> p (a b)")
            P.op("act", lambda e, psB2=psB2, M_=M_: e.activation(out=f2(M_["tb"]), in_=psB2, func=AF.Tanh, scale=0.5), reads=[kB2], writes=[f"m_tb"])
            P.op("dve", lambda e, psA=psA, M_=M_: e.scalar_tensor_tensor(f2(M_["ta"]), f2(M_["t0"]), 1.0, psA, op0=ADD, op1=MUL), reads=[f"m_t0", kA], writes=[f"m_t0"])
            P.op("dve", lambda e, psB1=psB1, M_=M_: e.scalar_tensor_tensor(f2(M_["tbb"]), f2(M_["tb"]), 1.0, psB1, op0=ADD, op1=MUL), reads=[f"m_tb", kB1], writes=[f"m_tb"])
            P.op("dve", lambda e, M_=M_: e.scalar_tensor_tensor(f2(M_["tbb"]), f2(M_["t1"]), 1.0, f2(M_["tbb"]), op0=ADD, op1=MUL), reads=[f"m_t1", f"m_tb"], writes=[f"m_tb"])
            P.op("dve", lambda e, M_=M_, ch=ch: e.scalar_tensor_tensor(f2(mT[:, ch * 4:(ch + 1) * 4, :]), f2(M_["tbb"]), 0.5, f2(M_["ta"]), op0=MUL, op1=ADD), reads=[f"m_tb", f"m_t0"], writes=[f"mT{ch}"])
        if s + 1 < NT:
            load_ys(s + 1)
        obk = []
        for hlf in range(2):
            ps, key = nb()
            obk.append((ps, key))
            for cc in range(8):
                P.op("pe", lambda e, ps=ps, hlf=hlf, cc=cc: e.matmul(ps, mT[:, cc, :], WOU[:, cc, hlf * 512:(hlf + 1) * 512], start=(cc == 0), stop=(cc == 7)),
                     reads=[f"mT{cc // 4}", f"WOU{cc}"], writes=[key])
        for hlf in range(2):
            ps, key = obk[hlf]
            P.op("dve", lambda e, ps=ps, hlf=hlf, sl=sl: e.scalar_tensor_tensor(V[:, hlf * 512:(hlf + 1) * 512], XIN[sl][:, hlf * 512:(hlf + 1) * 512], ALPHA, ps, op0=MUL, op1=ADD),
                 reads=[f"xin{sl}", key], writes=["V"])
            P.op("dve", lambda e, hlf=hlf: e.bn_stats(STATS[:, hlf, :], V[:, hlf * 512:(hlf + 1) * 512]), reads=["V"], writes=["stats"])
        P.op("dve", lambda e: e.bn_aggr(MV, STATS.rearrange("p a b -> p (a b)")), reads=["stats"], writes=["mv"])
        P.op("pool", lambda e: e.tensor_scalar(RSTD, MV[:, 1:2], EPS, None, op0=ADD), reads=["mv"], writes=["rstd"])
        P.op("pool", lambda e: e.tensor_tensor(RSTD, RSTD, MHALF, op=ALU.pow), reads=["rstd", "mhalf"], writes=["rstd"])
        P.op("dve", lambda e: e.tensor_scalar(V, V, MV[:, 0:1], RSTD, op0=ALU.subtract, op1=MUL), reads=["V", "mv", "rstd"], writes=["V"])
        P.op("pool", lambda e, sl=sl: e.tensor_tensor(XIN[sl], V, LN1G, op=MUL), reads=["V", "LN1G"], writes=[f"xin{sl}"])
        P.op("pool", lambda e, sl=sl: e.tensor_tensor(XIN[sl], XIN[sl], LN1B, op=ADD), reads=[f"xin{sl}", "LN1B"], writes=[f"xin{sl}"])
        P.op("dve", lambda e: e.tensor_tensor(H2, V, P1, op=MUL), reads=["V", "P1"], writes=["H2"])
        P.op("dve", lambda e: e.tensor_tensor(H2, H2, P2, op=ADD), reads=["H2", "P2"], writes=["H2"])
        P.op("sp", lambda e, g=g, sl=sl: e.dma_start(out=c.x1_d[g * 128:(g + 1) * 128, :], in_=XIN[sl]), reads=[f"xin{sl}"], writes=["x1_d"], dma=f"d_x1st{sl}")
        P.op("pool", lambda e, g=g: e.dma_start(out=c.h2_d[g * 128:(g + 1) * 128, :], in_=H2), reads=["H2"], writes=["h2_d"], dma="d_h2st")
        for kh in range(2):
            ps, key = nb()
            for kk in range(4):
                k = kh * 4 + kk
                P.op("pe", lambda e, ps=ps, kk=kk, k=k: e.transpose(ps[:, kk * 128:(kk + 1) * 128], H2[:, k * 128:(k + 1) * 128], ident), reads=["H2", "cst"], writes=[key])
            P.op("act", lambda e, ps=ps, kh=kh: e.activation(out=H2T[:, kh * 4:(kh + 1) * 4, :], in_=ps.rearrange("p (a b) -> p a b", a=4), func=AF.Identity), reads=[key], writes=["V"])
        psl, kl = nb()
        for k in range(8):
            P.op("pe", lambda e, psl=psl, k=k: e.matmul(psl[:, 0:NE], H2T[:, k, :], WR[:, k, :], start=(k == 0), stop=(k == 7)), reads=["V", "WR"], writes=[kl])
        P.op("dve", lambda e, psl=psl, g=g: e.tensor_tensor(L[:, g, :], psl[:, 0:NE], BRB, op=ADD), reads=[kl, "BRB"], writes=["L"])
    P.op("sp", lambda e: e.dma_start(out=c.lg_d, in_=L), reads=["L"], writes=["lg_d"], dma="d_lgst")


def s5_prepare(c):
    import math
    P, A, psum_t, cst, ident = c.P, c.A, c.psum_t, c.cst, c.ident
    NJ = 64
    half = cst[:, 512:514]
    mask16 = cst[:, 256:384]
    K_ = types.SimpleNamespace()
    K_.WINU = A.alloc([8, 512], BF16)
    K_.WinT = A.alloc([2, 4, 1024], BF16)
    K_.KTA = A.alloc([4, 1024], BF16)
    K_.WinT3 = A.alloc([2, 4, 1024], BF16)
    K_.COUT = A.alloc([2, 16, 320], BF16)
    K_.COS = A.alloc([16, NJ], F32)
    K_.SIN = A.alloc([16, NJ], F32)
    K_.RHO = A.alloc([16, NJ], F32)
    K_.PW = A.alloc([9, 2, 16], F32)
    K_.CAR = A.alloc([2, 16], F32)
    K_.mark = A.mark()
    LAM = A.alloc([3, 16], F32)
    Bt = A.alloc([2, 16, 16], F32)
    Ct = A.alloc([2, 16, 16], F32)
    DCOL = A.alloc([4], F32)
    ED = A.alloc([8, 2, 512], F32)
    CX = A.alloc([2, 512], F32)
    BB = A.alloc([2, 16, 16], F32)
    W3 = [A.alloc([16, 16], F32) for _ in range(6)]
    s = [A.alloc([16], F32) for _ in range(16)]
    TJ = [A.alloc([16, 32], F32) for _ in range(4)]
    KT0 = A.alloc([128], F32)
    PRE = ["s5pre"]

    def dve(fn):
        P.op("dve", fn, reads=PRE, writes=PRE)

    def act(fn):
        P.op("act", fn, reads=PRE, writes=PRE)

    def tt(o, a, b, op):
        dve(lambda e: e.tensor_tensor(o, a, b, op=op))

    def ts(o, a, s1, op0, s2=None, op1=None):
        if op1 is None:
            dve(lambda e: e.tensor_scalar(o, a, s1, None, op0=op0))
        else:
            dve(lambda e: e.tensor_scalar(o, a, s1, s2, op0=op0, op1=op1))

    MUL, ADD, SUB = ALU.mult, ALU.add, ALU.subtract

    def cmul(ore, oim, are, aim, bre, bim, t1, t2):
        tt(t1, aim, bim, MUL)
        tt(t2, are, bre, MUL)
        tt(ore, t2, t1, SUB)
        tt(t1, are, bim, MUL)
        tt(t2, aim, bre, MUL)
        tt(oim, t2, t1, ADD)

    P.op("sp", lambda e: e.dma_start(out=LAM, in_=c.s5lam_d), writes=PRE, dma="d_s5lam")
    P.op("sp", lambda e: e.dma_start(out=Bt, in_=c.s5b_d), writes=["s5Bt"], dma="d_s5b")
    P.op("sp", lambda e: e.dma_start(out=Ct, in_=c.s5c_d), writes=["s5Ct"], dma="d_s5c")
    P.op("sp", lambda e: e.dma_start(out=DCOL, in_=c.s5d_d), writes=["s5D"], dma="d_s5d")
    P.op("pool", lambda e: e.dma_start(out=K_.WINU, in_=c.w_in_d.rearrange("(k p) n -> p k n", p=128)[:, :, 2048:2560]), writes=["WINU"], dma="d_winu")
    lr, li, ldt = LAM[:, 0, :], LAM[:, 1, :], LAM[:, 2, :]
    dt, rl, th, mag, rho8, sn, cs, t1, t2, t3, are, aim, qre, qim, den, am1 = s
    act(lambda e: e.activation(out=dt, in_=ldt, func=AF.Exp))
    tt(rl, lr, dt, MUL)
    tt(th, li, dt, MUL)
    act(lambda e: e.activation(out=mag, in_=rl, func=AF.Exp))
    act(lambda e: e.activation(out=rho8, in_=rl, func=AF.Exp, scale=8.0))
    ts(t1, th, 1.0 / 32, MUL)
    ts(t2, th, 1.0 / 32, MUL, math.pi / 2, ADD)
    act(lambda e: e.activation(out=sn, in_=t1, func=AF.Sin))
    act(lambda e: e.activation(out=cs, in_=t2, func=AF.Sin))
    for _ in range(5):
        tt(t1, cs, cs, MUL)
        tt(t2, sn, sn, MUL)
        tt(t3, cs, sn, MUL)
        tt(cs, t1, t2, SUB)
        ts(sn, t3, 2.0, MUL)
    tt(are, mag, cs, MUL)
    tt(aim, mag, sn, MUL)
    tt(t1, lr, lr, MUL)
    tt(t2, li, li, MUL)
    tt(den, t1, t2, ADD)
    dve(lambda e: e.reciprocal(den, den))
    ts(am1, are, -1.0, ADD)
    tt(t1, am1, lr, MUL)
    tt(t2, aim, li, MUL)
    tt(t1, t1, t2, ADD)
    tt(qre, t1, den, MUL)
    tt(t1, aim, lr, MUL)
    tt(t2, am1, li, MUL)
    tt(t1, t1, t2, SUB)
    tt(qim, t1, den, MUL)
    PRE.extend(["s5Bt", "s5Ct", "s5D"])

    def bc(a):
        return a.unsqueeze(2).to_broadcast([128, 16, 16])

    cmul(BB[:, 0], BB[:, 1], bc(qre), bc(qim), Bt[:, 0], Bt[:, 1], W3[0], W3[1])
    PW = K_.PW
    dve(lambda e: e.memset(PW[:, 0, 0, :], 1.0))
    dve(lambda e: e.memset(PW[:, 0, 1, :], 0.0))
    for k in range(1, 9):
        cmul(PW[:, k, 0, :], PW[:, k, 1, :], PW[:, k - 1, 0, :], PW[:, k - 1, 1, :], are, aim, t1, t2)
    for d in range(8):
        cmul(W3[2], W3[3], bc(PW[:, d, 0, :]), bc(PW[:, d, 1, :]), BB[:, 0], BB[:, 1], W3[0], W3[1])
        for part in range(2):
            ev = ED[:, d, part, :].rearrange("p (m g c) -> p m g c", m=16, g=2)
            for g in range(2):
                ts(ev[:, :, g, :], W3[2 + part], half[:, g:g + 1], MUL)
    for part in range(2):
        cv = CX[:, part, :].rearrange("p (m g c) -> p m g c", m=16, g=2)
        for g in range(2):
            ts(cv[:, :, g, :], Ct[:, part], half[:, g:g + 1], MUL, (1.0 if part == 0 else -1.0), MUL)
    n = 0
    for part in range(2):
        for q in range(4):
            for dh in range(2):
                bank = n % 2
                n += 1
                for dd in range(4):
                    d = dh * 4 + dd
                    P.op("pe", lambda e, bank=bank, dd=dd, d=d, part=part, q=q: e.transpose(psum_t[:, bank, dd * 128:(dd + 1) * 128], ED[:, d, part, q * 128:(q + 1) * 128], ident),
                         reads=PRE + ["cst"], writes=[f"bank{bank}"])
                P.op("act", lambda e, bank=bank, part=part, q=q, dh=dh: e.activation(out=K_.WinT[:, part, q, dh * 512:(dh + 1) * 512], in_=psum_t[:, bank, :], func=AF.Identity),
                     reads=[f"bank{bank}"], writes=["WinT"])
    P.op("dve", lambda e: e.tensor_copy(K_.WinT3[64:128], K_.WinT[64:128]), reads=["WinT"], writes=["WinT3"])
    P.op("dve", lambda e: e.memset(K_.WinT3[64:96], 0.0), reads=["WinT3"], writes=["WinT3"])
    for q in range(4):
        for d in range(8):
            bank = 2 + (n % 2)
            n += 1
            P.op("pe", lambda e, bank=bank, d=d, q=q: e.matmul(psum_t[:, bank, 0:128], ED[:, d, 0, q * 128:(q + 1) * 128], CX[:, 0, q * 128:(q + 1) * 128], start=True, stop=False),
                 reads=PRE, writes=[f"bank{bank}"])
            P.op("pe", lambda e, bank=bank, d=d, q=q: e.matmul(psum_t[:, bank, 0:128], ED[:, d, 1, q * 128:(q + 1) * 128], CX[:, 1, q * 128:(q + 1) * 128], start=False, stop=True),
                 reads=PRE, writes=[f"bank{bank}"])
            if d == 0:
                P.op("dve", lambda e, bank=bank: e.tensor_tensor(KT0, psum_t[:, bank, 0:128], mask16, op=MUL), reads=[f"bank{bank}", "cst"], writes=["KT0"])
                P.op("dve", lambda e, q=q: e.scalar_tensor_tensor(K_.KTA[:, q, 0:128], ident, DCOL[:, q:q + 1], KT0, op0=MUL, op1=ADD), reads=["KT0", "cst"] + PRE, writes=["KTA"])
            else:
                P.op("dve", lambda e, bank=bank, q=q, d=d: e.tensor_tensor(K_.KTA[:, q, d * 128:(d + 1) * 128], psum_t[:, bank, 0:128], mask16, op=MUL),
                     reads=[f"bank{bank}", "cst"], writes=["KTA"])
    for mp in range(8):
        pr, pi_ = bc(PW[:, mp + 1, 0, :]), bc(PW[:, mp + 1, 1, :])
        tt(W3[0], Ct[:, 0], pr, MUL)
        tt(W3[1], Ct[:, 1], pi_, MUL)
        tt(W3[2], W3[0], W3[1], SUB)
        tt(W3[0], Ct[:, 0], pi_, MUL)
        tt(W3[1], Ct[:, 1], pr, MUL)
        tt(W3[3], W3[0], W3[1], ADD)
        for part in range(2):
            ov = K_.COUT[:, part, :, :].rearrange("p m (a f) -> p m a f", a=8)
            for g in range(2):
                ts(ov[:, :, mp, g * 16:(g + 1) * 16], W3[2 + part], half[:, g:g + 1], MUL, (1.0 if part == 0 else -1.0), MUL)
    e1c, e1s = t1, t2
    dve(lambda e: e.reciprocal(t3, rho8))
    tt(e1c, PW[:, 8, 0, :], t3, MUL)
    tt(e1s, PW[:, 8, 1, :], t3, MUL)
    COS, SIN, RHO = K_.COS, K_.SIN, K_.RHO
    dve(lambda e: e.memset(COS[:, :, 0:1], 1.0))
    dve(lambda e: e.memset(SIN[:, :, 0:1], 0.0))
    dve(lambda e: e.tensor_copy(COS[:, :, 1:2], e1c.unsqueeze(2)))
    dve(lambda e: e.tensor_copy(SIN[:, :, 1:2], e1s.unsqueeze(2)))
    pc, ps_ = den, am1
    dve(lambda e: e.tensor_copy(pc, e1c))
    dve(lambda e: e.tensor_copy(ps_, e1s))
    nn = 2
    while nn < NJ:
        tt(qre, pc, pc, MUL)
        tt(qim, ps_, ps_, MUL)
        tt(t3, pc, ps_, MUL)
        tt(pc, qre, qim, SUB)
        ts(ps_, t3, 2.0, MUL)
        bcn = lambda a, nn=nn: a.unsqueeze(2).to_broadcast([128, 16, nn])
        a1, a2, a3, a4 = (TJ[i][:, :, 0:nn] for i in range(4))
        tt(a1, COS[:, :, 0:nn], bcn(pc), MUL)
        tt(a2, SIN[:, :, 0:nn], bcn(ps_), MUL)
        tt(a3, SIN[:, :, 0:nn], bcn(pc), MUL)
        tt(a4, COS[:, :, 0:nn], bcn(ps_), MUL)
        tt(COS[:, :, nn:2 * nn], a1, a2, SUB)
        tt(SIN[:, :, nn:2 * nn], a3, a4, ADD)
        nn *= 2
    dve(lambda e: e.tensor_copy(RHO, rho8.unsqueeze(2).to_broadcast([128, 16, NJ])))
    dve(lambda e: e.memset(RHO[:, :, 0:1], 0.0))
    dve(lambda e: e.memset(K_.CAR, 0.0))
    return K_


def s5_pass(c):
    P, A, psum_t, K_ = c.P, c.A, c.psum_t, c.K5
    ident, identb, smallv, modcol = c.ident, c.identb, c.smallv, c.modcol
    TT, NJ = 512, 64
    NST = c.dbg.get("nst5", S // TT)
    b_in_c = smallv[:, 0:36]
    XIN = [A.alloc([D], F32) for _ in range(4)]
    hTs = [A.alloc([8, TT], BF16) for _ in range(2)]
    XQs = [A.alloc([4, TT], BF16) for _ in range(2)]
    Tm = [A.alloc([16, NJ], F32) for _ in range(6)]
    ST = A.alloc([2, 16, NJ], BF16)
    YSC = A.alloc([1024], BF16)
    YSF = A.alloc([4, TT], BF16)
    CT = A.alloc([2, 16], F32)
    COS, SIN, RHO, PW, CAR = K_.COS, K_.SIN, K_.RHO, K_.PW, K_.CAR
    MUL, ADD, SUB = ALU.mult, ALU.add, ALU.subtract
    psTb = psum_t[:, 1, :].bitcast(BF16)

    def v4(t):
        return t.rearrange("p (q ml) j -> p q ml j", q=4)

    def f2(t):
        return t.rearrange("p m j -> p (m j)")

    def load_x(g):
        sl = g % 4
        P.op("sp", lambda e, g=g, sl=sl: e.dma_start(out=XIN[sl], in_=c.x_d[g * 128:(g + 1) * 128, :]), writes=[f"xin{sl}"], dma=f"d_xin{sl}")

    for g in range(4):
        load_x(g)
    for s in range(NST):
        t0 = s * TT
        hT = hTs[s % 2]
        XQ = XQs[s % 2]
        sp_ = s % 2
        for k in range(8):
            for tt_ in range(4):
                sl = (4 * s + tt_) % 4
                P.op("pe", lambda e, tt_=tt_, sl=sl, k=k: e.transpose(psum_t[:, 0, tt_ * 128:(tt_ + 1) * 128], XIN[sl][:, k * 128:(k + 1) * 128], ident),
                     reads=[f"xin{sl}", "cst"], writes=["bank0"])
            P.op("act", lambda e, k=k, hT=hT: e.activation(out=hT[:, k, :], in_=psum_t[:, 0, :], func=AF.Identity, scale=modcol[:, k:k + 1], bias=modcol[:, 8 + k:9 + k]),
                 reads=["bank0", "modcol", "modcol2"], writes=[f"hT{sp_}_{k}"])
        if s + 1 < S // TT:
            for tt_ in range(4):
                load_x(4 * (s + 1) + tt_)
        for q in range(4):
            for k in range(8):
                P.op("pe", lambda e, q=q, k=k, hT=hT: e.matmul(psum_t[:, 1, :], K_.WINU[:, k, q * 128:(q + 1) * 128], hT[:, k, :], start=(k == 0), stop=(k == 7)),
                     reads=["WINU", f"hT{sp_}_{k}"], writes=["bank1"])
            P.op("act", lambda e, q=q, XQ=XQ: e.activation(out=XQ[:, q, :], in_=psum_t[:, 1, :], func=AF.Identity, bias=b_in_c[:, 16 + q:17 + q]), reads=["bank1", "smallv"], writes=[f"XQ{sp_}_{q}"])
        for ml in range(4):
            for part in range(2):
                for q in range(4):
                    reg = (part * 4 + q) * 64
                    for k in range(8):
                        if ml < 3:
                            P.op("pe", lambda e, ml=ml, part=part, q=q, k=k, reg=reg, XQ=XQ: e.matmul(psum_t[:, 2 + ml, reg:reg + 64], K_.WinT[32 * ml:32 * ml + 32, part, q, (7 - k) * 128:(8 - k) * 128],
                                                                                              XQ[32 * ml:32 * ml + 32, q, k:TT:8], start=(k == 0), stop=(k == 7)),
                                 reads=["WinT", f"XQ{sp_}_{q}"], writes=[f"bank{2 + ml}"])
                        else:
                            P.op("pe", lambda e, ml=ml, part=part, q=q, k=k, reg=reg, XQ=XQ: e.matmul(psum_t[:, 2 + ml, reg:reg + 64], K_.WinT3[64:128, part, q, (7 - k) * 128:(8 - k) * 128],
                                                                                              XQ[64:128, q, k:TT:8], start=(k == 0), stop=(k == 7)),
                                 reads=["WinT3", f"XQ{sp_}_{q}"], writes=[f"bank{2 + ml}"])
        vbanks = [f"bank{2 + ml}" for ml in range(4)]
        Vre = psum_t[:, 2:6, 0:256].rearrange("p ml (q j) -> p q ml j", q=4)
        Vim = psum_t[:, 2:6, 256:512].rearrange("p ml (q j) -> p q ml j", q=4)
        T0, T1, T2, T3, T4, T5 = Tm
        P.op("dve", lambda e: e.tensor_tensor(v4(T0), v4(COS), Vre, op=MUL), reads=vbanks + ["s5pre"], writes=["T0"])
        P.op("dve", lambda e: e.tensor_tensor(v4(T1), v4(SIN), Vim, op=MUL), reads=vbanks + ["s5pre"], writes=["T1"])
        P.op("dve", lambda e: e.tensor_tensor(v4(T2), v4(COS), Vim, op=MUL), reads=vbanks + ["s5pre"], writes=["T2"])
        P.op("dve", lambda e: e.tensor_tensor(v4(T3), v4(SIN), Vre, op=MUL), reads=vbanks + ["s5pre"], writes=["T3"])
        P.op("pool", lambda e: e.tensor_tensor(T0, T0, T1, op=ADD), reads=["T0", "T1"], writes=["T0"])
        P.op("pool", lambda e: e.tensor_tensor(T2, T2, T3, op=SUB), reads=["T2", "T3"], writes=["T2"])
        P.op("dve", lambda e: e.tensor_tensor(CT[:, 0, :], PW[:, 8, 0, :], CAR[:, 0, :], op=MUL), reads=["car", "s5pre"], writes=["CT"])
        P.op("dve", lambda e: e.tensor_tensor(CT[:, 1, :], PW[:, 8, 1, :], CAR[:, 1, :], op=MUL), reads=["car", "s5pre"], writes=["CT"])
        P.op("dve", lambda e: e.tensor_tensor(CT[:, 0, :], CT[:, 0, :], CT[:, 1, :], op=SUB), reads=["CT"], writes=["CT"])
        P.op("dve", lambda e: e.tensor_tensor(T0[:, :, 0], T0[:, :, 0], CT[:, 0, :], op=ADD), reads=["CT", "T0"], writes=["T0"])
        P.op("dve", lambda e: e.tensor_tensor(CT[:, 0, :], PW[:, 8, 0, :], CAR[:, 1, :], op=MUL), reads=["car", "s5pre", "T0"], writes=["CT"])
        P.op("dve", lambda e: e.tensor_tensor(CT[:, 1, :], PW[:, 8, 1, :], CAR[:, 0, :], op=MUL), reads=["car", "s5pre"], writes=["CT"])
        P.op("dve", lambda e: e.tensor_tensor(CT[:, 0, :], CT[:, 0, :], CT[:, 1, :], op=ADD), reads=["CT"], writes=["CT"])
        P.op("dve", lambda e: e.tensor_tensor(T2[:, :, 0], T2[:, :, 0], CT[:, 0, :], op=ADD), reads=["CT", "T2"], writes=["T2"])
        P.op("pool", lambda e: e.tensor_copy(ST[:, :, :, 0], CAR), reads=["car"], writes=["ST"])
        P.op("dve", lambda e: e.tensor_tensor_scan(f2(T1), f2(RHO), f2(T0), 0.0, op0=MUL, op1=ADD), reads=["T0", "s5pre"], writes=["T1"])
        P.op("dve", lambda e: e.tensor_tensor_scan(f2(T3), f2(RHO), f2(T2), 0.0, op0=MUL, op1=ADD), reads=["T2", "s5pre"], writes=["T3"])
        P.op("pool", lambda e: e.tensor_tensor(T0, COS, T1, op=MUL), reads=["T1", "s5pre"], writes=["T0"])
        P.op("pool", lambda e: e.tensor_tensor(T2, SIN, T3, op=MUL), reads=["T3", "s5pre"], writes=["T2"])
        P.op("dve", lambda e: e.tensor_tensor(T4, SIN, T1, op=MUL), reads=["T1", "s5pre"], writes=["T4"])
        P.op("dve", lambda e: e.tensor_tensor(T5, COS, T3, op=MUL), reads=["T3", "s5pre"], writes=["T5"])
        P.op("pool", lambda e: e.tensor_tensor(T0, T0, T2, op=SUB), reads=["T0", "T2"], writes=["T0"])
        P.op("dve", lambda e: e.tensor_tensor(T4, T4, T5, op=ADD), reads=["T4", "T5"], writes=["T4"])
        P.op("pool", lambda e: e.tensor_copy(ST[:, 0, :, 1:NJ], T0[:, :, 0:NJ - 1]), reads=["T0"], writes=["ST"])
        P.op("pool", lambda e: e.tensor_copy(ST[:, 1, :, 1:NJ], T4[:, :, 0:NJ - 1]), reads=["T4"], writes=["ST"])
        P.op("dve", lambda e: e.tensor_copy(CAR[:, 0, :], T0[:, :, NJ - 1]), reads=["T0"], writes=["car"])
        P.op("dve", lambda e: e.tensor_copy(CAR[:, 1, :], T4[:, :, NJ - 1]), reads=["T4"], writes=["car"])
        for q in range(4):
            for k in range(8):
                lhs = XQ[:, q, k:TT:8]
                if k < 4:
                    P.op("pe", lambda e, lhs=lhs, k=k, q=q: e.matmul(psum_t[0:NJ, 6, k * 128:512], lhs, K_.KTA[:, q, 0:(4 - k) * 128], start=(k == 0), stop=False),
                         reads=[f"XQ{q}", "KTA"], writes=["bank6"])
                    P.op("pe", lambda e, lhs=lhs, k=k, q=q: e.matmul(psum_t[0:NJ, 7, :], lhs, K_.KTA[:, q, (4 - k) * 128:(8 - k) * 128], start=(k == 0), stop=False),
                         reads=[f"XQ{q}", "KTA"], writes=["bank7"])
                else:
                    P.op("pe", lambda e, lhs=lhs, k=k, q=q: e.matmul(psum_t[0:NJ, 7, (k - 4) * 128:512], lhs, K_.KTA[:, q, 0:(8 - k) * 128], start=False, stop=False),
                         reads=[f"XQ{q}", "KTA"], writes=["bank7"])
            for ml in range(4):
                m = 4 * q + ml
                for part in range(2):
                    last = (ml == 3 and part == 1)
                    for mp in range(8):
                        bb, ma = mp // 4, mp % 4
                        P.op("pe", lambda e, m=m, ml=ml, part=part, bb=bb, ma=ma, mp=mp, last=last: e.matmul(
                            psum_t[0:NJ, 6 + bb, ma * 128 + 32 * ml:ma * 128 + 32 * ml + 32], ST[:, part, m, :],
                            K_.COUT[:, part, m, mp * 40:mp * 40 + 32], start=False, stop=(last and ma == 3)),
                            reads=["ST", "COUT"], writes=[f"bank{6 + bb}"])
            P.op("act", lambda e: e.activation(out=YSC[0:NJ, :], in_=psum_t[0:NJ, 6:8, :].rearrange("p a b -> p (a b)"), func=AF.Gelu_apprx_tanh), reads=["bank6", "bank7"], writes=["YSC"])
            for mp in range(8):
                P.op("pe", lambda e, mp=mp: e.transpose(psTb[:, mp * 64:(mp + 1) * 64], YSC[0:NJ, mp * 128:(mp + 1) * 128], identb[0:NJ, 0:NJ]), reads=["YSC", "identb"], writes=["bank1"])
            P.op("dve", lambda e, q=q: e.tensor_copy(YSF[:, q, :].rearrange("p (j m) -> p m j", m=8), psTb[:, 0:512].rearrange("p (m j) -> p m j", m=8)), reads=["bank1"], writes=["YSF"])
        P.op("sp", lambda e, t0=t0: e.dma_start(out=c.ys_d[:, :, t0:t0 + TT].rearrange("q p t -> p q t"), in_=YSF), reads=["YSF"], writes=["ys_d"], dma="d_ysst")


def moe_phase(c):
    P, A, psum_t, dbg = c.P, c.A, c.psum_t, c.dbg
    cst, L, identb, onesb, ident = c.cst, c.L, c.identb, c.onesb, c.ident
    NEX = dbg.get("ne", NE)
    NBK = dbg.get("nbk", NB)
    NTC = dbg.get("ntc", NT)
    NBLK = NE * NB
    ecap1 = cst[:, 520:552]
    tokc = cst[:, 552:584]
    utri = cst[:, 128:256]
    mR = A.mark()
    if "lg" in dbg.get("inject", ()):
        P.op("sp", lambda e: e.dma_start(out=L, in_=c.lg_d), writes=["L"], dma="d_lgin")
    TB = [A.alloc([256], F32) for _ in range(2)]
    IDXF = A.alloc([NBLK], F32)
    IDX = A.alloc([NBLK], I32)
    WSL = A.alloc([NBLK], F32)
    mT_ = A.mark()
    def t3():
        return A.alloc([NT, NE], F32)
    M, E_, POS, TOT, OFF, SIDM, TMP = t3(), t3(), t3(), t3(), t3(), t3(), t3()
    Mb = A.alloc([NT * NE], BF16)
    utb = A.alloc([128], BF16)
    m8 = A.alloc([NT, 8], F32)
    s8 = A.alloc([NT, 8], F32)
    den = A.alloc([NT], F32)
    w4 = A.alloc([NT, 4], F32)
    SC = A.alloc([NT, 4, 2], F32)
    SIDI = A.alloc([NT, 4], I32)
    ZT = A.alloc([8448], F32)
    P.op("pool", lambda e: e.memset(ZT, 0.0), writes=["ZT"])
    P.op("sp", lambda e: e.dma_start(out=c.tab_d, in_=c.tabinit_d), writes=["tab_d"], dma="d_tabz")
    accv = c.acc_d.rearrange("(p r) d -> p (r d)", p=128)
    for j in range(4):
        P.op("sp", lambda e, j=j: e.dma_start(out=accv[:, j * 8448:(j + 1) * 8448], in_=ZT), reads=["ZT"], writes=[f"acc_z{j}"], dma=f"d_accz{j}")
    P.op("sp", lambda e: e.dma_start(out=c.h2_d[S:S + 128, :], in_=ZT[:, 0:512].bitcast(BF16)), reads=["ZT"], writes=["h2_pad"], dma="d_h2pad")
    P.op("dve", lambda e: e.tensor_copy(utb, utri), reads=["cst"], writes=["utb"])
    for i in range(NT):
        P.op("dve", lambda e, i=i: e.max(out=m8[:, i, :], in_=L[:, i, :]), reads=["L"], writes=["m8"])
    P.op("dve", lambda e: e.tensor_tensor(M, L, m8[:, :, 3:4].to_broadcast([128, NT, NE]), op=ALU.is_ge), reads=["L", "m8"], writes=["M"])
    P.op("dve", lambda e: e.tensor_tensor(E_, L, m8[:, :, 0:1].to_broadcast([128, NT, NE]), op=ALU.subtract), reads=["L", "m8"], writes=["E"])
    P.op("act", lambda e: e.activation(out=E_, in_=E_, func=AF.Exp), reads=["E"], writes=["E"])
    P.op("dve", lambda e: e.tensor_tensor(E_, E_, M, op=ALU.mult), reads=["E", "M"], writes=["E"])
    P.op("dve", lambda e: e.tensor_reduce(out=den, in_=E_, axis=AX.X, op=ALU.add), reads=["E"], writes=["den"])
    P.op("dve", lambda e: e.reciprocal(den, den), reads=["den"], writes=["den"])
    P.op("dve", lambda e: e.tensor_tensor(E_, E_, den.unsqueeze(2).to_broadcast([128, NT, NE]), op=ALU.mult), reads=["E", "den"], writes=["E"])
    P.op("dve", lambda e: e.tensor_copy(Mb, M.rearrange("p a b -> p (a b)")), reads=["M"], writes=["Mb"])
    for hlf in range(2):
        P.op("pe", lambda e, hlf=hlf: e.matmul(psum_t[:, hlf, :], utb, Mb[:, hlf * 512:(hlf + 1) * 512], start=True, stop=True), reads=["utb", "Mb"], writes=[f"bank{hlf}"])
        P.op("pe", lambda e, hlf=hlf: e.matmul(psum_t[:, 2 + hlf, :], onesb, Mb[:, hlf * 512:(hlf + 1) * 512], start=True, stop=True), reads=["onesb", "Mb"], writes=[f"bank{2 + hlf}"])
        P.op("dve", lambda e, hlf=hlf: e.tensor_copy(POS.rearrange("p a b -> p (a b)")[:, hlf * 512:(hlf + 1) * 512], psum_t[:, hlf, :]), reads=[f"bank{hlf}"], writes=["POS"])
        P.op("act", lambda e, hlf=hlf: e.activation(out=TOT.rearrange("p a b -> p (a b)")[:, hlf * 512:(hlf + 1) * 512], in_=psum_t[:, 2 + hlf, :], func=AF.Identity), reads=[f"bank{2 + hlf}"], writes=["TOT"])
    P.op("dve", lambda e: e.memset(OFF[:, 0, :], 0.0), writes=["OFF"])
    for i in range(1, NT):
        P.op("dve", lambda e, i=i: e.tensor_tensor(OFF[:, i, :], OFF[:, i - 1, :], TOT[:, i - 1, :], op=ALU.add), reads=["OFF", "TOT"], writes=["OFF"])
    P.op("dve", lambda e: e.tensor_tensor(POS, POS, OFF, op=ALU.add), reads=["POS", "OFF"], writes=["POS"])
    P.op("dve", lambda e: e.tensor_scalar(TMP, POS, float(CAP), None, op0=ALU.is_lt), reads=["POS"], writes=["TMP"])
    P.op("dve", lambda e: e.tensor_tensor(TMP, TMP, M, op=ALU.mult), reads=["TMP", "M"], writes=["TMP"])
    P.op("dve", lambda e: e.tensor_tensor(SIDM, POS, ecap1.unsqueeze(1).to_broadcast([128, NT, NE]), op=ALU.add), reads=["POS", "cst"], writes=["SIDM"])
    P.op("dve", lambda e: e.tensor_tensor(SIDM, SIDM, TMP, op=ALU.mult), reads=["SIDM", "TMP"], writes=["SIDM"])
    P.op("dve", lambda e: e.tensor_scalar(SIDM, SIDM, -1.0, None, op0=ALU.add), reads=["SIDM"], writes=["SIDM"])
    for i in range(NT):
        P.op("dve", lambda e, i=i: e.max(out=s8[:, i, :], in_=SIDM[:, i, :]), reads=["SIDM"], writes=["s8"])
    for k in range(4):
        P.op("dve", lambda e, k=k: e.tensor_tensor(TMP, SIDM, s8[:, :, k:k + 1].to_broadcast([128, NT, NE]), op=ALU.is_equal), reads=["SIDM", "s8"], writes=["TMP"])
        P.op("dve", lambda e: e.tensor_tensor(TMP, TMP, E_, op=ALU.mult), reads=["TMP", "E"], writes=["TMP"])
        P.op("dve", lambda e, k=k: e.tensor_reduce(out=w4[:, :, k], in_=TMP, axis=AX.X, op=ALU.add), reads=["TMP"], writes=["w4"])
    for k in range(4):
        P.op("dve", lambda e, k=k: e.tensor_copy(SC[:, :, k, 0], tokc), reads=["cst"], writes=["SC"])
    P.op("dve", lambda e: e.tensor_copy(SC[:, :, :, 1], w4), reads=["w4"], writes=["SC"])
    P.op("dve", lambda e: e.tensor_copy(SIDI, s8[:, :, 0:4]), reads=["s8"], writes=["SIDI"])
    regc = {}

    def breg(e):
        if "r" not in regc:
            regc["r"] = e.to_reg(NE * CAP - 1)
        return regc["r"]

    for i in range(NT):
        for k in range(4):
            P.op("pool", lambda e, i=i, k=k: e.indirect_dma_start(out=c.tab_d, out_offset=bass.IndirectOffsetOnAxis(ap=SIDI[:, i, k:k + 1], axis=0),
                                                                  in_=SC[:, i, k, :], in_offset=None, bounds_check=breg(e), oob_is_err=False),
                 reads=["SC", "SIDI", "tab_d"], dma="d_scat")
    P.barrier()
    if dbg.get("mstop") == "route":
        return
    tabv = c.tab_d.rearrange("(b s) two -> b (s two)", s=128)
    for hb_ in range(2):
        P.op("sp", lambda e, hb_=hb_: e.dma_start(out=TB[hb_], in_=tabv[hb_ * 128:(hb_ + 1) * 128, :]), writes=[f"TB{hb_}"], dma=f"d_tb{hb_}")
        tv = TB[hb_].rearrange("p (s two) -> p s two", two=2)
        P.op("pe", lambda e, hb_=hb_, tv=tv: e.transpose(psum_t[:, hb_, 0:128], tv[:, :, 0], ident), reads=[f"TB{hb_}", "cst"], writes=[f"bank{hb_}"])
        P.op("pe", lambda e, hb_=hb_, tv=tv: e.transpose(psum_t[:, hb_, 128:256], tv[:, :, 1], ident), reads=[f"TB{hb_}", "cst"], writes=[f"bank{hb_}"])
        P.op("dve", lambda e, hb_=hb_: e.tensor_copy(IDXF[:, hb_ * 128:(hb_ + 1) * 128], psum_t[:, hb_, 0:128]), reads=[f"bank{hb_}"], writes=["IDXF"])
        P.op("dve", lambda e, hb_=hb_: e.tensor_copy(WSL[:, hb_ * 128:(hb_ + 1) * 128], psum_t[:, hb_, 128:256]), reads=[f"bank{hb_}"], writes=["WSL"])
    P.op("dve", lambda e: e.tensor_copy(IDX, IDXF), reads=["IDXF"], writes=["IDX"])
    A.reset(mT_)
    mE = A.mark()
    if dbg.get("mstop") == "table":
        return
    WGU = [A.alloc([8, 2 * D], BF16) for _ in range(2)]
    WDN = [A.alloc([8, D], BF16) for _ in range(2)]
    BGU = A.alloc([NE, 16], F32)
    BDB = [A.alloc([D], F32) for _ in range(2)]
    NXG = 8
    XG = [A.alloc([D], BF16) for _ in range(NXG)]
    XT = [A.alloc([8, 512], BF16) for _ in range(2)]
    Gt = [A.alloc([512], F32) for _ in range(2)]
    Ut = [A.alloc([512], F32) for _ in range(2)]
    SG = [A.alloc([512], F32) for _ in range(2)]
    ACT_ = [A.alloc([8, 512], BF16) for _ in range(2)]
    TY = [A.alloc([D], F32) for _ in range(2)]
    YS = [A.alloc([D], F32) for _ in range(4)]
    P.op("sp", lambda e: e.dma_start(out=BGU, in_=c.b_gu_d), writes=["BGU"], dma="d_bgu")
    P.op("dve", lambda e: e.tensor_scalar(BGU[:, :, 8:16], BGU[:, :, 8:16], 1.0, None, op0=ALU.add), reads=["BGU"], writes=["BGU"])

    def load_w(e_):
        bf = e_ % 2
        gv = c.w_gu_d[e_].rearrange("(k p) n -> p k n", p=128)
        for hk in range(2):
            P.op("pool", lambda e, bf=bf, hk=hk, gv=gv: e.dma_start(out=WGU[bf][:, hk * 4:(hk + 1) * 4, :], in_=gv[:, hk * 4:(hk + 1) * 4, :]),
                 writes=[f"WGU{bf}_{hk}"], dma=f"d_wgu{bf}_{hk}")
        P.op("pool", lambda e, bf=bf, e_=e_: e.dma_start(out=WDN[bf], in_=c.w_down_d[e_].rearrange("(k p) n -> p k n", p=128)), writes=[f"WDN{bf}"], dma=f"d_wdn{bf}")
        P.op("sp", lambda e, bf=bf, e_=e_: e.dma_start(out=BDB[bf], in_=c.b_down_d[e_:e_ + 1, :].partition_broadcast(128)), writes=[f"BDB{bf}"], dma=f"d_bdb{bf}")

    NGRP = NBK // 4
    groups = [(e_, gq) for e_ in range(NEX) for gq in range(NGRP)]
    psTs = [psum_t[:, 0, :].bitcast(BF16), psum_t[:, 7, :].bitcast(BF16)]
    ycnt = [0]
    tcnt = [0]
    deferred = []

    def gather(gi, defer=False):
        e_, gq = groups[gi]
        for bl in range(4):
            blk = e_ * NB + gq * 4 + bl
            sl = (gi * 4 + bl) % NXG

            def rec(sl=sl, blk=blk):
                P.op("pool", lambda e, sl=sl, blk=blk: e.indirect_dma_start(out=XG[sl], out_offset=None, in_=c.h2_d,
                                                                            in_offset=bass.IndirectOffsetOnAxis(ap=IDX[:, blk:blk + 1], axis=0)),
                     reads=["IDX", "h2_pad"], writes=[f"XG{sl}"], dma=f"d_xg{sl}")
            if defer:
                deferred.append(rec)
            else:
                rec()

    def front(gi):
        gb = gi % 2
        for bl in range(4):
            sl = (gi * 4 + bl) % NXG
            ti = tcnt[0] % 2
            tcnt[0] += 1
            psT = psTs[ti]
            tkey = "bank0" if ti == 0 else "bank7"
            for k in range(8):
                P.op("pe", lambda e, k=k, sl=sl, psT=psT: e.transpose(psT[:, k * 128:(k + 1) * 128], XG[sl][:, k * 128:(k + 1) * 128], identb), reads=[f"XG{sl}", "identb"], writes=[tkey])
            P.op("act", lambda e, gb=gb, bl=bl, psT=psT: e.activation(out=XT[gb][:, :, bl * 128:(bl + 1) * 128], in_=psT.rearrange("p (k s) -> p k s", k=8), func=AF.Identity),
                 reads=[tkey], writes=[f"XT{gb}"])

    def gu(gi):
        e_, gq = groups[gi]
        gb = gi % 2
        bf = e_ % 2
        for j in range(8):
            tb = j % 2
            bG, bU = 1 + 2 * tb, 2 + 2 * tb
            for k in range(8):
                P.op("pe", lambda e, bG=bG, j=j, k=k, bf=bf, gb=gb: e.matmul(psum_t[:, bG, :], WGU[bf][:, k, j * 128:(j + 1) * 128], XT[gb][:, k, :], start=(k == 0), stop=(k == 7)),
                     reads=[f"WGU{bf}_{k // 4}", f"XT{gb}"], writes=[f"bank{bG}"])
            for k in range(8):
                P.op("pe", lambda e, bU=bU, j=j, k=k, bf=bf, gb=gb: e.matmul(psum_t[:, bU, :], WGU[bf][:, k, (8 + j) * 128:(9 + j) * 128], XT[gb][:, k, :], start=(k == 0), stop=(k == 7)),
                     reads=[f"WGU{bf}_{k // 4}", f"XT{gb}"], writes=[f"bank{bU}"])
            P.op("dve", lambda e, bG=bG, j=j, e_=e_, tb=tb: e.tensor_scalar(Gt[tb], psum_t[:, bG, :], BGU[:, e_, j:j + 1], 7.0, op0=ALU.add, op1=ALU.min),
                 reads=[f"bank{bG}", "BGU"], writes=[f"G{tb}"])
            P.op("dve", lambda e, bU=bU, j=j, e_=e_, tb=tb: e.tensor_scalar(Ut[tb], psum_t[:, bU, :], BGU[:, e_, 8 + j:9 + j], 8.0, op0=ALU.add, op1=ALU.min),
                 reads=[f"bank{bU}", "BGU"], writes=[f"U{tb}"])
            P.op("act", lambda e, tb=tb: e.activation(out=SG[tb], in_=Gt[tb], func=AF.Silu, scale=1.702), reads=[f"G{tb}"], writes=[f"SG{tb}"])
            P.op("dve", lambda e, tb=tb, gb=gb, j=j: e.scalar_tensor_tensor(ACT_[gb][:, j, :], Ut[tb], -6.0, SG[tb], op0=ALU.max, op1=ALU.mult),
                 reads=[f"SG{tb}", f"U{tb}"], writes=[f"ACT{gb}_{j}"])
            if deferred:
                deferred.pop(0)()

    def down(gi):
        e_, gq = groups[gi]
        gb = gi % 2
        bf = e_ % 2
        for bl in range(4):
            blk = e_ * NB + gq * 4 + bl
            pb = ycnt[0] % 2
            ycnt[0] += 1
            yb_ = bl
            for hlf in range(2):
                bank = 5 + hlf
                for fc in range(8):
                    P.op("pe", lambda e, bank=bank, hlf=hlf, fc=fc, gb=gb, bf=bf, bl=bl: e.matmul(psum_t[:, bank, :], ACT_[gb][:, fc, bl * 128:(bl + 1) * 128], WDN[bf][:, fc, hlf * 512:(hlf + 1) * 512],
                                                                                                 start=(fc == 0), stop=(fc == 7)),
                         reads=[f"ACT{gb}_{fc}", f"WDN{bf}"], writes=[f"bank{bank}"])
                P.op("dve", lambda e, bank=bank, hlf=hlf, bf=bf, pb=pb: e.scalar_tensor_tensor(TY[pb][:, hlf * 512:(hlf + 1) * 512], psum_t[:, bank, :], 1.0 / 1.702, BDB[bf][:, hlf * 512:(hlf + 1) * 512], op0=ALU.mult, op1=ALU.add),
                     reads=[f"bank{bank}", f"BDB{bf}"], writes=[f"TY{pb}"])
            P.op("act", lambda e, pb=pb, yb_=yb_, blk=blk: e.activation(out=YS[yb_], in_=TY[pb], func=AF.Copy, scale=WSL[:, blk:blk + 1]), reads=[f"TY{pb}", "WSL"], writes=[f"YS{yb_}"])
            if "yb" in dbg.get("dump", ()):
                P.op("sp", lambda e, yb_=yb_, blk=blk: e.dma_start(out=c.yb_d[blk * 128:(blk + 1) * 128, :], in_=YS[yb_]), reads=[f"YS{yb_}"], writes=["yb_d"], dma=f"d_ybst{yb_}")

            def rec(yb_=yb_, blk=blk, first=(gq == 0 and bl == 0), e_=e_):
                P.op("pool", lambda e, yb_=yb_, blk=blk: e.indirect_dma_start(out=c.acc_d, out_offset=bass.IndirectOffsetOnAxis(ap=IDX[:, blk:blk + 1], axis=0),
                                                                            in_=YS[yb_], in_offset=None, compute_op=ALU.add),
                     reads=[f"YS{yb_}", "IDX"] + [f"acc_z{q_}" for q_ in range(4)],
                     writes=["acc_d"], dma=f"d_acc{yb_}")
            deferred.append(rec)

    load_w(0)
    gather(0)
    front(0)
    if len(groups) > 1:
        gather(1)
    for gi in range(len(groups)):
        e_, gq = groups[gi]
        gu(gi)
        while deferred:
            deferred.pop(0)()
        if gi + 1 < len(groups):
            front(gi + 1)
        if gq == 0 and e_ + 1 < NEX:
            load_w(e_ + 1)
        down(gi)
        if gi + 2 < len(groups):
            gather(gi + 2, defer=True)
    while deferred:
        deferred.pop(0)()
    P.barrier()
    if dbg.get("mstop") == "experts":
        return
    A.reset(mR)
    G2B = A.alloc([D], F32)
    LN2G = A.alloc([D], F32)
    LN2B = A.alloc([D], F32)
    GA = [A.alloc([D], F32) for _ in range(2)]
    X1T = [A.alloc([D], F32) for _ in range(2)]
    VV = [A.alloc([D], F32) for _ in range(2)]
    STATS = A.alloc([2, 2, 6], F32)
    MV = A.alloc([2, 2], F32)
    RSTD = A.alloc([2, 1], F32)
    MHALF = A.alloc([1], F32)
    P.op("pool", lambda e: e.memset(MHALF, -0.5), writes=["mhalf"])
    P.op("sp", lambda e: e.dma_start(out=G2B, in_=c.mod_d[0:1, 5 * D:6 * D].partition_broadcast(128)), writes=["G2B"], dma="d_g2b")
    P.op("sp", lambda e: e.dma_start(out=LN2G, in_=c.lnrows_d[2:3, :].partition_broadcast(128)), writes=["LN2G"], dma="d_ln2g")
    P.op("sp", lambda e: e.dma_start(out=LN2B, in_=c.lnrows_d[3:4, :].partition_broadcast(128)), writes=["LN2B"], dma="d_ln2b")
    P.op("dve", lambda e: e.tensor_scalar(G2B, G2B, 1.0, None, op0=ALU.add), reads=["G2B"], writes=["G2B"])
    def comb(i):
        pb = i % 2
        P.op("sp", lambda e, i=i, pb=pb: e.dma_start(out=GA[pb], in_=c.acc_d[i * 128:(i + 1) * 128, :]), writes=[f"GA{pb}"], dma=f"d_ga{pb}")
        P.op("sp", lambda e, i=i, pb=pb: e.dma_start(out=X1T[pb], in_=c.x1_d[i * 128:(i + 1) * 128, :]), writes=[f"X1T{pb}"], dma=f"d_x1t{pb}")
        g, v = GA[pb], VV[pb]
        P.op("pool", lambda e, g=g: e.tensor_tensor(g, g, G2B, op=ALU.mult), reads=[f"GA{pb}", "G2B"], writes=[f"GA{pb}"])
        P.op("dve", lambda e, g=g, v=v, pb=pb: e.scalar_tensor_tensor(v, X1T[pb], ALPHA, g, op0=ALU.mult, op1=ALU.add), reads=[f"X1T{pb}", f"GA{pb}"], writes=[f"VV{pb}"])
        for hlf in range(2):
            P.op("dve", lambda e, v=v, hlf=hlf, pb=pb: e.bn_stats(STATS[:, pb, hlf, :], v[:, hlf * 512:(hlf + 1) * 512]), reads=[f"VV{pb}"], writes=[f"stats{pb}"])
        P.op("dve", lambda e, pb=pb: e.bn_aggr(MV[:, pb, :], STATS[:, pb, :, :].rearrange("p a b -> p (a b)")), reads=[f"stats{pb}"], writes=[f"mv{pb}"])
        P.op("pool", lambda e, pb=pb: e.tensor_scalar(RSTD[:, pb, :], MV[:, pb, 1:2], EPS, None, op0=ALU.add), reads=[f"mv{pb}"], writes=[f"rstd{pb}"])
        P.op("pool", lambda e, pb=pb: e.tensor_tensor(RSTD[:, pb, :], RSTD[:, pb, :], MHALF, op=ALU.pow), reads=[f"rstd{pb}", "mhalf"], writes=[f"rstd{pb}"])
        P.op("dve", lambda e, v=v, pb=pb: e.tensor_scalar(v, v, MV[:, pb, 0:1], RSTD[:, pb, :], op0=ALU.subtract, op1=ALU.mult), reads=[f"VV{pb}", f"mv{pb}", f"rstd{pb}"], writes=[f"VV{pb}"])
        P.op("dve", lambda e, v=v: e.tensor_tensor(v, v, LN2G, op=ALU.mult), reads=[f"VV{pb}", "LN2G"], writes=[f"VV{pb}"])
        P.op("pool", lambda e, v=v: e.tensor_tensor(v, v, LN2B, op=ALU.add), reads=[f"VV{pb}", "LN2B"], writes=[f"VV{pb}"])
        P.op("sp", lambda e, i=i, v=v: e.dma_start(out=c.out_d[i * 128:(i + 1) * 128, :], in_=v), reads=[f"VV{pb}"], writes=["out_d"], dma=f"d_out{pb}")

    for i in range(0, NTC, 2):
        interleave(P, [lambda i=i: comb(i)] + ([lambda i=i: comb(i + 1)] if i + 1 < NTC else []))


def _consts():
    cst = np.zeros((128, 640), np.float32)
    cst[:, 0:128] = np.eye(128, dtype=np.float32)
    cst[:, 128:256] = np.triu(np.ones((128, 128), np.float32), 1)
    idx = np.arange(128)
    cst[:, 256:384] = (idx[:, None] // 16 == idx[None, :] // 16).astype(np.float32)
    cst[:, 384:512] = 1.0
    cst[:, 512] = (idx < 64).astype(np.float32)
    cst[:, 513] = (idx >= 64).astype(np.float32)
    cst[:, 514] = idx.astype(np.float32)
    cst[:, 520:552] = (np.arange(32) * CAP + 1)[None, :].astype(np.float32)
    cst[:, 552:584] = (idx[:, None] + 128 * np.arange(32)[None, :]).astype(np.float32)
    return cst


def prep_core_inputs(inp, b):
    f = lambda a: np.ascontiguousarray(np.asarray(a, dtype=np.float32))
    m = {}
    m["x"] = f(inp["x"][b])
    m["ccol"] = f(inp["c"][b].reshape(8, 128).T)
    m["w_ada"] = f(inp["w_ada"][0])
    m["b_ada"] = f(inp["b_ada"][0][None, :])
    m["w_in"] = f(inp["w_in"][0])
    sv = np.concatenate([
        inp["b_in"][0].reshape(36, 128),
        inp["conv_w"][0].reshape(4, 8, 128).transpose(1, 0, 2).reshape(32, 128),
        inp["conv_b"][0].reshape(8, 128), inp["b_rg_a"][0].reshape(8, 128), inp["b_rg_x"][0].reshape(8, 128),
        inp["lru_lambda"][0].reshape(8, 128)], axis=0)
    m["smallv"] = f(sv.T)
    m["w_rg"] = f(np.stack([inp["w_rg_a"][0], inp["w_rg_x"][0]], axis=0))
    m["w_rnn_out"] = f(inp["w_rnn_out"][0])
    m["w_glu"] = f(inp["w_glu"][0])
    m["w_out"] = f(inp["w_out"][0])
    m["lnrows"] = f(np.stack([inp["ln1_g"][0], inp["ln1_b"][0], inp["ln2_g"][0], inp["ln2_b"][0]], axis=0))
    m["w_router"] = f(inp["w_router"][0])
    m["b_router"] = f(inp["b_router"][0][None, :])
    m["w_gu"] = f(inp["w_gu"][0])
    m["b_gu"] = f(np.asarray(inp["b_gu"][0]).reshape(32, 16, 128).transpose(2, 0, 1))
    m["w_down"] = f(inp["w_down"][0])
    m["b_down"] = f(inp["b_down"][0])
    lane = lambda a: np.asarray(a).reshape(16, 128).T
    ldt = np.repeat(np.asarray(inp["s5_log_dt"][0]), 64).reshape(16, 128).T
    m["s5lam"] = f(np.stack([lane(inp["s5_lambda_re"][0]), lane(inp["s5_lambda_im"][0]), ldt], axis=1))
    lb = lambda a: np.asarray(a).reshape(16, 128, 16).transpose(1, 0, 2)
    m["s5b"] = f(np.stack([lb(inp["s5_b_re"][0]), lb(inp["s5_b_im"][0])], axis=1))
    lc = lambda a: np.asarray(a).reshape(16, 2, 16, 64).transpose(1, 3, 0, 2).reshape(128, 16, 16)
    m["s5c"] = f(np.stack([lc(inp["s5_c_re"][0]), lc(inp["s5_c_im"][0])], axis=1))
    m["s5d"] = f(np.asarray(inp["s5_d"][0]).reshape(4, 128).T)
    m["cst"] = _consts()
    ti = np.zeros((NE * CAP, 2), np.float32)
    ti[:, 0] = S + (np.arange(NE * CAP) % 128)
    m["tabinit"] = ti
    return m


_NC_CACHE = {}


def kernel(**inputs):
    if "nc" not in _NC_CACHE:
        _NC_CACHE["nc"] = build_program()
    nc = _NC_CACHE["nc"]
    in_maps = [prep_core_inputs(inputs, b) for b in range(8)]
    res = run_bass_kernel_spmd(nc, in_maps, core_ids=list(range(8)))
    return np.stack([np.asarray(r["out"], dtype=np.float32) for r in res.results], axis=0)
```

```python
import contextlib
import types
import numpy as np
import ml_dtypes
import concourse.bass as bass
import concourse.mybir as mybir
from concourse.bass_utils import run_bass_kernel_spmd

F32 = mybir.dt.float32
BF16 = mybir.dt.bfloat16
I32 = mybir.dt.int32
U8 = mybir.dt.uint8
ALU = mybir.AluOpType
AF = mybir.ActivationFunctionType
AX = mybir.AxisListType

D = 1024
S = 4096
NT = S // 128
NE = 32
CAP = 1024
NB = CAP // 128
ALPHA = 2.0 ** 0.25
EPS = 1e-5
ENGS = ("pe", "act", "dve", "pool", "sp")
EPOCH = 12000


class Prog:
    def __init__(self, nc):
        self.nc = nc
        self.ops = {e: [] for e in ENGS}
        self.cnt = {e: 0 for e in ENGS}
        self.res = {}
        self.dmacnt = {}
        self.waited = {e: {} for e in ENGS}
        self.semnames = []

    def _sem(self, name):
        if name not in self.semnames:
            self.semnames.append(name)
        return name

    def _tok_engine(self, eng):
        self.cnt[eng] += 1
        c = self.cnt[eng]
        ep = (c - 1) // EPOCH
        return (self._sem(f"E{eng}{ep}"), c - ep * EPOCH, eng)

    def op(self, eng, fn, reads=(), writes=(), dma=None):
        waits = []
        for k in reads:
            st = self.res.get(k)
            if st and st["w"] is not None:
                waits.append(st["w"])
        for k in writes:
            st = self.res.get(k)
            if st:
                if st["w"] is not None:
                    waits.append(st["w"])
                waits.extend(st["r"])
        if dma is not None:
            self.dmacnt[dma] = self.dmacnt.get(dma, 0) + 16
            tok = (self._sem(dma), self.dmacnt[dma], "dma")
        else:
            tok = self._tok_engine(eng)
        need = []
        for (s, v, e) in waits:
            if e == eng and dma is None and eng == "pe":
                continue
            if self.waited[eng].get(s, 0) >= v:
                continue
            self.waited[eng][s] = v
            need.append((s, v))
        for k in reads:
            st = self.res.setdefault(k, {"w": None, "r": []})
            st["r"].append(tok)
        for k in writes:
            self.res[k] = {"w": tok, "r": []}
        self.ops[eng].append((need, fn, (tok[0], 16 if dma is not None else 1)))
        return tok

    def barrier(self):
        toks = []
        for e in ENGS:
            if self.cnt[e] > 0:
                c = self.cnt[e]
                ep = (c - 1) // EPOCH
                toks.append((f"E{e}{ep}", c - ep * EPOCH))
        for s, v in self.dmacnt.items():
            toks.append((s, v))
        for e in ENGS:
            need = []
            for (s, v) in toks:
                if s.startswith(f"E{e}"):
                    continue
                if self.waited[e].get(s, 0) >= v:
                    continue
                self.waited[e][s] = v
                need.append((s, v))
            if need:
                self.ops[e].append((need, None, None))

    def emit(self):
        nc = self.nc
        with contextlib.ExitStack() as es:
            sems = {n: es.enter_context(nc.semaphore(n)) for n in self.semnames}
            block = es.enter_context(nc.Block())

            def run(engname, eng):
                for (need, fn, inc) in self.ops[engname]:
                    for (s, v) in need:
                        eng.wait_ge(sems[s], v)
                    if fn is not None:
                        fn(eng).then_inc(sems[inc[0]], inc[1])

            @block.sync
            def _(e):
                run("sp", e)

            @block.scalar
            def _(e):
                run("act", e)

            @block.vector
            def _(e):
                run("dve", e)

            @block.gpsimd
            def _(e):
                run("pool", e)

            @block.tensor
            def _(e):
                run("pe", e)


def interleave(P, builders):
    chains = []
    prev = P.__dict__.get("op")
    for b in builders:
        lst = []
        P.op = lambda *a, _l=lst, **k: _l.append((a, k))
        try:
            b()
        finally:
            if prev is None:
                del P.op
            else:
                P.op = prev
        chains.append(lst)
    n = max(len(l) for l in chains)
    for i in range(n):
        for l in chains:
            if i < len(l):
                P.op(*l[i][0], **l[i][1])


class Arena:
    def __init__(self, t, size):
        self.t = t
        self.size = size
        self.off = 0

    def mark(self):
        return self.off

    def reset(self, m):
        self.off = m

    def alloc(self, shape, dt):
        esz = {F32: 4, BF16: 2, I32: 4, U8: 1}[dt]
        n = int(np.prod(shape))
        nb = (n * esz + 31) // 32 * 32
        assert self.off + nb <= self.size, f"arena overflow {self.off + nb} > {self.size}"
        v = self.t[:, self.off:self.off + n * esz].bitcast(dt)
        self.off += nb
        if len(shape) == 2:
            v = v.rearrange("p (a b) -> p a b", a=shape[0])
        elif len(shape) == 3:
            v = v.rearrange("p (a b c) -> p a b c", a=shape[0], b=shape[1])
        return v


def build_program(dbg=None):
    dbg = dbg or {}
    stop = dbg.get("stop", "end")
    nc = bass.Bass("TRN2", target_bir_lowering=False)
    P = Prog(nc)
    outs_dbg = {}

    def dram_in(name, shape, dt=F32):
        return nc.dram_tensor(name, list(shape), dt, kind="ExternalInput").ap()

    def dram_scr(name, shape, dt, inject=False):
        if inject and name in dbg.get("inject", ()):
            kind = "ExternalInput"
        elif name in dbg.get("dump", ()):
            kind = "ExternalOutput"
        else:
            kind = "Internal"
        return nc.dram_tensor(name, list(shape), dt, kind=kind).ap()

    x_d = dram_in("x", [S, D])
    ccol_d = dram_in("ccol", [128, 8])
    w_ada_d = dram_in("w_ada", [D, 6 * D])
    b_ada_d = dram_in("b_ada", [1, 6 * D])
    w_in_d = dram_in("w_in", [D, 4608])
    smallv_d = dram_in("smallv", [128, 100])
    w_rg_d = dram_in("w_rg", [2, 8, 128, 128])
    w_rnn_out_d = dram_in("w_rnn_out", [D, D])
    w_glu_d = dram_in("w_glu", [512, 2 * D])
    w_out_d = dram_in("w_out", [D, D])
    lnrows_d = dram_in("lnrows", [4, D])
    w_router_d = dram_in("w_router", [D, NE])
    b_router_d = dram_in("b_router", [1, NE])
    w_gu_d = dram_in("w_gu", [NE, D, 2 * D])
    b_gu_d = dram_in("b_gu", [128, NE, 16])
    w_down_d = dram_in("w_down", [NE, D, D])
    b_down_d = dram_in("b_down", [NE, D])
    s5lam_d = dram_in("s5lam", [128, 3, 16])
    s5b_d = dram_in("s5b", [128, 2, 16, 16])
    s5c_d = dram_in("s5c", [128, 2, 16, 16])
    s5d_d = dram_in("s5d", [128, 4])
    cst_d = dram_in("cst", [128, 640])
    out_d = nc.dram_tensor("out", [S, D], F32, kind="ExternalOutput").ap()

    ys_d = dram_scr("ys", [4, 128, S], BF16, inject=True)
    x1_d = dram_scr("x1", [S, D], F32, inject=True)
    h2_d = dram_scr("h2", [S + 128, D], BF16, inject=True)
    lg_d = dram_scr("lg", [128, NT, NE], F32, inject=True)
    tab_d = dram_scr("tab", [NE * CAP, 2], F32)
    yb_d = dram_scr("yb", [NE * CAP if "yb" in dbg.get("dump", ()) else 128, D], F32)
    acc_d = dram_scr("acc", [S + 128, D], F32)
    tabinit_d = dram_in("tabinit", [NE * CAP, 2])
    mod_d = dram_scr("modr", [1, 6 * D], F32)

    ARENA = 197 * 1024
    with contextlib.ExitStack() as es:
        arena_t = es.enter_context(nc.sbuf_tensor("arena", [128, ARENA], U8))
        pers_t = es.enter_context(nc.sbuf_tensor("pers", [128, 10 * 1024], U8))
        psum_t = es.enter_context(nc.psum_tensor("ps", [128, 8, 512], F32))
        A = Arena(arena_t, ARENA)
        PA = Arena(pers_t, 10 * 1024)

        cst = PA.alloc([640], F32)
        ident = cst[:, 0:128]
        smallv = PA.alloc([100], F32)
        modcol = PA.alloc([16], F32)
        cf = PA.alloc([8], F32)
        L = PA.alloc([NT, NE], F32)
        identb = PA.alloc([128], BF16)
        onesb = PA.alloc([128], BF16)
        ones_f = cst[:, 384:512]
        epsc = PA.alloc([1], F32)

        b_in_c = smallv[:, 0:36]
        conv_w_c = smallv[:, 36:68]
        conv_b_c = smallv[:, 68:76]
        b_rga_c = smallv[:, 76:84]
        b_rgx_c = smallv[:, 84:92]
        lam_c = smallv[:, 92:100]

        def PS(bank, lo=0, hi=512):
            return psum_t[:, bank, lo:hi]

        P.op("sp", lambda e: e.dma_start(out=cst, in_=cst_d), writes=["cst"], dma="d_cst")
        P.op("sp", lambda e: e.dma_start(out=smallv, in_=smallv_d), writes=["smallv"], dma="d_smallv")
        P.op("dve", lambda e: e.tensor_copy(identb, ident), reads=["cst"], writes=["identb"])
        P.op("dve", lambda e: e.tensor_copy(onesb, ones_f), reads=["cst"], writes=["onesb"])
        P.op("dve", lambda e: e.memset(epsc, EPS), writes=["epsc"])
        P.op("dve", lambda e: e.memset(L, 0.0), writes=["L"])

        mark0 = A.mark()
        K5 = None
        if "ys" not in dbg.get("inject", ()):
            K5 = s5_prepare(types.SimpleNamespace(**locals()))
        mA = A.mark()
        ccol = A.alloc([8], F32)
        cact = A.alloc([8], F32)
        modrow = A.alloc([6 * D], F32)
        wada = [A.alloc([8, 512], F32) for _ in range(2)]
        P.op("sp", lambda e: e.dma_start(out=ccol, in_=ccol_d), writes=["ccol"], dma="d_ccol")
        P.op("sp", lambda e: e.dma_start(out=modrow[0:1, :], in_=b_ada_d), writes=["modrow"], dma="d_bada")
        P.op("act", lambda e: e.activation(out=cact, in_=ccol, func=AF.Silu), reads=["ccol"], writes=["cact"])
        wada_v = w_ada_d.rearrange("(k p) n -> p k n", p=128)
        for j in range(12):
            buf = wada[j % 2]
            P.op("sp", lambda e, buf=buf, j=j: e.dma_start(out=buf, in_=wada_v[:, :, j * 512:(j + 1) * 512]),
                 writes=[f"wada{j % 2}"], dma=f"d_wada{j % 2}")
            bank = j % 2
            for k in range(8):
                P.op("pe", lambda e, buf=buf, k=k, bank=bank: e.matmul(PS(bank)[0:1, :], cact[:, k:k + 1], buf[:, k, :],
                                                                      start=(k == 0), stop=(k == 7)),
                     reads=[f"wada{j % 2}", "cact"], writes=[f"ps{bank}"])
            P.op("dve", lambda e, j=j, bank=bank: e.tensor_tensor(modrow[0:1, j * 512:(j + 1) * 512], PS(bank)[0:1, :],
                                                                 modrow[0:1, j * 512:(j + 1) * 512], op=ALU.add),
                 reads=[f"ps{bank}", "modrow"], writes=["modrow"])
        P.op("act", lambda e: e.activation(out=cf, in_=lam_c, func=AF.Exp, scale=-1.0), reads=["smallv"], writes=["cf"])
        P.op("act", lambda e: e.activation(out=cf, in_=cf, func=AF.Ln, bias=1.0), reads=["cf"], writes=["cf"])
        P.op("dve", lambda e: e.tensor_scalar(cf, cf, -8.0, None, op0=ALU.mult), reads=["cf"], writes=["cf"])

        P.op("sp", lambda e: e.dma_start(out=mod_d, in_=modrow[0:1, :]), reads=["modrow"], writes=["mod_d"], dma="d_modst")
        P.op("sp", lambda e: e.dma_start(out=modcol[:, 0:8], in_=mod_d[0:1, D:2 * D].rearrange("o (k p) -> p (o k)", p=128), allow_slow_non_contiguous=True),
             reads=["mod_d"], writes=["modcol"], dma="d_mc0")
        P.op("sp", lambda e: e.dma_start(out=modcol[:, 8:16], in_=mod_d[0:1, 0:D].rearrange("o (k p) -> p (o k)", p=128), allow_slow_non_contiguous=True),
             reads=["mod_d"], writes=["modcol2"], dma="d_mc1")
        P.op("dve", lambda e: e.tensor_scalar(modcol[:, 0:8], modcol[:, 0:8], 1.0, None, op0=ALU.add), reads=["modcol"], writes=["modcol"])
        P.barrier()
        A.reset(K5.mark if K5 is not None else mark0)

        if stop == "phaseA":
            return finish(nc, P, out_d, dbg)

        if "ys" not in dbg.get("inject", ()):
            s5_pass(types.SimpleNamespace(**locals()))
            P.barrier()
        A.reset(mark0)
        if stop == "s5":
            return finish(nc, P, out_d, dbg)

        if "x1" not in dbg.get("inject", ()):
            (mixer_pass2 if dbg.get('newmixer') else mixer_pass)(types.SimpleNamespace(**locals()))
            P.barrier()
            A.reset(mark0)
        if stop == "mixer":
            return finish(nc, P, out_d, dbg)

        moe_phase(types.SimpleNamespace(**locals()))
        return finish(nc, P, out_d, dbg)


def finish(nc, P, out_d, dbg):
    P.barrier()
    P.emit()
    return nc


def mixer_pass(c):
    P, A, psum_t = c.P, c.A, c.psum_t
    ident, smallv, modcol, cf, L = c.ident, c.smallv, c.modcol, c.cf, c.L
    TT = 256
    NST = S // TT
    NX = 2
    WIN = A.alloc([8, 4096], BF16)
    WRO = A.alloc([8, D], BF16)
    WGL = A.alloc([4, 2 * D], BF16)
    WOU = A.alloc([8, D], BF16)
    WRG = A.alloc([2, 8, 128], BF16)
    WR = A.alloc([8, NE], F32)
    LN1G = A.alloc([D], F32)
    LN1B = A.alloc([D], F32)
    P1 = A.alloc([D], F32)
    P2 = A.alloc([D], F32)
    BRB = A.alloc([NE], F32)
    XIN = [A.alloc([D], F32) for _ in range(NX)]
    hTs = [A.alloc([8, TT], BF16) for _ in range(2)]
    YSTs = [A.alloc([4, TT], BF16) for _ in range(2)]
    zAs = [A.alloc([8, TT], BF16) for _ in range(2)]
    XRES = A.alloc([D], F32)
    mT = A.alloc([8, TT], BF16)
    TS = []
    for i in range(2):
        TS.append(dict(xc=A.alloc([TT + 8], F32), xr=A.alloc([TT], F32), xrb=A.alloc([TT], BF16), thr=A.alloc([TT], F32),
                       thi=A.alloc([TT], F32), a2=A.alloc([TT], F32), ix=A.alloc([TT], F32)))
        TS[-1]["gy"] = TS[-1]["xc"][:, 0:TT]
        TS[-1]["hs"] = TS[-1]["thi"]
    MTS = [dict(t0=A.alloc([TT], F32), t1=A.alloc([TT], F32), tb=A.alloc([TT], F32)) for _ in range(2)]
    for M__ in MTS:
        M__['ta'] = M__['t0']
        M__['tbb'] = M__['tb']
    V = A.alloc([D], F32)
    H2 = A.alloc([D], F32)
    H2T = V.rearrange("p (k t) -> p k t", k=8)
    HALO = A.alloc([8, 4], F32)
    CARRY = A.alloc([8], F32)
    hbias = A.alloc([36], F32)
    hbrg = A.alloc([16], F32)
    cfh = A.alloc([8], F32)
    STATS = A.alloc([2, 6], F32)
    MV = A.alloc([2], F32)
    RSTD = A.alloc([1], F32)
    MHALF = A.alloc([1], F32)

    b_in_c = smallv[:, 0:36]
    conv_w_c = smallv[:, 36:68]
    conv_b_c = smallv[:, 68:76]

    w_in_v = c.w_in_d.rearrange("(k p) n -> p k n", p=128)
    for k in range(8):
        P.op("pool", lambda e, k=k: e.dma_start(out=WIN[:, k, 0:2048], in_=w_in_v[:, k, 0:2048]), writes=[f"WIN{k}a"], dma=f"d_win{k}a")
    P.op("pool", lambda e: e.dma_start(out=WRG, in_=c.w_rg_d.rearrange("a h i j -> i a h j")), writes=["WRG"], dma="d_wrg")
    P.op("pool", lambda e: e.dma_start(out=WRO, in_=c.w_rnn_out_d.rearrange("(k p) n -> p k n", p=128)), writes=["WRO"], dma="d_wro")
    P.op("pool", lambda e: e.dma_start(out=WGL, in_=c.w_glu_d.rearrange("(k p) n -> p k n", p=128)), writes=["WGL"], dma="d_wgl")
    for k in range(8):
        P.op("pool", lambda e, k=k: e.dma_start(out=WIN[:, k, 2048:4096], in_=w_in_v[:, k, 2560:4608]), writes=[f"WIN{k}b"], dma=f"d_win{k}b")
    P.op("sp", lambda e: e.dma_start(out=WR, in_=c.w_router_d.rearrange("(k p) n -> p k n", p=128)), writes=["WR"], dma="d_wr")
    P.op("sp", lambda e: e.dma_start(out=BRB, in_=c.b_router_d.partition_broadcast(128)), writes=["BRB"], dma="d_brb")
    P.op("sp", lambda e: e.dma_start(out=LN1G, in_=c.lnrows_d[0:1, :].partition_broadcast(128)), writes=["LN1G"], dma="d_ln1g")
    P.op("sp", lambda e: e.dma_start(out=LN1B, in_=c.lnrows_d[1:2, :].partition_broadcast(128)), writes=["LN1B"], dma="d_ln1b")
    P.op("sp", lambda e: e.dma_start(out=P1, in_=c.mod_d[0:1, 4 * D:5 * D].partition_broadcast(128)), reads=["mod_d"], writes=["P1"], dma="d_p1")
    P.op("sp", lambda e: e.dma_start(out=H2, in_=c.mod_d[0:1, 3 * D:4 * D].partition_broadcast(128)), reads=["mod_d"], writes=["H2"], dma="d_h2")
    P.op("sp", lambda e: e.dma_start(out=V, in_=c.mod_d[0:1, 2 * D:3 * D].partition_broadcast(128)), reads=["mod_d"], writes=["V"], dma="d_v")
    P.op("dve", lambda e: e.tensor_scalar(P1, P1, 1.0, None, op0=ALU.add), reads=["P1"], writes=["P1"])
    P.op("dve", lambda e: e.tensor_tensor(P2, LN1B, P1, op=ALU.mult), reads=["LN1B", "P1"], writes=["P2"])
    P.op("dve", lambda e: e.tensor_tensor(P2, P2, H2, op=ALU.add), reads=["P2", "H2"], writes=["P2"])
    P.op("dve", lambda e: e.tensor_tensor(P1, P1, LN1G, op=ALU.mult), reads=["P1", "LN1G"], writes=["P1"])
    P.op("dve", lambda e: e.tensor_scalar(V, V, 1.0, 0.5, op0=ALU.add, op1=ALU.mult), reads=["V"], writes=["V"])
    w_out_v = c.w_out_d.rearrange("(k p) n -> p k n", p=128)
    for k in range(8):
        sl = k % NX
        P.op("sp", lambda e, k=k, sl=sl: e.dma_start(out=XIN[sl], in_=w_out_v[:, k, :]), writes=[f"xin{sl}"], dma=f"d_xin{sl}")
        P.op("dve", lambda e, k=k, sl=sl: e.tensor_tensor(WOU[:, k, :], XIN[sl], V, op=ALU.mult), reads=[f"xin{sl}", "V"], writes=[f"WOU{k}"])
    P.op("dve", lambda e: e.tensor_scalar(hbias, b_in_c, 0.5, None, op0=ALU.mult), reads=["smallv"], writes=["hbias"])
    P.op("dve", lambda e: e.tensor_scalar(hbrg, smallv[:, 76:92], 0.5, None, op0=ALU.mult), reads=["smallv"], writes=["hbrg"])
    P.op("dve", lambda e: e.tensor_scalar(cfh, cf, 0.5, None, op0=ALU.mult), reads=["cf"], writes=["cfh"])
    P.op("dve", lambda e: e.memset(HALO, 0.0), writes=["halo"])
    P.op("dve", lambda e: e.memset(CARRY, 0.0), writes=["carry"])
    P.op("pool", lambda e: e.memset(MHALF, -0.5), writes=["mhalf"])

    hbn = [0]

    pools = {"A": [0, 1, 2, 3], "B4": [4, 5, 6, 7], "B2": [4, 5]}
    pcnt = {"A": 0, "B4": 0, "B2": 0}
    cur_pool = ["A"]

    def _nb():
        pl = cur_pool[0]
        i = pools[pl][pcnt[pl] % len(pools[pl])]
        pcnt[pl] += 1
        return i

    def hb():
        i = _nb()
        return psum_t[:, i, 0:256], f"bank{i}"

    def hb2():
        i = _nb()
        return psum_t[:, i, 0:256], psum_t[:, i, 256:512], f"bank{i}"

    def load_x(g):
        sl = g % NX
        P.op("sp", lambda e, g=g, sl=sl: e.dma_start(out=XIN[sl], in_=c.x_d[g * 128:(g + 1) * 128, :]), writes=[f"xin{sl}"], dma=f"d_xin{sl}")

    load_x(0)
    load_x(1)

    def sec_front(s):
        cur_pool[0] = "A"
        t0 = s * TT
        hT = hTs[s % 2]
        YST = YSTs[s % 2]
        hp_ = s % 2
        P.op("sp", lambda e, t0=t0, YST=YST: e.dma_start(out=YST, in_=c.ys_d[:, :, t0:t0 + TT].rearrange("q p t -> p q t")), writes=[f"yst{hp_}"], dma=f"d_yst{hp_}")
        for k in range(8):
            ps, key = hb()
            for tt in range(2):
                sl = (2 * s + tt) % NX
                P.op("pe", lambda e, ps=ps, tt=tt, sl=sl, k=k: e.transpose(ps[:, tt * 128:(tt + 1) * 128], XIN[sl][:, k * 128:(k + 1) * 128], ident),
                     reads=[f"xin{sl}", "cst"], writes=[key])
            P.op("act", lambda e, ps=ps, k=k, hT=hT: e.activation(out=hT[:, k, :], in_=ps, func=AF.Identity, scale=modcol[:, k:k + 1], bias=modcol[:, 8 + k:9 + k]),
                 reads=[key, "modcol", "modcol2"], writes=[f"hT{hp_}_{k}"])
        if 2 * s + 2 < NT:
            load_x(2 * s + 2)
        if 2 * s + 3 < NT:
            load_x(2 * s + 3)

    def sec_rg(s, hps):
        cur_pool[0] = "A"
        hT = hTs[s % 2]
        zA = zAs[s % 2]
        hp_ = s % 2
        for hp in hps:
            st = {}
            def front_body(j):
                h = 2 * hp + j
                T_ = TS[j]
                psx, kx = hb()
                for k in range(8):
                    P.op("pe", lambda e, psx=psx, k=k, h=h: e.matmul(psx, WIN[:, k, h * 128:(h + 1) * 128], hT[:, k, :], start=(k == 0), stop=(k == 7)),
                         reads=[f"WIN{k}a", f"hT{hp_}_{k}"], writes=[kx])
                P.op("pool", lambda e, T_=T_, h=h: e.tensor_copy(T_["xc"][:, 0:3], HALO[:, h, 0:3]), reads=["halo"], writes=[f"xc{j}"])
                P.op("act", lambda e, T_=T_, psx=psx, h=h: e.activation(out=T_["xc"][:, 3:3 + TT], in_=psx, func=AF.Identity, bias=b_in_c[:, h:h + 1]),
                     reads=[kx, "smallv"], writes=[f"xc{j}"])
                P.op("pool", lambda e, T_=T_, h=h: e.tensor_copy(HALO[:, h, 0:3], T_["xc"][:, TT:TT + 3]), reads=[f"xc{j}"], writes=["halo"])
                P.op("dve", lambda e, T_=T_, h=h: e.tensor_scalar(T_["xr"], T_["xc"][:, 0:TT], conv_w_c[:, 4 * h:4 * h + 1], conv_b_c[:, h:h + 1], op0=ALU.mult, op1=ALU.add),
                     reads=[f"xc{j}", "smallv"], writes=[f"xr{j}"])
                for q in range(1, 4):
                    P.op("dve", lambda e, T_=T_, h=h, q=q: e.scalar_tensor_tensor(T_["xr"], T_["xc"][:, q:q + TT], conv_w_c[:, 4 * h + q:4 * h + q + 1], T_["xr"], op0=ALU.mult, op1=ALU.add),
                         reads=[f"xc{j}", f"xr{j}"], writes=[f"xr{j}"])
                P.op("pool", lambda e, T_=T_: e.tensor_copy(T_["xrb"], T_["xr"]), reads=[f"xr{j}"], writes=[f"xrb{j}"])
                psr, psi, kr = hb2()
                ki = kr
                P.op("pe", lambda e, psr=psr, T_=T_, h=h: e.matmul(psr, WRG[:, 0, h, :], T_["xrb"], start=True, stop=True), reads=["WRG", f"xrb{j}"], writes=[kr])
                P.op("pe", lambda e, psi=psi, T_=T_, h=h: e.matmul(psi, WRG[:, 1, h, :], T_["xrb"], start=True, stop=True), reads=["WRG", f"xrb{j}"], writes=[ki])
                P.op("act", lambda e, T_=T_, psr=psr, h=h: e.activation(out=T_["thr"], in_=psr, func=AF.Tanh, scale=0.5, bias=hbrg[:, h:h + 1]),
                     reads=[kr, "hbrg"], writes=[f"thr{j}"])
                P.op("act", lambda e, T_=T_, psi=psi, h=h: e.activation(out=T_["thi"], in_=psi, func=AF.Tanh, scale=0.5, bias=hbrg[:, 8 + h:9 + h]),
                     reads=[ki, "hbrg"], writes=[f"thi{j}"])
                P.op("act", lambda e, T_=T_, h=h: e.activation(out=T_["thr"], in_=T_["thr"], func=AF.Exp, scale=cfh[:, h:h + 1], bias=cfh[:, h:h + 1]),
                     reads=[f"thr{j}", "cfh"], writes=[f"thr{j}"])
                P.op("pool", lambda e, T_=T_: e.tensor_tensor(T_["a2"], T_["thr"], T_["thr"], op=ALU.mult), reads=[f"thr{j}"], writes=[f"a2{j}"])
                P.op("dve", lambda e, T_=T_: e.scalar_tensor_tensor(T_["ix"], T_["thi"], 1.0, T_["xr"], op0=ALU.add, op1=ALU.mult),
                     reads=[f"thi{j}", f"xr{j}"], writes=[f"ix{j}"])
            interleave(P, [lambda j=j: front_body(j) for j in range(2)])
            for j in range(2):
                T_ = TS[j]
                P.op("act", lambda e, T_=T_: e.activation(out=T_["a2"], in_=T_["a2"], func=AF.Sqrt, scale=-1.0, bias=1.0), reads=[f"a2{j}"], writes=[f"a2{j}"])
            def back_body(j):
                h = 2 * hp + j
                T_ = TS[j]
                psy, ky = hb()
                for k in range(8):
                    P.op("pe", lambda e, psy=psy, k=k, h=h: e.matmul(psy, WIN[:, k, 1024 + h * 128:1024 + (h + 1) * 128], hT[:, k, :], start=(k == 0), stop=(k == 7)),
                         reads=[f"WIN{k}a", f"hT{hp_}_{k}"], writes=[ky])
                P.op("act", lambda e, T_=T_, psy=psy, h=h: e.activation(out=T_["gy"], in_=psy, func=AF.Gelu_apprx_tanh, bias=b_in_c[:, 8 + h:9 + h]),
                     reads=[ky, "smallv"], writes=[f"xc{j}"])
                P.op("dve", lambda e, T_=T_: e.scalar_tensor_tensor(T_["ix"], T_["a2"], 0.5, T_["ix"], op0=ALU.mult, op1=ALU.mult),
                     reads=[f"a2{j}", f"ix{j}"], writes=[f"ix{j}"])
                P.op("dve", lambda e, T_=T_, h=h: e.tensor_tensor_scan(T_["hs"], T_["thr"], T_["ix"], CARRY[:, h:h + 1], op0=ALU.mult, op1=ALU.add),
                     reads=[f"thr{j}", f"ix{j}", "carry"], writes=[f"thi{j}"])
                P.op("dve", lambda e, T_=T_, h=h: e.tensor_copy(CARRY[:, h:h + 1], T_["hs"][:, TT - 1:TT]), reads=[f"thi{j}"], writes=["carry"])
                P.op("pool", lambda e, T_=T_, h=h, zA=zA: e.tensor_tensor(zA[:, h, :], T_["gy"], T_["hs"], op=ALU.mult), reads=[f"xc{j}", f"thi{j}"], writes=[f"zA{hp_}_{h}"])
            interleave(P, [lambda j=j: back_body(j) for j in range(2)])

    def sec_merge(s, ccs):
        cur_pool[0] = "B4"
        hT = hTs[s % 2]
        zA = zAs[s % 2]
        YST = YSTs[s % 2]
        hp_ = s % 2
        def merge_body(cc):
            psA, kA = hb()
            for h in range(8):
                P.op("pe", lambda e, psA=psA, h=h, cc=cc, zA=zA: e.matmul(psA, WRO[:, h, cc * 128:(cc + 1) * 128], zA[:, h, :], start=(h == 0), stop=(h == 7)),
                     reads=["WRO", f"zA{hp_}_{h}"], writes=[kA])
            psB1, psB2, kB1 = hb2()
            kB2 = kB1
            for q in range(4):
                P.op("pe", lambda e, psB1=psB1, q=q, cc=cc, YST=YST: e.matmul(psB1, WGL[:, q, cc * 128:(cc + 1) * 128], YST[:, q, :], start=(q == 0), stop=(q == 3)),
                     reads=["WGL", f"yst{hp_}"], writes=[kB1])
            for q in range(4):
                P.op("pe", lambda e, psB2=psB2, q=q, cc=cc, YST=YST: e.matmul(psB2, WGL[:, q, D + cc * 128:D + (cc + 1) * 128], YST[:, q, :], start=(q == 0), stop=(q == 3)),
                     reads=["WGL", f"yst{hp_}"], writes=[kB2])
            psG0, psG1, kG0 = hb2()
            kG1 = kG0
            for k in range(8):
                P.op("pe", lambda e, psG0=psG0, k=k, cc=cc: e.matmul(psG0, WIN[:, k, 2048 + cc * 128:2048 + (cc + 1) * 128], hT[:, k, :], start=(k == 0), stop=(k == 7)),
                     reads=[f"WIN{k}b", f"hT{hp_}_{k}"], writes=[kG0])
            for k in range(8):
                P.op("pe", lambda e, psG1=psG1, k=k, cc=cc: e.matmul(psG1, WIN[:, k, 3072 + cc * 128:3072 + (cc + 1) * 128], hT[:, k, :], start=(k == 0), stop=(k == 7)),
                     reads=[f"WIN{k}b", f"hT{hp_}_{k}"], writes=[kG1])
            M_ = MTS[cc % 2]
            mk = cc % 2
            P.op("act", lambda e, psG0=psG0, cc=cc, M_=M_: e.activation(out=M_["t0"], in_=psG0, func=AF.Tanh, scale=0.5, bias=hbias[:, 20 + cc:21 + cc]), reads=[kG0, "hbias"], writes=[f"m_t0{mk}"])
            P.op("act", lambda e, psG1=psG1, cc=cc, M_=M_: e.activation(out=M_["t1"], in_=psG1, func=AF.Tanh, scale=0.5, bias=hbias[:, 28 + cc:29 + cc]), reads=[kG1, "hbias"], writes=[f"m_t1{mk}"])
            P.op("act", lambda e, psB2=psB2, M_=M_: e.activation(out=M_["tb"], in_=psB2, func=AF.Tanh, scale=0.5), reads=[kB2], writes=[f"m_tb{mk}"])
            P.op("dve", lambda e, psA=psA, M_=M_: e.scalar_tensor_tensor(M_["ta"], M_["t0"], 1.0, psA, op0=ALU.add, op1=ALU.mult), reads=[f"m_t0{mk}", kA], writes=[f"m_t0{mk}"])
            P.op("dve", lambda e, psB1=psB1, M_=M_: e.scalar_tensor_tensor(M_["tbb"], M_["tb"], 1.0, psB1, op0=ALU.add, op1=ALU.mult), reads=[f"m_tb{mk}", kB1], writes=[f"m_tb{mk}"])
            P.op("dve", lambda e, M_=M_: e.scalar_tensor_tensor(M_["tbb"], M_["t1"], 1.0, M_["tbb"], op0=ALU.add, op1=ALU.mult), reads=[f"m_t1{mk}", f"m_tb{mk}"], writes=[f"m_tb{mk}"])
            P.op("dve", lambda e, cc=cc, M_=M_: e.scalar_tensor_tensor(mT[:, cc, :], M_["tbb"], 0.5, M_["ta"], op0=ALU.mult, op1=ALU.add), reads=[f"m_tb{mk}", f"m_t0{mk}"], writes=[f"mT{cc}"])
        for cc in ccs:
            merge_body(cc)

    def sec_out(s, tts):
        cur_pool[0] = "B2"
        for tt in tts:
            g = 2 * s + tt
            sl = 0
            P.op("sp", lambda e, g=g: e.dma_start(out=XRES, in_=c.x_d[g * 128:(g + 1) * 128, :]), writes=["xres"], dma="d_xres")
            for hlf in range(2):
                for cc in range(8):
                    P.op("pe", lambda e, hlf=hlf, cc=cc, tt=tt: e.matmul(psum_t[:, 6 + hlf, :], mT[:, cc, tt * 128:(tt + 1) * 128], WOU[:, cc, hlf * 512:(hlf + 1) * 512],
                                                                       start=(cc == 0), stop=(cc == 7)),
                         reads=[f"mT{cc}", f"WOU{cc}"], writes=[f"bank{6 + hlf}"])
                P.op("dve", lambda e, hlf=hlf, sl=sl: e.scalar_tensor_tensor(V[:, hlf * 512:(hlf + 1) * 512], XRES[:, hlf * 512:(hlf + 1) * 512], ALPHA,
                                                                            psum_t[:, 6 + hlf, :], op0=ALU.mult, op1=ALU.add),
                     reads=["xres", f"bank{6 + hlf}"], writes=["V"])
                P.op("dve", lambda e, hlf=hlf: e.bn_stats(STATS[:, hlf, :], V[:, hlf * 512:(hlf + 1) * 512]), reads=["V"], writes=["stats"])
            P.op("dve", lambda e: e.bn_aggr(MV, STATS.rearrange("p a b -> p (a b)")), reads=["stats"], writes=["mv"])
            P.op("pool", lambda e: e.tensor_scalar(RSTD, MV[:, 1:2], EPS, None, op0=ALU.add), reads=["mv"], writes=["rstd"])
            P.op("pool", lambda e: e.tensor_tensor(RSTD, RSTD, MHALF, op=ALU.pow), reads=["rstd", "mhalf"], writes=["rstd"])
            P.op("dve", lambda e: e.tensor_scalar(V, V, MV[:, 0:1], RSTD, op0=ALU.subtract, op1=ALU.mult), reads=["V", "mv", "rstd"], writes=["V"])
            P.op("pool", lambda e, sl=sl: e.tensor_tensor(XRES, V, LN1G, op=ALU.mult), reads=["V", "LN1G"], writes=["xres"])
            P.op("pool", lambda e, sl=sl: e.tensor_tensor(XRES, XRES, LN1B, op=ALU.add), reads=["xres", "LN1B"], writes=["xres"])
            P.op("dve", lambda e: e.tensor_tensor(H2, V, P1, op=ALU.mult), reads=["V", "P1"], writes=["H2"])
            P.op("dve", lambda e: e.tensor_tensor(H2, H2, P2, op=ALU.add), reads=["H2", "P2"], writes=["H2"])
            P.op("sp", lambda e, g=g, sl=sl: e.dma_start(out=c.x1_d[g * 128:(g + 1) * 128, :], in_=XRES), reads=["xres"], writes=["x1_d"], dma="d_x1st")
            P.op("pool", lambda e, g=g: e.dma_start(out=c.h2_d[g * 128:(g + 1) * 128, :], in_=H2), reads=["H2"], writes=["h2_d"], dma="d_h2st")
            for kp in range(4):
                ps, key = hb()
                for j in range(2):
                    k = 2 * kp + j
                    P.op("pe", lambda e, ps=ps, j=j, k=k: e.transpose(ps[:, j * 128:(j + 1) * 128], H2[:, k * 128:(k + 1) * 128], ident), reads=["H2", "cst"], writes=[key])
                P.op("act", lambda e, ps=ps, kp=kp: e.activation(out=H2T[:, 2 * kp:2 * kp + 2, :], in_=ps.rearrange("p (a b) -> p a b", a=2), func=AF.Identity),
                     reads=[key], writes=["V"])
            psl, kl = hb()
            for k in range(8):
                P.op("pe", lambda e, psl=psl, k=k: e.matmul(psl[:, 0:NE], H2T[:, k, :], WR[:, k, :], start=(k == 0), stop=(k == 7)), reads=["V", "WR"], writes=[kl])
            P.op("dve", lambda e, psl=psl, g=g: e.tensor_tensor(L[:, g, :], psl[:, 0:NE], BRB, op=ALU.add), reads=[kl, "BRB"], writes=["L"])
    nst_ = c.dbg.get('nst', NST)
    sec_front(0)
    for s in range(nst_ + 1):
        for hp in range(4):
            bl_ = []
            if s < nst_:
                bl_.append(lambda s=s, hp=hp: sec_rg(s, [hp]))
            if s >= 1:
                bl_.append(lambda s=s, hp=hp: sec_merge(s - 1, [2 * hp, 2 * hp + 1]))
            interleave(P, bl_)
        bl_ = []
        if s >= 1:
            bl_.append(lambda s=s: sec_out(s - 1, [0, 1]))
        if s + 1 < nst_:
            bl_.append(lambda s=s: sec_front(s + 1))
        if bl_:
            interleave(P, bl_)
    P.op("sp", lambda e: e.dma_start(out=c.lg_d, in_=L), reads=["L"], writes=["lg_d"], dma="d_lgst")


def mixer_pass2(c):
    P, A, psum_t = c.P, c.A, c.psum_t
    ident, smallv, modcol, cf, L = c.ident, c.smallv, c.modcol, c.cf, c.L
    TT = 128
    NST = c.dbg.get("nst", S // TT)
    WIN = A.alloc([8, 4096], BF16)
    WRO = A.alloc([8, D], BF16)
    WGL = A.alloc([4, 2 * D], BF16)
    WOU = A.alloc([8, D], BF16)
    WRG = A.alloc([2, 8, 128], BF16)
    WR = A.alloc([8, NE], F32)
    LN1G = A.alloc([D], F32)
    LN1B = A.alloc([D], F32)
    P1 = A.alloc([D], F32)
    P2 = A.alloc([D], F32)
    BRB = A.alloc([NE], F32)
    XIN = [A.alloc([D], F32) for _ in range(3)]
    hTs = [A.alloc([8, TT], BF16) for _ in range(2)]
    YSTs = [A.alloc([4, TT], BF16)]
    zA = A.alloc([8, TT], BF16)
    mT = A.alloc([8, TT], BF16)
    XC = A.alloc([8, TT + 8], F32)
    XR = A.alloc([8, TT], F32)
    XRB = A.alloc([8, TT], BF16)
    THR = A.alloc([8, TT], F32)
    THI = A.alloc([8, TT], F32)
    A2 = A.alloc([8, TT], F32)
    IX = A.alloc([8, TT], F32)
    GY = XC[:, :, 0:TT]
    HS = THI
    MTS = [dict(t0=A.alloc([4, TT], F32), t1=A.alloc([4, TT], F32), tb=A.alloc([4, TT], F32)) for _ in range(1)]
    MTS[0]['ta'] = MTS[0]['t0']
    MTS[0]['tbb'] = MTS[0]['tb']
    V = A.alloc([D], F32)
    H2 = A.alloc([D], F32)
    H2T = V.rearrange("p (k t) -> p k t", k=8)
    HALO = A.alloc([8, 4], F32)
    CARRY = A.alloc([8], F32)
    hbias = A.alloc([36], F32)
    hbrg = A.alloc([16], F32)
    cfh = A.alloc([8], F32)
    cf1 = A.alloc([8], F32)
    STATS = A.alloc([2, 6], F32)
    MV = A.alloc([2], F32)
    RSTD = A.alloc([1], F32)
    MHALF = A.alloc([1], F32)
    b_in_c = smallv[:, 0:36]
    conv_w_c = smallv[:, 36:68]
    conv_b_c = smallv[:, 68:76]
    MUL, ADD = ALU.mult, ALU.add

    w_in_v = c.w_in_d.rearrange("(k p) n -> p k n", p=128)
    for k in range(8):
        P.op("pool", lambda e, k=k: e.dma_start(out=WIN[:, k, 0:2048], in_=w_in_v[:, k, 0:2048]), writes=[f"WIN{k}a"], dma=f"d_win{k}a")
    P.op("pool", lambda e: e.dma_start(out=WRG, in_=c.w_rg_d.rearrange("a h i j -> i a h j")), writes=["WRG"], dma="d_wrg")
    P.op("pool", lambda e: e.dma_start(out=WRO, in_=c.w_rnn_out_d.rearrange("(k p) n -> p k n", p=128)), writes=["WRO"], dma="d_wro")
    P.op("pool", lambda e: e.dma_start(out=WGL, in_=c.w_glu_d.rearrange("(k p) n -> p k n", p=128)), writes=["WGL"], dma="d_wgl")
    for k in range(8):
        P.op("pool", lambda e, k=k: e.dma_start(out=WIN[:, k, 2048:4096], in_=w_in_v[:, k, 2560:4608]), writes=[f"WIN{k}b"], dma=f"d_win{k}b")
    P.op("sp", lambda e: e.dma_start(out=WR, in_=c.w_router_d.rearrange("(k p) n -> p k n", p=128)), writes=["WR"], dma="d_wr")
    P.op("sp", lambda e: e.dma_start(out=BRB, in_=c.b_router_d.partition_broadcast(128)), writes=["BRB"], dma="d_brb")
    P.op("sp", lambda e: e.dma_start(out=LN1G, in_=c.lnrows_d[0:1, :].partition_broadcast(128)), writes=["LN1G"], dma="d_ln1g")
    P.op("sp", lambda e: e.dma_start(out=LN1B, in_=c.lnrows_d[1:2, :].partition_broadcast(128)), writes=["LN1B"], dma="d_ln1b")
    P.op("sp", lambda e: e.dma_start(out=P1, in_=c.mod_d[0:1, 4 * D:5 * D].partition_broadcast(128)), reads=["mod_d"], writes=["P1"], dma="d_p1")
    P.op("sp", lambda e: e.dma_start(out=H2, in_=c.mod_d[0:1, 3 * D:4 * D].partition_broadcast(128)), reads=["mod_d"], writes=["H2"], dma="d_h2")
    P.op("sp", lambda e: e.dma_start(out=V, in_=c.mod_d[0:1, 2 * D:3 * D].partition_broadcast(128)), reads=["mod_d"], writes=["V"], dma="d_v")
    P.op("dve", lambda e: e.tensor_scalar(P1, P1, 1.0, None, op0=ADD), reads=["P1"], writes=["P1"])
    P.op("dve", lambda e: e.tensor_tensor(P2, LN1B, P1, op=MUL), reads=["LN1B", "P1"], writes=["P2"])
    P.op("dve", lambda e: e.tensor_tensor(P2, P2, H2, op=ADD), reads=["P2", "H2"], writes=["P2"])
    P.op("dve", lambda e: e.tensor_tensor(P1, P1, LN1G, op=MUL), reads=["P1", "LN1G"], writes=["P1"])
    P.op("dve", lambda e: e.tensor_scalar(V, V, 1.0, 0.5, op0=ADD, op1=MUL), reads=["V"], writes=["V"])
    w_out_v = c.w_out_d.rearrange("(k p) n -> p k n", p=128)
    for k in range(8):
        sl = k % 3
        P.op("sp", lambda e, k=k, sl=sl: e.dma_start(out=XIN[sl], in_=w_out_v[:, k, :]), writes=[f"xin{sl}"], dma=f"d_xin{sl}")
        P.op("dve", lambda e, k=k, sl=sl: e.tensor_tensor(WOU[:, k, :], XIN[sl], V, op=MUL), reads=[f"xin{sl}", "V"], writes=[f"WOU{k}"])
    P.op("dve", lambda e: e.tensor_scalar(hbias, b_in_c, 0.5, None, op0=MUL), reads=["smallv"], writes=["hbias"])
    P.op("dve", lambda e: e.tensor_scalar(hbrg, smallv[:, 76:92], 0.5, None, op0=MUL), reads=["smallv"], writes=["hbrg"])
    P.op("dve", lambda e: e.tensor_scalar(cfh, cf, 0.5, None, op0=MUL), reads=["cf"], writes=["cfh"])
    P.op("dve", lambda e: e.tensor_copy(cf1, cf), reads=["cf"], writes=["cf1"])
    P.op("dve", lambda e: e.memset(HALO, 0.0), writes=["halo"])
    P.op("dve", lambda e: e.memset(CARRY, 0.0), writes=["carry"])
    P.op("pool", lambda e: e.memset(MHALF, -0.5), writes=["mhalf"])

    bn = [0]

    def nb():
        i = bn[0] % 8
        bn[0] += 1
        return psum_t[:, i, :], f"bank{i}"

    def load_x(g):
        sl = g % 3
        P.op("sp", lambda e, g=g, sl=sl: e.dma_start(out=XIN[sl], in_=c.x_d[g * 128:(g + 1) * 128, :]), writes=[f"xin{sl}"], dma=f"d_xin{sl}")

    def load_ys(g):
        P.op("sp", lambda e, g=g: e.dma_start(out=YSTs[0], in_=c.ys_d[:, :, g * TT:(g + 1) * TT].rearrange("q p t -> p q t")), writes=["yst0"], dma="d_yst0")

    load_x(0)
    load_x(1)
    load_ys(0)
    for s in range(NST):
        g = s
        sl = g % 3
        hT = hTs[s % 2]
        YST = YSTs[0]
        hp_ = s % 2
        if s + 2 < NT:
            load_x(s + 2)
        for kh in range(2):
            ps, key = nb()
            for kk in range(4):
                k = kh * 4 + kk
                P.op("pe", lambda e, ps=ps, kk=kk, k=k, sl=sl: e.transpose(ps[:, kk * 128:(kk + 1) * 128], XIN[sl][:, k * 128:(k + 1) * 128], ident), reads=[f"xin{sl}", "cst"], writes=[key])
            for kk in range(4):
                k = kh * 4 + kk
                P.op("act", lambda e, ps=ps, kk=kk, k=k, hT=hT: e.activation(out=hT[:, k, :], in_=ps[:, kk * 128:(kk + 1) * 128], func=AF.Identity, scale=modcol[:, k:k + 1], bias=modcol[:, 8 + k:9 + k]),
                     reads=[key, "modcol", "modcol2"], writes=[f"hT{hp_}"])
        hk = f"hT{hp_}"
        P.op("pool", lambda e: e.tensor_copy(XC[:, :, 0:3], HALO[:, :, 0:3]), reads=["halo"], writes=[f"XC{h_}" for h_ in range(8)])
        for hh in range(2):
            ps, key = nb()
            for hq in range(4):
                h = hh * 4 + hq
                for k in range(8):
                    P.op("pe", lambda e, ps=ps, hq=hq, h=h, k=k, hT=hT: e.matmul(ps[:, hq * 128:(hq + 1) * 128], WIN[:, k, h * 128:(h + 1) * 128], hT[:, k, :], start=(k == 0), stop=(k == 7)),
                         reads=[f"WIN{k}a", hk], writes=[key])
            for hq in range(4):
                h = hh * 4 + hq
                P.op("act", lambda e, ps=ps, hq=hq, h=h: e.activation(out=XC[:, h, 3:3 + TT], in_=ps[:, hq * 128:(hq + 1) * 128], func=AF.Identity, bias=b_in_c[:, h:h + 1]),
                     reads=[key, "smallv"], writes=[f"XC{h}"])
        P.op("pool", lambda e: e.tensor_copy(HALO[:, :, 0:3], XC[:, :, TT:TT + 3]), reads=[f"XC{h_}" for h_ in range(8)], writes=["halo"])
        for h in range(8):
            P.op("dve", lambda e, h=h: e.tensor_scalar(XR[:, h, :], XC[:, h, 0:TT], conv_w_c[:, 4 * h:4 * h + 1], conv_b_c[:, h:h + 1], op0=MUL, op1=ADD), reads=[f"XC{h}", "smallv"], writes=[f"XR{h}"])
        for q in range(1, 4):
            for h in range(8):
                P.op("dve", lambda e, h=h, q=q: e.scalar_tensor_tensor(XR[:, h, :], XC[:, h, q:q + TT], conv_w_c[:, 4 * h + q:4 * h + q + 1], XR[:, h, :], op0=MUL, op1=ADD),
                     reads=[f"XC{h}", f"XR{h}"], writes=[f"XR{h}"])
        for hh in range(2):
            P.op("dve", lambda e, hh=hh: e.tensor_copy(XRB[:, hh * 4:(hh + 1) * 4, :], XR[:, hh * 4:(hh + 1) * 4, :]), reads=[f"XR{h_}" for h_ in range(hh * 4, hh * 4 + 4)], writes=[f"XRB{hh}"])
        gbanks = []
        for a_ in range(2):
            for hh in range(2):
                ps, key = nb()
                gbanks.append((ps, key))
                for hq in range(4):
                    h = hh * 4 + hq
                    P.op("pe", lambda e, ps=ps, hq=hq, h=h, a_=a_: e.matmul(ps[:, hq * 128:(hq + 1) * 128], WRG[:, a_, h, :], XRB[:, h, :], start=True, stop=True), reads=["WRG", f"XRB{hh}"], writes=[key])
        for a_ in range(2):
            for hh in range(2):
                ps, key = gbanks[a_ * 2 + hh]
                for hq in range(4):
                    h = hh * 4 + hq
                    dst = THR if a_ == 0 else THI
                    P.op("act", lambda e, ps=ps, hq=hq, h=h, a_=a_, dst=dst: e.activation(out=dst[:, h, :], in_=ps[:, hq * 128:(hq + 1) * 128], func=AF.Tanh, scale=0.5, bias=hbrg[:, a_ * 8 + h:a_ * 8 + h + 1]),
                         reads=[key, "hbrg"], writes=[(f"THR{h}" if a_ == 0 else f"THI{h}")])
        for h in range(8):
            P.op("act", lambda e, h=h: e.activation(out=A2[:, h, :], in_=THR[:, h, :], func=AF.Exp, scale=cf1[:, h:h + 1], bias=cf1[:, h:h + 1]), reads=[f"THR{h}", "cf1"], writes=[f"A2{h}"])
        for h in range(8):
            P.op("act", lambda e, h=h: e.activation(out=THR[:, h, :], in_=THR[:, h, :], func=AF.Exp, scale=cfh[:, h:h + 1], bias=cfh[:, h:h + 1]), reads=[f"THR{h}", "cfh"], writes=[f"THR{h}"])
        def hv(t, hh):
            return t[:, hh * 4:(hh + 1) * 4, :].rearrange("p a b -> p (a b)")
        for hh in range(2):
            P.op("dve", lambda e, hh=hh: e.scalar_tensor_tensor(hv(IX, hh), hv(THI, hh), 1.0, hv(XR, hh), op0=ADD, op1=MUL),
                 reads=[f"THI{h_}" for h_ in range(hh * 4, hh * 4 + 4)] + [f"XR{h_}" for h_ in range(hh * 4, hh * 4 + 4)], writes=[f"IX{hh}"])
        for hh in range(2):
            P.op("act", lambda e, hh=hh: e.activation(out=hv(A2, hh), in_=hv(A2, hh), func=AF.Sqrt, scale=-1.0, bias=1.0), reads=[f"A2{h_}" for h_ in range(hh * 4, hh * 4 + 4)], writes=[f"A2s{hh}"])
        for hh in range(2):
            ps, key = nb()
            for hq in range(4):
                h = hh * 4 + hq
                for k in range(8):
                    P.op("pe", lambda e, ps=ps, hq=hq, h=h, k=k, hT=hT: e.matmul(ps[:, hq * 128:(hq + 1) * 128], WIN[:, k, 1024 + h * 128:1024 + (h + 1) * 128], hT[:, k, :], start=(k == 0), stop=(k == 7)),
                         reads=[f"WIN{k}a", hk], writes=[key])
            for hq in range(4):
                h = hh * 4 + hq
                P.op("act", lambda e, ps=ps, hq=hq, h=h: e.activation(out=GY[:, h, :], in_=ps[:, hq * 128:(hq + 1) * 128], func=AF.Gelu_apprx_tanh, bias=b_in_c[:, 8 + h:9 + h]),
                     reads=[key, "smallv"], writes=[f"XC{h}"])
        for hh in range(2):
            P.op("dve", lambda e, hh=hh: e.scalar_tensor_tensor(hv(IX, hh), hv(A2, hh), 0.5, hv(IX, hh), op0=MUL, op1=MUL), reads=[f"A2s{hh}", f"IX{hh}"], writes=[f"IX{hh}"])
        for h in range(8):
            P.op("dve", lambda e, h=h: e.tensor_tensor_scan(HS[:, h, :], THR[:, h, :], IX[:, h, :], CARRY[:, h:h + 1], op0=MUL, op1=ADD), reads=[f"THR{h}", f"IX{h // 4}", "carry", f"THI{h}"], writes=[f"THI{h}"])
        P.op("dve", lambda e: e.tensor_copy(CARRY, HS[:, :, TT - 1]), reads=[f"THI{h_}" for h_ in range(8)], writes=["carry"])
        for hh in range(2):
            P.op("dve", lambda e, hh=hh: e.tensor_tensor(hv(zA, hh), GY[:, hh * 4:(hh + 1) * 4, :], HS[:, hh * 4:(hh + 1) * 4, :], op=MUL),
                 reads=[f"XC{h_}" for h_ in range(hh * 4, hh * 4 + 4)] + [f"THI{h_}" for h_ in range(hh * 4, hh * 4 + 4)], writes=[f"zA{hh}"])
        for ch in range(2):
            M_ = MTS[0]
            psA, kA = nb()
            for cq in range(4):
                cc = ch * 4 + cq
                for h in range(8):
                    P.op("pe", lambda e, psA=psA, cq=cq, cc=cc, h=h: e.matmul(psA[:, cq * 128:(cq + 1) * 128], WRO[:, h, cc * 128:(cc + 1) * 128], zA[:, h, :], start=(h == 0), stop=(h == 7)),
                         reads=["WRO", f"zA{h // 4}"], writes=[kA])
            psB1, kB1 = nb()
            for cq in range(4):
                cc = ch * 4 + cq
                for q in range(4):
                    P.op("pe", lambda e, psB1=psB1, cq=cq, cc=cc, q=q, YST=YST: e.matmul(psB1[:, cq * 128:(cq + 1) * 128], WGL[:, q, cc * 128:(cc + 1) * 128], YST[:, q, :], start=(q == 0), stop=(q == 3)),
                         reads=["WGL", "yst0"], writes=[kB1])
            psB2, kB2 = nb()
            for cq in range(4):
                cc = ch * 4 + cq
                for q in range(4):
                    P.op("pe", lambda e, psB2=psB2, cq=cq, cc=cc, q=q, YST=YST: e.matmul(psB2[:, cq * 128:(cq + 1) * 128], WGL[:, q, D + cc * 128:D + (cc + 1) * 128], YST[:, q, :], start=(q == 0), stop=(q == 3)),
                         reads=["WGL", "yst0"], writes=[kB2])
            psG = []
            for gi_ in range(2):
                psg, kg = nb()
                psG.append((psg, kg))
                for cq in range(4):
                    cc = ch * 4 + cq
                    for k in range(8):
                        P.op("pe", lambda e, psg=psg, cq=cq, cc=cc, k=k, gi_=gi_, hT=hT: e.matmul(psg[:, cq * 128:(cq + 1) * 128], WIN[:, k, 2048 + gi_ * 1024 + cc * 128:2048 + gi_ * 1024 + (cc + 1) * 128], hT[:, k, :],
                                                                                                 start=(k == 0), stop=(k == 7)),
                             reads=[f"WIN{k}b", hk], writes=[kg])
            for gi_ in range(2):
                psg, kg = psG[gi_]
                dst = M_["t0"] if gi_ == 0 else M_["t1"]
                for cq in range(4):
                    cc = ch * 4 + cq
                    P.op("act", lambda e, psg=psg, cq=cq, cc=cc, gi_=gi_, dst=dst: e.activation(out=dst[:, cq, :], in_=psg[:, cq * 128:(cq + 1) * 128], func=AF.Tanh, scale=0.5, bias=hbias[:, 20 + gi_ * 8 + cc:21 + gi_ * 8 + cc]),
                         reads=[kg, "hbias"], writes=[f"m_t{gi_}"])
            f2 = lambda t: t.rearrange("p a b -> p (a b)")
            P.op("act", lambda e, psB2=psB2, M_=M_: e.activation(out=f2(M_["tb"]), in_=psB2, func=AF.Tanh, scale=0.5), reads=[kB2], writes=[f"m_tb"])
            P.op("dve", lambda e, psA=psA, M_=M_: e.scalar_tensor_tensor(f2(M_["ta"]), f2(M_["t0"]), 1.0, psA, op0=ADD, op1=MUL), reads=[f"m_t0", kA], writes=[f"m_t0"])
            P.op("dve", lambda e, psB1=psB1, M_=M_: e.scalar_tensor_tensor(f2(M_["tbb"]), f2(M_["tb"]), 1.0, psB1, op0=ADD, op1=MUL), reads=[f"m_tb", kB1], writes=[f"m_tb"])
            P.op("dve", lambda e, M_=M_: e.scalar_tensor_tensor(f2(M_["tbb"]), f2(M_["t1"]), 1.0, f2(M_["tbb"]), op0=ADD, op1=MUL), reads=[f"m_t1", f"m_tb"], writes=[f"m_tb"])
            P.op("dve", lambda e, M_=M_, ch=ch: e.scalar_tensor_tensor(f2(mT[:, ch * 4:(ch + 1) * 4, :]), f2(M_["tbb"]), 0.5, f2(M_["ta"]), op0=MUL, op1=ADD), reads=[f"m_tb", f"m_t0"], writes=[f"mT{ch}"])
        if s + 1 < NT:
            load_ys(s + 1)
        obk = []
        for hlf in range(2):
            ps, key = nb()
            obk.append((ps, key))
            for cc in range(8):
                P.op("pe", lambda e, ps=ps, hlf=hlf, cc=cc: e.matmul(ps, mT[:, cc, :], WOU[:, cc, hlf * 512:(hlf + 1) * 512], start=(cc == 0), stop=(cc == 7)),
                     reads=[f"mT{cc // 4}", f"WOU{cc}"], writes=[key])
        for hlf in range(2):
            ps, key = obk[hlf]
            P.op("dve", lambda e, ps=ps, hlf=hlf, sl=sl: e.scalar_tensor_tensor(V[:, hlf * 512:(hlf + 1) * 512], XIN[sl][:, hlf * 512:(hlf + 1) * 512], ALPHA, ps, op0=MUL, op1=ADD),
                 reads=[f"xin{sl}", key], writes=["V"])
            P.op("dve", lambda e, hlf=hlf: e.bn_stats(STATS[:, hlf, :], V[:, hlf * 512:(hlf + 1) * 512]), reads=["V"], writes=["stats"])
        P.op("dve", lambda e: e.bn_aggr(MV, STATS.rearrange("p a b -> p (a b)")), reads=["stats"], writes=["mv"])
        P.op("pool", lambda e: e.tensor_scalar(RSTD, MV[:, 1:2], EPS, None, op0=ADD), reads=["mv"], writes=["rstd"])
        P.op("pool", lambda e: e.tensor_tensor(RSTD, RSTD, MHALF, op=ALU.pow), reads=["rstd", "mhalf"], writes=["rstd"])
        P.op("dve", lambda e: e.tensor_scalar(V, V, MV[:, 0:1], RSTD, op0=ALU.subtract, op1=MUL), reads=["V", "mv", "rstd"], writes=["V"])
        P.op("pool", lambda e, sl=sl: e.tensor_tensor(XIN[sl], V, LN1G, op=MUL), reads=["V", "LN1G"], writes=[f"xin{sl}"])
        P.op("pool", lambda e, sl=sl: e.tensor_tensor(XIN[sl], XIN[sl], LN1B, op=ADD), reads=[f"xin{sl}", "LN1B"], writes=[f"xin{sl}"])
        P.op("dve", lambda e: e.tensor_tensor(H2, V, P1, op=MUL), reads=["V", "P1"], writes=["H2"])
        P.op("dve", lambda e: e.tensor_tensor(H2, H2, P2, op=ADD), reads=["H2", "P2"], writes=["H2"])
        P.op("sp", lambda e, g=g, sl=sl: e.dma_start(out=c.x1_d[g * 128:(g + 1) * 128, :], in_=XIN[sl]), reads=[f"xin{sl}"], writes=["x1_d"], dma=f"d_x1st{sl}")
        P.op("pool", lambda e, g=g: e.dma_start(out=c.h2_d[g * 128:(g + 1) * 128, :], in_=H2), reads=["H2"], writes=["h2_d"], dma="d_h2st")
        for kh in range(2):
            ps, key = nb()
            for kk in range(4):
                k = kh * 4 + kk
                P.op("pe", lambda e, ps=ps, kk=kk, k=k: e.transpose(ps[:, kk * 128:(kk + 1) * 128], H2[:, k * 128:(k + 1) * 128], ident), reads=["H2", "cst"], writes=[key])
            P.op("act", lambda e, ps=ps, kh=kh: e.activation(out=H2T[:, kh * 4:(kh + 1) * 4, :], in_=ps.rearrange("p (a b) -> p a b", a=4), func=AF.Identity), reads=[key], writes=["V"])
        psl, kl = nb()
        for k in range(8):
            P.op("pe", lambda e, psl=psl, k=k: e.matmul(psl[:, 0:NE], H2T[:, k, :], WR[:, k, :], start=(k == 0), stop=(k == 7)), reads=["V", "WR"], writes=[kl])
        P.op("dve", lambda e, psl=psl, g=g: e.tensor_tensor(L[:, g, :], psl[:, 0:NE], BRB, op=ADD), reads=[kl, "BRB"], writes=["L"])
    P.op("sp", lambda e: e.dma_start(out=c.lg_d, in_=L), reads=["L"], writes=["lg_d"], dma="d_lgst")


def s5_prepare(c):
    import math
    P, A, psum_t, cst, ident = c.P, c.A, c.psum_t, c.cst, c.ident
    NJ = 64
    half = cst[:, 512:514]
    mask16 = cst[:, 256:384]
    K_ = types.SimpleNamespace()
    K_.WINU = A.alloc([8, 512], BF16)
    K_.WinT = A.alloc([2, 4, 1024], BF16)
    K_.KTA = A.alloc([4, 1024], BF16)
    K_.WinT3 = A.alloc([2, 4, 1024], BF16)
    K_.COUT = A.alloc([2, 16, 320], BF16)
    K_.COS = A.alloc([16, NJ], F32)
    K_.SIN = A.alloc([16, NJ], F32)
    K_.RHO = A.alloc([16, NJ], F32)
    K_.PW = A.alloc([9, 2, 16], F32)
    K_.CAR = A.alloc([2, 16], F32)
    K_.mark = A.mark()
    LAM = A.alloc([3, 16], F32)
    Bt = A.alloc([2, 16, 16], F32)
    Ct = A.alloc([2, 16, 16], F32)
    DCOL = A.alloc([4], F32)
    ED = A.alloc([8, 2, 512], F32)
    CX = A.alloc([2, 512], F32)
    BB = A.alloc([2, 16, 16], F32)
    W3 = [A.alloc([16, 16], F32) for _ in range(6)]
    s = [A.alloc([16], F32) for _ in range(16)]
    TJ = [A.alloc([16, 32], F32) for _ in range(4)]
    KT0 = A.alloc([128], F32)
    PRE = ["s5pre"]

    def dve(fn):
        P.op("dve", fn, reads=PRE, writes=PRE)

    def act(fn):
        P.op("act", fn, reads=PRE, writes=PRE)

    def tt(o, a, b, op):
        dve(lambda e: e.tensor_tensor(o, a, b, op=op))

    def ts(o, a, s1, op0, s2=None, op1=None):
        if op1 is None:
            dve(lambda e: e.tensor_scalar(o, a, s1, None, op0=op0))
        else:
            dve(lambda e: e.tensor_scalar(o, a, s1, s2, op0=op0, op1=op1))

    MUL, ADD, SUB = ALU.mult, ALU.add, ALU.subtract

    def cmul(ore, oim, are, aim, bre, bim, t1, t2):
        tt(t1, aim, bim, MUL)
        tt(t2, are, bre, MUL)
        tt(ore, t2, t1, SUB)
        tt(t1, are, bim, MUL)
        tt(t2, aim, bre, MUL)
        tt(oim, t2, t1, ADD)

    P.op("sp", lambda e: e.dma_start(out=LAM, in_=c.s5lam_d), writes=PRE, dma="d_s5lam")
    P.op("sp", lambda e: e.dma_start(out=Bt, in_=c.s5b_d), writes=["s5Bt"], dma="d_s5b")
    P.op("sp", lambda e: e.dma_start(out=Ct, in_=c.s5c_d), writes=["s5Ct"], dma="d_s5c")
    P.op("sp", lambda e: e.dma_start(out=DCOL, in_=c.s5d_d), writes=["s5D"], dma="d_s5d")
    P.op("pool", lambda e: e.dma_start(out=K_.WINU, in_=c.w_in_d.rearrange("(k p) n -> p k n", p=128)[:, :, 2048:2560]), writes=["WINU"], dma="d_winu")
    lr, li, ldt = LAM[:, 0, :], LAM[:, 1, :], LAM[:, 2, :]
    dt, rl, th, mag, rho8, sn, cs, t1, t2, t3, are, aim, qre, qim, den, am1 = s
    act(lambda e: e.activation(out=dt, in_=ldt, func=AF.Exp))
    tt(rl, lr, dt, MUL)
    tt(th, li, dt, MUL)
    act(lambda e: e.activation(out=mag, in_=rl, func=AF.Exp))
    act(lambda e: e.activation(out=rho8, in_=rl, func=AF.Exp, scale=8.0))
    ts(t1, th, 1.0 / 32, MUL)
    ts(t2, th, 1.0 / 32, MUL, math.pi / 2, ADD)
    act(lambda e: e.activation(out=sn, in_=t1, func=AF.Sin))
    act(lambda e: e.activation(out=cs, in_=t2, func=AF.Sin))
    for _ in range(5):
        tt(t1, cs, cs, MUL)
        tt(t2, sn, sn, MUL)
        tt(t3, cs, sn, MUL)
        tt(cs, t1, t2, SUB)
        ts(sn, t3, 2.0, MUL)
    tt(are, mag, cs, MUL)
    tt(aim, mag, sn, MUL)
    tt(t1, lr, lr, MUL)
    tt(t2, li, li, MUL)
    tt(den, t1, t2, ADD)
    dve(lambda e: e.reciprocal(den, den))
    ts(am1, are, -1.0, ADD)
    tt(t1, am1, lr, MUL)
    tt(t2, aim, li, MUL)
    tt(t1, t1, t2, ADD)
    tt(qre, t1, den, MUL)
    tt(t1, aim, lr, MUL)
    tt(t2, am1, li, MUL)
    tt(t1, t1, t2, SUB)
    tt(qim, t1, den, MUL)
    PRE.extend(["s5Bt", "s5Ct", "s5D"])

    def bc(a):
        return a.unsqueeze(2).to_broadcast([128, 16, 16])

    cmul(BB[:, 0], BB[:, 1], bc(qre), bc(qim), Bt[:, 0], Bt[:, 1], W3[0], W3[1])
    PW = K_.PW
    dve(lambda e: e.memset(PW[:, 0, 0, :], 1.0))
    dve(lambda e: e.memset(PW[:, 0, 1, :], 0.0))
    for k in range(1, 9):
        cmul(PW[:, k, 0, :], PW[:, k, 1, :], PW[:, k - 1, 0, :], PW[:, k - 1, 1, :], are, aim, t1, t2)
    for d in range(8):
        cmul(W3[2], W3[3], bc(PW[:, d, 0, :]), bc(PW[:, d, 1, :]), BB[:, 0], BB[:, 1], W3[0], W3[1])
        for part in range(2):
            ev = ED[:, d, part, :].rearrange("p (m g c) -> p m g c", m=16, g=2)
            for g in range(2):
                ts(ev[:, :, g, :], W3[2 + part], half[:, g:g + 1], MUL)
    for part in range(2):
        cv = CX[:, part, :].rearrange("p (m g c) -> p m g c", m=16, g=2)
        for g in range(2):
            ts(cv[:, :, g, :], Ct[:, part], half[:, g:g + 1], MUL, (1.0 if part == 0 else -1.0), MUL)
    n = 0
    for part in range(2):
        for q in range(4):
            for dh in range(2):
                bank = n % 2
                n += 1
                for dd in range(4):
                    d = dh * 4 + dd
                    P.op("pe", lambda e, bank=bank, dd=dd, d=d, part=part, q=q: e.transpose(psum_t[:, bank, dd * 128:(dd + 1) * 128], ED[:, d, part, q * 128:(q + 1) * 128], ident),
                         reads=PRE + ["cst"], writes=[f"bank{bank}"])
                P.op("act", lambda e, bank=bank, part=part, q=q, dh=dh: e.activation(out=K_.WinT[:, part, q, dh * 512:(dh + 1) * 512], in_=psum_t[:, bank, :], func=AF.Identity),
                     reads=[f"bank{bank}"], writes=["WinT"])
    P.op("dve", lambda e: e.tensor_copy(K_.WinT3[64:128], K_.WinT[64:128]), reads=["WinT"], writes=["WinT3"])
    P.op("dve", lambda e: e.memset(K_.WinT3[64:96], 0.0), reads=["WinT3"], writes=["WinT3"])
    for q in range(4):
        for d in range(8):
            bank = 2 + (n % 2)
            n += 1
            P.op("pe", lambda e, bank=bank, d=d, q=q: e.matmul(psum_t[:, bank, 0:128], ED[:, d, 0, q * 128:(q + 1) * 128], CX[:, 0, q * 128:(q + 1) * 128], start=True, stop=False),
                 reads=PRE, writes=[f"bank{bank}"])
            P.op("pe", lambda e, bank=bank, d=d, q=q: e.matmul(psum_t[:, bank, 0:128], ED[:, d, 1, q * 128:(q + 1) * 128], CX[:, 1, q * 128:(q + 1) * 128], start=False, stop=True),
                 reads=PRE, writes=[f"bank{bank}"])
            if d == 0:
                P.op("dve", lambda e, bank=bank: e.tensor_tensor(KT0, psum_t[:, bank, 0:128], mask16, op=MUL), reads=[f"bank{bank}", "cst"], writes=["KT0"])
                P.op("dve", lambda e, q=q: e.scalar_tensor_tensor(K_.KTA[:, q, 0:128], ident, DCOL[:, q:q + 1], KT0, op0=MUL, op1=ADD), reads=["KT0", "cst"] + PRE, writes=["KTA"])
            else:
                P.op("dve", lambda e, bank=bank, q=q, d=d: e.tensor_tensor(K_.KTA[:, q, d * 128:(d + 1) * 128], psum_t[:, bank, 0:128], mask16, op=MUL),
                     reads=[f"bank{bank}", "cst"], writes=["KTA"])
    for mp in range(8):
        pr, pi_ = bc(PW[:, mp + 1, 0, :]), bc(PW[:, mp + 1, 1, :])
        tt(W3[0], Ct[:, 0], pr, MUL)
        tt(W3[1], Ct[:, 1], pi_, MUL)
        tt(W3[2], W3[0], W3[1], SUB)
        tt(W3[0], Ct[:, 0], pi_, MUL)
        tt(W3[1], Ct[:, 1], pr, MUL)
        tt(W3[3], W3[0], W3[1], ADD)
        for part in range(2):
            ov = K_.COUT[:, part, :, :].rearrange("p m (a f) -> p m a f", a=8)
            for g in range(2):
                ts(ov[:, :, mp, g * 16:(g + 1) * 16], W3[2 + part], half[:, g:g + 1], MUL, (1.0 if part == 0 else -1.0), MUL)
    e1c, e1s = t1, t2
    dve(lambda e: e.reciprocal(t3, rho8))
    tt(e1c, PW[:, 8, 0, :], t3, MUL)
    tt(e1s, PW[:, 8, 1, :], t3, MUL)
    COS, SIN, RHO = K_.COS, K_.SIN, K_.RHO
    dve(lambda e: e.memset(COS[:, :, 0:1], 1.0))
    dve(lambda e: e.memset(SIN[:, :, 0:1], 0.0))
    dve(lambda e: e.tensor_copy(COS[:, :, 1:2], e1c.unsqueeze(2)))
    dve(lambda e: e.tensor_copy(SIN[:, :, 1:2], e1s.unsqueeze(2)))
    pc, ps_ = den, am1
    dve(lambda e: e.tensor_copy(pc, e1c))
    dve(lambda e: e.tensor_copy(ps_, e1s))
    nn = 2
    while nn < NJ:
        tt(qre, pc, pc, MUL)
        tt(qim, ps_, ps_, MUL)
        tt(t3, pc, ps_, MUL)
        tt(pc, qre, qim, SUB)
        ts(ps_, t3, 2.0, MUL)
        bcn = lambda a, nn=nn: a.unsqueeze(2).to_broadcast([128, 16, nn])
        a1, a2, a3, a4 = (TJ[i][:, :, 0:nn] for i in range(4))
        tt(a1, COS[:, :, 0:nn], bcn(pc), MUL)
        tt(a2, SIN[:, :, 0:nn], bcn(ps_), MUL)
        tt(a3, SIN[:, :, 0:nn], bcn(pc), MUL)
        tt(a4, COS[:, :, 0:nn], bcn(ps_), MUL)
        tt(COS[:, :, nn:2 * nn], a1, a2, SUB)
        tt(SIN[:, :, nn:2 * nn], a3, a4, ADD)
        nn *= 2
    dve(lambda e: e.tensor_copy(RHO, rho8.unsqueeze(2).to_broadcast([128, 16, NJ])))
    dve(lambda e: e.memset(RHO[:, :, 0:1], 0.0))
    dve(lambda e: e.memset(K_.CAR, 0.0))
    return K_


def s5_pass(c):
    P, A, psum_t, K_ = c.P, c.A, c.psum_t, c.K5
    ident, identb, smallv, modcol = c.ident, c.identb, c.smallv, c.modcol
    TT, NJ = 512, 64
    NST = c.dbg.get("nst5", S // TT)
    b_in_c = smallv[:, 0:36]
    XIN = [A.alloc([D], F32) for _ in range(4)]
    hTs = [A.alloc([8, TT], BF16) for _ in range(2)]
    XQs = [A.alloc([4, TT], BF16) for _ in range(2)]
    Tm = [A.alloc([16, NJ], F32) for _ in range(6)]
    ST = A.alloc([2, 16, NJ], BF16)
    YSC = A.alloc([1024], BF16)
    YSF = A.alloc([4, TT], BF16)
    CT = A.alloc([2, 16], F32)
    COS, SIN, RHO, PW, CAR = K_.COS, K_.SIN, K_.RHO, K_.PW, K_.CAR
    MUL, ADD, SUB = ALU.mult, ALU.add, ALU.subtract
    psTb = psum_t[:, 1, :].bitcast(BF16)

    def v4(t):
        return t.rearrange("p (q ml) j -> p q ml j", q=4)

    def f2(t):
        return t.rearrange("p m j -> p (m j)")

    def load_x(g):
        sl = g % 4
        P.op("sp", lambda e, g=g, sl=sl: e.dma_start(out=XIN[sl], in_=c.x_d[g * 128:(g + 1) * 128, :]), writes=[f"xin{sl}"], dma=f"d_xin{sl}")

    for g in range(4):
        load_x(g)
    for s in range(NST):
        t0 = s * TT
        hT = hTs[s % 2]
        XQ = XQs[s % 2]
        sp_ = s % 2
        for k in range(8):
            for tt_ in range(4):
                sl = (4 * s + tt_) % 4
                P.op("pe", lambda e, tt_=tt_, sl=sl, k=k: e.transpose(psum_t[:, 0, tt_ * 128:(tt_ + 1) * 128], XIN[sl][:, k * 128:(k + 1) * 128], ident),
                     reads=[f"xin{sl}", "cst"], writes=["bank0"])
            P.op("act", lambda e, k=k, hT=hT: e.activation(out=hT[:, k, :], in_=psum_t[:, 0, :], func=AF.Identity, scale=modcol[:, k:k + 1], bias=modcol[:, 8 + k:9 + k]),
                 reads=["bank0", "modcol", "modcol2"], writes=[f"hT{sp_}_{k}"])
        if s + 1 < S // TT:
            for tt_ in range(4):
                load_x(4 * (s + 1) + tt_)
        for q in range(4):
            for k in range(8):
                P.op("pe", lambda e, q=q, k=k, hT=hT: e.matmul(psum_t[:, 1, :], K_.WINU[:, k, q * 128:(q + 1) * 128], hT[:, k, :], start=(k == 0), stop=(k == 7)),
                     reads=["WINU", f"hT{sp_}_{k}"], writes=["bank1"])
            P.op("act", lambda e, q=q, XQ=XQ: e.activation(out=XQ[:, q, :], in_=psum_t[:, 1, :], func=AF.Identity, bias=b_in_c[:, 16 + q:17 + q]), reads=["bank1", "smallv"], writes=[f"XQ{sp_}_{q}"])
        for ml in range(4):
            for part in range(2):
                for q in range(4):
                    reg = (part * 4 + q) * 64
                    for k in range(8):
                        if ml < 3:
                            P.op("pe", lambda e, ml=ml, part=part, q=q, k=k, reg=reg, XQ=XQ: e.matmul(psum_t[:, 2 + ml, reg:reg + 64], K_.WinT[32 * ml:32 * ml + 32, part, q, (7 - k) * 128:(8 - k) * 128],
                                                                                              XQ[32 * ml:32 * ml + 32, q, k:TT:8], start=(k == 0), stop=(k == 7)),
                                 reads=["WinT", f"XQ{sp_}_{q}"], writes=[f"bank{2 + ml}"])
                        else:
                            P.op("pe", lambda e, ml=ml, part=part, q=q, k=k, reg=reg, XQ=XQ: e.matmul(psum_t[:, 2 + ml, reg:reg + 64], K_.WinT3[64:128, part, q, (7 - k) * 128:(8 - k) * 128],
                                                                                              XQ[64:128, q, k:TT:8], start=(k == 0), stop=(k == 7)),
                                 reads=["WinT3", f"XQ{sp_}_{q}"], writes=[f"bank{2 + ml}"])
        vbanks = [f"bank{2 + ml}" for ml in range(4)]
        Vre = psum_t[:, 2:6, 0:256].rearrange("p ml (q j) -> p q ml j", q=4)
        Vim = psum_t[:, 2:6, 256:512].rearrange("p ml (q j) -> p q ml j", q=4)
        T0, T1, T2, T3, T4, T5 = Tm
        P.op("dve", lambda e: e.tensor_tensor(v4(T0), v4(COS), Vre, op=MUL), reads=vbanks + ["s5pre"], writes=["T0"])
        P.op("dve", lambda e: e.tensor_tensor(v4(T1), v4(SIN), Vim, op=MUL), reads=vbanks + ["s5pre"], writes=["T1"])
        P.op("dve", lambda e: e.tensor_tensor(v4(T2), v4(COS), Vim, op=MUL), reads=vbanks + ["s5pre"], writes=["T2"])
        P.op("dve", lambda e: e.tensor_tensor(v4(T3), v4(SIN), Vre, op=MUL), reads=vbanks + ["s5pre"], writes=["T3"])
        P.op("pool", lambda e: e.tensor_tensor(T0, T0, T1, op=ADD), reads=["T0", "T1"], writes=["T0"])
        P.op("pool", lambda e: e.tensor_tensor(T2, T2, T3, op=SUB), reads=["T2", "T3"], writes=["T2"])
        P.op("dve", lambda e: e.tensor_tensor(CT[:, 0, :], PW[:, 8, 0, :], CAR[:, 0, :], op=MUL), reads=["car", "s5pre"], writes=["CT"])
        P.op("dve", lambda e: e.tensor_tensor(CT[:, 1, :], PW[:, 8, 1, :], CAR[:, 1, :], op=MUL), reads=["car", "s5pre"], writes=["CT"])
        P.op("dve", lambda e: e.tensor_tensor(CT[:, 0, :], CT[:, 0, :], CT[:, 1, :], op=SUB), reads=["CT"], writes=["CT"])
        P.op("dve", lambda e: e.tensor_tensor(T0[:, :, 0], T0[:, :, 0], CT[:, 0, :], op=ADD), reads=["CT", "T0"], writes=["T0"])
        P.op("dve", lambda e: e.tensor_tensor(CT[:, 0, :], PW[:, 8, 0, :], CAR[:, 1, :], op=MUL), reads=["car", "s5pre", "T0"], writes=["CT"])
        P.op("dve", lambda e: e.tensor_tensor(CT[:, 1, :], PW[:, 8, 1, :], CAR[:, 0, :], op=MUL), reads=["car", "s5pre"], writes=["CT"])
        P.op("dve", lambda e: e.tensor_tensor(CT[:, 0, :], CT[:, 0, :], CT[:, 1, :], op=ADD), reads=["CT"], writes=["CT"])
        P.op("dve", lambda e: e.tensor_tensor(T2[:, :, 0], T2[:, :, 0], CT[:, 0, :], op=ADD), reads=["CT", "T2"], writes=["T2"])
        P.op("pool", lambda e: e.tensor_copy(ST[:, :, :, 0], CAR), reads=["car"], writes=["ST"])
        P.op("dve", lambda e: e.tensor_tensor_scan(f2(T1), f2(RHO), f2(T0), 0.0, op0=MUL, op1=ADD), reads=["T0", "s5pre"], writes=["T1"])
        P.op("dve", lambda e: e.tensor_tensor_scan(f2(T3), f2(RHO), f2(T2), 0.0, op0=MUL, op1=ADD), reads=["T2", "s5pre"], writes=["T3"])
        P.op("pool", lambda e: e.tensor_tensor(T0, COS, T1, op=MUL), reads=["T1", "s5pre"], writes=["T0"])
        P.op("pool", lambda e: e.tensor_tensor(T2, SIN, T3, op=MUL), reads=["T3", "s5pre"], writes=["T2"])
        P.op("dve", lambda e: e.tensor_tensor(T4, SIN, T1, op=MUL), reads=["T1", "s5pre"], writes=["T4"])
        P.op("dve", lambda e: e.tensor_tensor(T5, COS, T3, op=MUL), reads=["T3", "s5pre"], writes=["T5"])
        P.op("pool", lambda e: e.tensor_tensor(T0, T0, T2, op=SUB), reads=["T0", "T2"], writes=["T0"])
        P.op("dve", lambda e: e.tensor_tensor(T4, T4, T5, op=ADD), reads=["T4", "T5"], writes=["T4"])
        P.op("pool", lambda e: e.tensor_copy(ST[:, 0, :, 1:NJ], T0[:, :, 0:NJ - 1]), reads=["T0"], writes=["ST"])
        P.op("pool", lambda e: e.tensor_copy(ST[:, 1, :, 1:NJ], T4[:, :, 0:NJ - 1]), reads=["T4"], writes=["ST"])
        P.op("dve", lambda e: e.tensor_copy(CAR[:, 0, :], T0[:, :, NJ - 1]), reads=["T0"], writes=["car"])
        P.op("dve", lambda e: e.tensor_copy(CAR[:, 1, :], T4[:, :, NJ - 1]), reads=["T4"], writes=["car"])
        for q in range(4):
            for k in range(8):
                lhs = XQ[:, q, k:TT:8]
                if k < 4:
                    P.op("pe", lambda e, lhs=lhs, k=k, q=q: e.matmul(psum_t[0:NJ, 6, k * 128:512], lhs, K_.KTA[:, q, 0:(4 - k) * 128], start=(k == 0), stop=False),
                         reads=[f"XQ{q}", "KTA"], writes=["bank6"])
                    P.op("pe", lambda e, lhs=lhs, k=k, q=q: e.matmul(psum_t[0:NJ, 7, :], lhs, K_.KTA[:, q, (4 - k) * 128:(8 - k) * 128], start=(k == 0), stop=False),
                         reads=[f"XQ{q}", "KTA"], writes=["bank7"])
                else:
                    P.op("pe", lambda e, lhs=lhs, k=k, q=q: e.matmul(psum_t[0:NJ, 7, (k - 4) * 128:512], lhs, K_.KTA[:, q, 0:(8 - k) * 128], start=False, stop=False),
                         reads=[f"XQ{q}", "KTA"], writes=["bank7"])
            for ml in range(4):
                m = 4 * q + ml
                for part in range(2):
                    last = (ml == 3 and part == 1)
                    for mp in range(8):
                        bb, ma = mp // 4, mp % 4
                        P.op("pe", lambda e, m=m, ml=ml, part=part, bb=bb, ma=ma, mp=mp, last=last: e.matmul(
                            psum_t[0:NJ, 6 + bb, ma * 128 + 32 * ml:ma * 128 + 32 * ml + 32], ST[:, part, m, :],
                            K_.COUT[:, part, m, mp * 40:mp * 40 + 32], start=False, stop=(last and ma == 3)),
                            reads=["ST", "COUT"], writes=[f"bank{6 + bb}"])
            P.op("act", lambda e: e.activation(out=YSC[0:NJ, :], in_=psum_t[0:NJ, 6:8, :].rearrange("p a b -> p (a b)"), func=AF.Gelu_apprx_tanh), reads=["bank6", "bank7"], writes=["YSC"])
            for mp in range(8):
                P.op("pe", lambda e, mp=mp: e.transpose(psTb[:, mp * 64:(mp + 1) * 64], YSC[0:NJ, mp * 128:(mp + 1) * 128], identb[0:NJ, 0:NJ]), reads=["YSC", "identb"], writes=["bank1"])
            P.op("dve", lambda e, q=q: e.tensor_copy(YSF[:, q, :].rearrange("p (j m) -> p m j", m=8), psTb[:, 0:512].rearrange("p (m j) -> p m j", m=8)), reads=["bank1"], writes=["YSF"])
        P.op("sp", lambda e, t0=t0: e.dma_start(out=c.ys_d[:, :, t0:t0 + TT].rearrange("q p t -> p q t"), in_=YSF), reads=["YSF"], writes=["ys_d"], dma="d_ysst")


def moe_phase(c):
    P, A, psum_t, dbg = c.P, c.A, c.psum_t, c.dbg
    cst, L, identb, onesb, ident = c.cst, c.L, c.identb, c.onesb, c.ident
    NEX = dbg.get("ne", NE)
    NBK = dbg.get("nbk", NB)
    NTC = dbg.get("ntc", NT)
    NBLK = NE * NB
    ecap1 = cst[:, 520:552]
    tokc = cst[:, 552:584]
    utri = cst[:, 128:256]
    mR = A.mark()
    if "lg" in dbg.get("inject", ()):
        P.op("sp", lambda e: e.dma_start(out=L, in_=c.lg_d), writes=["L"], dma="d_lgin")
    TB = [A.alloc([256], F32) for _ in range(2)]
    IDXF = A.alloc([NBLK], F32)
    IDX = A.alloc([NBLK], I32)
    WSL = A.alloc([NBLK], F32)
    mT_ = A.mark()
    def t3():
        return A.alloc([NT, NE], F32)
    M, E_, POS, TOT, OFF, SIDM, TMP = t3(), t3(), t3(), t3(), t3(), t3(), t3()
    Mb = A.alloc([NT * NE], BF16)
    utb = A.alloc([128], BF16)
    m8 = A.alloc([NT, 8], F32)
    s8 = A.alloc([NT, 8], F32)
    den = A.alloc([NT], F32)
    w4 = A.alloc([NT, 4], F32)
    SC = A.alloc([NT, 4, 2], F32)
    SIDI = A.alloc([NT, 4], I32)
    ZT = A.alloc([8448], F32)
    P.op("pool", lambda e: e.memset(ZT, 0.0), writes=["ZT"])
    P.op("sp", lambda e: e.dma_start(out=c.tab_d, in_=c.tabinit_d), writes=["tab_d"], dma="d_tabz")
    accv = c.acc_d.rearrange("(p r) d -> p (r d)", p=128)
    for j in range(4):
        P.op("sp", lambda e, j=j: e.dma_start(out=accv[:, j * 8448:(j + 1) * 8448], in_=ZT), reads=["ZT"], writes=[f"acc_z{j}"], dma=f"d_accz{j}")
    P.op("sp", lambda e: e.dma_start(out=c.h2_d[S:S + 128, :], in_=ZT[:, 0:512].bitcast(BF16)), reads=["ZT"], writes=["h2_pad"], dma="d_h2pad")
    P.op("dve", lambda e: e.tensor_copy(utb, utri), reads=["cst"], writes=["utb"])
    for i in range(NT):
        P.op("dve", lambda e, i=i: e.max(out=m8[:, i, :], in_=L[:, i, :]), reads=["L"], writes=["m8"])
    P.op("dve", lambda e: e.tensor_tensor(M, L, m8[:, :, 3:4].to_broadcast([128, NT, NE]), op=ALU.is_ge), reads=["L", "m8"], writes=["M"])
    P.op("dve", lambda e: e.tensor_tensor(E_, L, m8[:, :, 0:1].to_broadcast([128, NT, NE]), op=ALU.subtract), reads=["L", "m8"], writes=["E"])
    P.op("act", lambda e: e.activation(out=E_, in_=E_, func=AF.Exp), reads=["E"], writes=["E"])
    P.op("dve", lambda e: e.tensor_tensor(E_, E_, M, op=ALU.mult), reads=["E", "M"], writes=["E"])
    P.op("dve", lambda e: e.tensor_reduce(out=den, in_=E_, axis=AX.X, op=ALU.add), reads=["E"], writes=["den"])
    P.op("dve", lambda e: e.reciprocal(den, den), reads=["den"], writes=["den"])
    P.op("dve", lambda e: e.tensor_tensor(E_, E_, den.unsqueeze(2).to_broadcast([128, NT, NE]), op=ALU.mult), reads=["E", "den"], writes=["E"])
    P.op("dve", lambda e: e.tensor_copy(Mb, M.rearrange("p a b -> p (a b)")), reads=["M"], writes=["Mb"])
    for hlf in range(2):
        P.op("pe", lambda e, hlf=hlf: e.matmul(psum_t[:, hlf, :], utb, Mb[:, hlf * 512:(hlf + 1) * 512], start=True, stop=True), reads=["utb", "Mb"], writes=[f"bank{hlf}"])
        P.op("pe", lambda e, hlf=hlf: e.matmul(psum_t[:, 2 + hlf, :], onesb, Mb[:, hlf * 512:(hlf + 1) * 512], start=True, stop=True), reads=["onesb", "Mb"], writes=[f"bank{2 + hlf}"])
        P.op("dve", lambda e, hlf=hlf: e.tensor_copy(POS.rearrange("p a b -> p (a b)")[:, hlf * 512:(hlf + 1) * 512], psum_t[:, hlf, :]), reads=[f"bank{hlf}"], writes=["POS"])
        P.op("act", lambda e, hlf=hlf: e.activation(out=TOT.rearrange("p a b -> p (a b)")[:, hlf * 512:(hlf + 1) * 512], in_=psum_t[:, 2 + hlf, :], func=AF.Identity), reads=[f"bank{2 + hlf}"], writes=["TOT"])
    P.op("dve", lambda e: e.memset(OFF[:, 0, :], 0.0), writes=["OFF"])
    for i in range(1, NT):
        P.op("dve", lambda e, i=i: e.tensor_tensor(OFF[:, i, :], OFF[:, i - 1, :], TOT[:, i - 1, :], op=ALU.add), reads=["OFF", "TOT"], writes=["OFF"])
    P.op("dve", lambda e: e.tensor_tensor(POS, POS, OFF, op=ALU.add), reads=["POS", "OFF"], writes=["POS"])
    P.op("dve", lambda e: e.tensor_scalar(TMP, POS, float(CAP), None, op0=ALU.is_lt), reads=["POS"], writes=["TMP"])
    P.op("dve", lambda e: e.tensor_tensor(TMP, TMP, M, op=ALU.mult), reads=["TMP", "M"], writes=["TMP"])
    P.op("dve", lambda e: e.tensor_tensor(SIDM, POS, ecap1.unsqueeze(1).to_broadcast([128, NT, NE]), op=ALU.add), reads=["POS", "cst"], writes=["SIDM"])
    P.op("dve", lambda e: e.tensor_tensor(SIDM, SIDM, TMP, op=ALU.mult), reads=["SIDM", "TMP"], writes=["SIDM"])
    P.op("dve", lambda e: e.tensor_scalar(SIDM, SIDM, -1.0, None, op0=ALU.add), reads=["SIDM"], writes=["SIDM"])
    for i in range(NT):
        P.op("dve", lambda e, i=i: e.max(out=s8[:, i, :], in_=SIDM[:, i, :]), reads=["SIDM"], writes=["s8"])
    for k in range(4):
        P.op("dve", lambda e, k=k: e.tensor_tensor(TMP, SIDM, s8[:, :, k:k + 1].to_broadcast([128, NT, NE]), op=ALU.is_equal), reads=["SIDM", "s8"], writes=["TMP"])
        P.op("dve", lambda e: e.tensor_tensor(TMP, TMP, E_, op=ALU.mult), reads=["TMP", "E"], writes=["TMP"])
        P.op("dve", lambda e, k=k: e.tensor_reduce(out=w4[:, :, k], in_=TMP, axis=AX.X, op=ALU.add), reads=["TMP"], writes=["w4"])
    for k in range(4):
        P.op("dve", lambda e, k=k: e.tensor_copy(SC[:, :, k, 0], tokc), reads=["cst"], writes=["SC"])
    P.op("dve", lambda e: e.tensor_copy(SC[:, :, :, 1], w4), reads=["w4"], writes=["SC"])
    P.op("dve", lambda e: e.tensor_copy(SIDI, s8[:, :, 0:4]), reads=["s8"], writes=["SIDI"])
    regc = {}

    def breg(e):
        if "r" not in regc:
            regc["r"] = e.to_reg(NE * CAP - 1)
        return regc["r"]

    for i in range(NT):
        for k in range(4):
            P.op("pool", lambda e, i=i, k=k: e.indirect_dma_start(out=c.tab_d, out_offset=bass.IndirectOffsetOnAxis(ap=SIDI[:, i, k:k + 1], axis=0),
                                                                  in_=SC[:, i, k, :], in_offset=None, bounds_check=breg(e), oob_is_err=False),
                 reads=["SC", "SIDI", "tab_d"], dma="d_scat")
    P.barrier()
    if dbg.get("mstop") == "route":
        return
    tabv = c.tab_d.rearrange("(b s) two -> b (s two)", s=128)
    for hb_ in range(2):
        P.op("sp", lambda e, hb_=hb_: e.dma_start(out=TB[hb_], in_=tabv[hb_ * 128:(hb_ + 1) * 128, :]), writes=[f"TB{hb_}"], dma=f"d_tb{hb_}")
        tv = TB[hb_].rearrange("p (s two) -> p s two", two=2)
        P.op("pe", lambda e, hb_=hb_, tv=tv: e.transpose(psum_t[:, hb_, 0:128], tv[:, :, 0], ident), reads=[f"TB{hb_}", "cst"], writes=[f"bank{hb_}"])
        P.op("pe", lambda e, hb_=hb_, tv=tv: e.transpose(psum_t[:, hb_, 128:256], tv[:, :, 1], ident), reads=[f"TB{hb_}", "cst"], writes=[f"bank{hb_}"])
        P.op("dve", lambda e, hb_=hb_: e.tensor_copy(IDXF[:, hb_ * 128:(hb_ + 1) * 128], psum_t[:, hb_, 0:128]), reads=[f"bank{hb_}"], writes=["IDXF"])
        P.op("dve", lambda e, hb_=hb_: e.tensor_copy(WSL[:, hb_ * 128:(hb_ + 1) * 128], psum_t[:, hb_, 128:256]), reads=[f"bank{hb_}"], writes=["WSL"])
    P.op("dve", lambda e: e.tensor_copy(IDX, IDXF), reads=["IDXF"], writes=["IDX"])
    A.reset(mT_)
    mE = A.mark()
    if dbg.get("mstop") == "table":
        return
    WGU = [A.alloc([8, 2 * D], BF16) for _ in range(2)]
    WDN = [A.alloc([8, D], BF16) for _ in range(2)]
    BGU = A.alloc([NE, 16], F32)
    BDB = [A.alloc([D], F32) for _ in range(2)]
    NXG = 8
    XG = [A.alloc([D], BF16) for _ in range(NXG)]
    XT = [A.alloc([8, 512], BF16) for _ in range(2)]
    Gt = [A.alloc([512], F32) for _ in range(2)]
    Ut = [A.alloc([512], F32) for _ in range(2)]
    SG = [A.alloc([512], F32) for _ in range(2)]
    ACT_ = [A.alloc([8, 512], BF16) for _ in range(2)]
    TY = [A.alloc([D], F32) for _ in range(2)]
    YS = [A.alloc([D], F32) for _ in range(4)]
    P.op("sp", lambda e: e.dma_start(out=BGU, in_=c.b_gu_d), writes=["BGU"], dma="d_bgu")
    P.op("dve", lambda e: e.tensor_scalar(BGU[:, :, 8:16], BGU[:, :, 8:16], 1.0, None, op0=ALU.add), reads=["BGU"], writes=["BGU"])

    def load_w(e_):
        bf = e_ % 2
        gv = c.w_gu_d[e_].rearrange("(k p) n -> p k n", p=128)
        for hk in range(2):
            P.op("pool", lambda e, bf=bf, hk=hk, gv=gv: e.dma_start(out=WGU[bf][:, hk * 4:(hk + 1) * 4, :], in_=gv[:, hk * 4:(hk + 1) * 4, :]),
                 writes=[f"WGU{bf}_{hk}"], dma=f"d_wgu{bf}_{hk}")
        P.op("pool", lambda e, bf=bf, e_=e_: e.dma_start(out=WDN[bf], in_=c.w_down_d[e_].rearrange("(k p) n -> p k n", p=128)), writes=[f"WDN{bf}"], dma=f"d_wdn{bf}")
        P.op("sp", lambda e, bf=bf, e_=e_: e.dma_start(out=BDB[bf], in_=c.b_down_d[e_:e_ + 1, :].partition_broadcast(128)), writes=[f"BDB{bf}"], dma=f"d_bdb{bf}")

    NGRP = NBK // 4
    groups = [(e_, gq) for e_ in range(NEX) for gq in range(NGRP)]
    psTs = [psum_t[:, 0, :].bitcast(BF16), psum_t[:, 7, :].bitcast(BF16)]
    ycnt = [0]
    tcnt = [0]
    deferred = []

    def gather(gi, defer=False):
        e_, gq = groups[gi]
        for bl in range(4):
            blk = e_ * NB + gq * 4 + bl
            sl = (gi * 4 + bl) % NXG

            def rec(sl=sl, blk=blk):
                P.op("pool", lambda e, sl=sl, blk=blk: e.indirect_dma_start(out=XG[sl], out_offset=None, in_=c.h2_d,
                                                                            in_offset=bass.IndirectOffsetOnAxis(ap=IDX[:, blk:blk + 1], axis=0)),
                     reads=["IDX", "h2_pad"], writes=[f"XG{sl}"], dma=f"d_xg{sl}")
            if defer:
                deferred.append(rec)
            else:
                rec()

    def front(gi):
        gb = gi % 2
        for bl in range(4):
            sl = (gi * 4 + bl) % NXG
            ti = tcnt[0] % 2
            tcnt[0] += 1
            psT = psTs[ti]
            tkey = "bank0" if ti == 0 else "bank7"
            for k in range(8):
                P.op("pe", lambda e, k=k, sl=sl, psT=psT: e.transpose(psT[:, k * 128:(k + 1) * 128], XG[sl][:, k * 128:(k + 1) * 128], identb), reads=[f"XG{sl}", "identb"], writes=[tkey])
            P.op("act", lambda e, gb=gb, bl=bl, psT=psT: e.activation(out=XT[gb][:, :, bl * 128:(bl + 1) * 128], in_=psT.rearrange("p (k s) -> p k s", k=8), func=AF.Identity),
                 reads=[tkey], writes=[f"XT{gb}"])

    def gu(gi):
        e_, gq = groups[gi]
        gb = gi % 2
        bf = e_ % 2
        for j in range(8):
            tb = j % 2
            bG, bU = 1 + 2 * tb, 2 + 2 * tb
            for k in range(8):
                P.op("pe", lambda e, bG=bG, j=j, k=k, bf=bf, gb=gb: e.matmul(psum_t[:, bG, :], WGU[bf][:, k, j * 128:(j + 1) * 128], XT[gb][:, k, :], start=(k == 0), stop=(k == 7)),
                     reads=[f"WGU{bf}_{k // 4}", f"XT{gb}"], writes=[f"bank{bG}"])
            for k in range(8):
                P.op("pe", lambda e, bU=bU, j=j, k=k, bf=bf, gb=gb: e.matmul(psum_t[:, bU, :], WGU[bf][:, k, (8 + j) * 128:(9 + j) * 128], XT[gb][:, k, :], start=(k == 0), stop=(k == 7)),
                     reads=[f"WGU{bf}_{k // 4}", f"XT{gb}"], writes=[f"bank{bU}"])
            P.op("dve", lambda e, bG=bG, j=j, e_=e_, tb=tb: e.tensor_scalar(Gt[tb], psum_t[:, bG, :], BGU[:, e_, j:j + 1], 7.0, op0=ALU.add, op1=ALU.min),
                 reads=[f"bank{bG}", "BGU"], writes=[f"G{tb}"])
            P.op("dve", lambda e, bU=bU, j=j, e_=e_, tb=tb: e.tensor_scalar(Ut[tb], psum_t[:, bU, :], BGU[:, e_, 8 + j:9 + j], 8.0, op0=ALU.add, op1=ALU.min),
                 reads=[f"bank{bU}", "BGU"], writes=[f"U{tb}"])
            P.op("act", lambda e, tb=tb: e.activation(out=SG[tb], in_=Gt[tb], func=AF.Silu, scale=1.702), reads=[f"G{tb}"], writes=[f"SG{tb}"])
            P.op("dve", lambda e, tb=tb, gb=gb, j=j: e.scalar_tensor_tensor(ACT_[gb][:, j, :], Ut[tb], -6.0, SG[tb], op0=ALU.max, op1=ALU.mult),
                 reads=[f"SG{tb}", f"U{tb}"], writes=[f"ACT{gb}_{j}"])
            if deferred:
                deferred.pop(0)()

    def down(gi):
        e_, gq = groups[gi]
        gb = gi % 2
        bf = e_ % 2
        for bl in range(4):
            blk = e_ * NB + gq * 4 + bl
            pb = ycnt[0] % 2
            ycnt[0] += 1
            yb_ = bl
            for hlf in range(2):
                bank = 5 + hlf
                for fc in range(8):
                    P.op("pe", lambda e, bank=bank, hlf=hlf, fc=fc, gb=gb, bf=bf, bl=bl: e.matmul(psum_t[:, bank, :], ACT_[gb][:, fc, bl * 128:(bl + 1) * 128], WDN[bf][:, fc, hlf * 512:(hlf + 1) * 512],
                                                                                                 start=(fc == 0), stop=(fc == 7)),
                         reads=[f"ACT{gb}_{fc}", f"WDN{bf}"], writes=[f"bank{bank}"])
                P.op("dve", lambda e, bank=bank, hlf=hlf, bf=bf, pb=pb: e.scalar_tensor_tensor(TY[pb][:, hlf * 512:(hlf + 1) * 512], psum_t[:, bank, :], 1.0 / 1.702, BDB[bf][:, hlf * 512:(hlf + 1) * 512], op0=ALU.mult, op1=ALU.add),
                     reads=[f"bank{bank}", f"BDB{bf}"], writes=[f"TY{pb}"])
            P.op("act", lambda e, pb=pb, yb_=yb_, blk=blk: e.activation(out=YS[yb_], in_=TY[pb], func=AF.Copy, scale=WSL[:, blk:blk + 1]), reads=[f"TY{pb}", "WSL"], writes=[f"YS{yb_}"])
            if "yb" in dbg.get("dump", ()):
                P.op("sp", lambda e, yb_=yb_, blk=blk: e.dma_start(out=c.yb_d[blk * 128:(blk + 1) * 128, :], in_=YS[yb_]), reads=[f"YS{yb_}"], writes=["yb_d"], dma=f"d_ybst{yb_}")

            def rec(yb_=yb_, blk=blk, first=(gq == 0 and bl == 0), e_=e_):
                P.op("pool", lambda e, yb_=yb_, blk=blk: e.indirect_dma_start(out=c.acc_d, out_offset=bass.IndirectOffsetOnAxis(ap=IDX[:, blk:blk + 1], axis=0),
                                                                            in_=YS[yb_], in_offset=None, compute_op=ALU.add),
                     reads=[f"YS{yb_}", "IDX"] + ([f"acc_{(e_ - 1) * NB + q_}" for q_ in range(NB)] + [f"acc_z{q_}" for q_ in range(4)] if first else []),
                     writes=[f"acc_{blk}"], dma=f"d_acc{yb_}")
            deferred.append(rec)

    load_w(0)
    gather(0)
    front(0)
    if len(groups) > 1:
        gather(1)
    for gi in range(len(groups)):
        e_, gq = groups[gi]
        gu(gi)
        while deferred:
            deferred.pop(0)()
        if gi + 1 < len(groups):
            front(gi + 1)
        if gq == 0 and e_ + 1 < NEX:
            load_w(e_ + 1)
        down(gi)
        if gi + 2 < len(groups):
            gather(gi + 2, defer=True)
    while deferred:
        deferred.pop(0)()
    P.barrier()
    if dbg.get("mstop") == "experts":
        return
    A.reset(mR)
    G2B = A.alloc([D], F32)
    LN2G = A.alloc([D], F32)
    LN2B = A.alloc([D], F32)
    GA = [A.alloc([D], F32) for _ in range(2)]
    X1T = [A.alloc([D], F32) for _ in range(2)]
    VV = [A.alloc([D], F32) for _ in range(2)]
    STATS = A.alloc([2, 2, 6], F32)
    MV = A.alloc([2, 2], F32)
    RSTD = A.alloc([2, 1], F32)
    MHALF = A.alloc([1], F32)
    P.op("pool", lambda e: e.memset(MHALF, -0.5), writes=["mhalf"])
    P.op("sp", lambda e: e.dma_start(out=G2B, in_=c.mod_d[0:1, 5 * D:6 * D].partition_broadcast(128)), writes=["G2B"], dma="d_g2b")
    P.op("sp", lambda e: e.dma_start(out=LN2G, in_=c.lnrows_d[2:3, :].partition_broadcast(128)), writes=["LN2G"], dma="d_ln2g")
    P.op("sp", lambda e: e.dma_start(out=LN2B, in_=c.lnrows_d[3:4, :].partition_broadcast(128)), writes=["LN2B"], dma="d_ln2b")
    P.op("dve", lambda e: e.tensor_scalar(G2B, G2B, 1.0, None, op0=ALU.add), reads=["G2B"], writes=["G2B"])
    def comb(i):
        pb = i % 2
        P.op("sp", lambda e, i=i, pb=pb: e.dma_start(out=GA[pb], in_=c.acc_d[i * 128:(i + 1) * 128, :]), writes=[f"GA{pb}"], dma=f"d_ga{pb}")
        P.op("sp", lambda e, i=i, pb=pb: e.dma_start(out=X1T[pb], in_=c.x1_d[i * 128:(i + 1) * 128, :]), writes=[f"X1T{pb}"], dma=f"d_x1t{pb}")
        g, v = GA[pb], VV[pb]
        P.op("pool", lambda e, g=g: e.tensor_tensor(g, g, G2B, op=ALU.mult), reads=[f"GA{pb}", "G2B"], writes=[f"GA{pb}"])
        P.op("dve", lambda e, g=g, v=v, pb=pb: e.scalar_tensor_tensor(v, X1T[pb], ALPHA, g, op0=ALU.mult, op1=ALU.add), reads=[f"X1T{pb}", f"GA{pb}"], writes=[f"VV{pb}"])
        for hlf in range(2):
            P.op("dve", lambda e, v=v, hlf=hlf, pb=pb: e.bn_stats(STATS[:, pb, hlf, :], v[:, hlf * 512:(hlf + 1) * 512]), reads=[f"VV{pb}"], writes=[f"stats{pb}"])
        P.op("dve", lambda e, pb=pb: e.bn_aggr(MV[:, pb, :], STATS[:, pb, :, :].rearrange("p a b -> p (a b)")), reads=[f"stats{pb}"], writes=[f"mv{pb}"])
        P.op("pool", lambda e, pb=pb: e.tensor_scalar(RSTD[:, pb, :], MV[:, pb, 1:2], EPS, None, op0=ALU.add), reads=[f"mv{pb}"], writes=[f"rstd{pb}"])
        P.op("pool", lambda e, pb=pb: e.tensor_tensor(RSTD[:, pb, :], RSTD[:, pb, :], MHALF, op=ALU.pow), reads=[f"rstd{pb}", "mhalf"], writes=[f"rstd{pb}"])
        P.op("dve", lambda e, v=v, pb=pb: e.tensor_scalar(v, v, MV[:, pb, 0:1], RSTD[:, pb, :], op0=ALU.subtract, op1=ALU.mult), reads=[f"VV{pb}", f"mv{pb}", f"rstd{pb}"], writes=[f"VV{pb}"])
        P.op("dve", lambda e, v=v: e.tensor_tensor(v, v, LN2G, op=ALU.mult), reads=[f"VV{pb}", "LN2G"], writes=[f"VV{pb}"])
        P.op("pool", lambda e, v=v: e.tensor_tensor(v, v, LN2B, op=ALU.add), reads=[f"VV{pb}", "LN2B"], writes=[f"VV{pb}"])
        P.op("sp", lambda e, i=i, v=v: e.dma_start(out=c.out_d[i * 128:(i + 1) * 128, :], in_=v), reads=[f"VV{pb}"], writes=["out_d"], dma=f"d_out{pb}")

    for i in range(0, NTC, 2):
        interleave(P, [lambda i=i: comb(i)] + ([lambda i=i: comb(i + 1)] if i + 1 < NTC else []))


def _consts():
    cst = np.zeros((128, 640), np.float32)
    cst[:, 0:128] = np.eye(128, dtype=np.float32)
    cst[:, 128:256] = np.triu(np.ones((128, 128), np.float32), 1)
    idx = np.arange(128)
    cst[:, 256:384] = (idx[:, None] // 16 == idx[None, :] // 16).astype(np.float32)
    cst[:, 384:512] = 1.0
    cst[:, 512] = (idx < 64).astype(np.float32)
    cst[:, 513] = (idx >= 64).astype(np.float32)
    cst[:, 514] = idx.astype(np.float32)
    cst[:, 520:552] = (np.arange(32) * CAP + 1)[None, :].astype(np.float32)
    cst[:, 552:584] = (idx[:, None] + 128 * np.arange(32)[None, :]).astype(np.float32)
    return cst


def prep_core_inputs(inp, b):
    f = lambda a: np.ascontiguousarray(np.asarray(a, dtype=np.float32))
    m = {}
    m["x"] = f(inp["x"][b])
    m["ccol"] = f(inp["c"][b].reshape(8, 128).T)
    m["w_ada"] = f(inp["w_ada"][0])
    m["b_ada"] = f(inp["b_ada"][0][None, :])
    m["w_in"] = f(inp["w_in"][0])
    sv = np.concatenate([
        inp["b_in"][0].reshape(36, 128),
        inp["conv_w"][0].reshape(4, 8, 128).transpose(1, 0, 2).reshape(32, 128),
        inp["conv_b"][0].reshape(8, 128), inp["b_rg_a"][0].reshape(8, 128), inp["b_rg_x"][0].reshape(8, 128),
        inp["lru_lambda"][0].reshape(8, 128)], axis=0)
    m["smallv"] = f(sv.T)
    m["w_rg"] = f(np.stack([inp["w_rg_a"][0], inp["w_rg_x"][0]], axis=0))
    m["w_rnn_out"] = f(inp["w_rnn_out"][0])
    m["w_glu"] = f(inp["w_glu"][0])
    m["w_out"] = f(inp["w_out"][0])
    m["lnrows"] = f(np.stack([inp["ln1_g"][0], inp["ln1_b"][0], inp["ln2_g"][0], inp["ln2_b"][0]], axis=0))
    m["w_router"] = f(inp["w_router"][0])
    m["b_router"] = f(inp["b_router"][0][None, :])
    m["w_gu"] = f(inp["w_gu"][0])
    m["b_gu"] = f(np.asarray(inp["b_gu"][0]).reshape(32, 16, 128).transpose(2, 0, 1))
    m["w_down"] = f(inp["w_down"][0])
    m["b_down"] = f(inp["b_down"][0])
    lane = lambda a: np.asarray(a).reshape(16, 128).T
    ldt = np.repeat(np.asarray(inp["s5_log_dt"][0]), 64).reshape(16, 128).T
    m["s5lam"] = f(np.stack([lane(inp["s5_lambda_re"][0]), lane(inp["s5_lambda_im"][0]), ldt], axis=1))
    lb = lambda a: np.asarray(a).reshape(16, 128, 16).transpose(1, 0, 2)
    m["s5b"] = f(np.stack([lb(inp["s5_b_re"][0]), lb(inp["s5_b_im"][0])], axis=1))
    lc = lambda a: np.asarray(a).reshape(16, 2, 16, 64).transpose(1, 3, 0, 2).reshape(128, 16, 16)
    m["s5c"] = f(np.stack([lc(inp["s5_c_re"][0]), lc(inp["s5_c_im"][0])], axis=1))
    m["s5d"] = f(np.asarray(inp["s5_d"][0]).reshape(4, 128).T)
    m["cst"] = _consts()
    ti = np.zeros((NE * CAP, 2), np.float32)
    ti[:, 0] = S + (np.arange(NE * CAP) % 128)
    m["tabinit"] = ti
    return m


_NC_CACHE = {}


def kernel(**inputs):
    if "nc" not in _NC_CACHE:
        _NC_CACHE["nc"] = build_program()
    nc = _NC_CACHE["nc"]
    in_maps = [prep_core_inputs(inputs, b) for b in range(8)]
    res = run_bass_kernel_spmd(nc, in_maps, core_ids=list(range(8)))
    return np.stack([np.asarray(r["out"], dtype=np.float32) for r in res.results], axis=0)
```

```python
import contextlib
import types
import numpy as np
import ml_dtypes
import concourse.bass as bass
import concourse.mybir as mybir
from concourse.bass_utils import run_bass_kernel_spmd

F32 = mybir.dt.float32
BF16 = mybir.dt.bfloat16
I32 = mybir.dt.int32
U8 = mybir.dt.uint8
ALU = mybir.AluOpType
AF = mybir.ActivationFunctionType
AX = mybir.AxisListType

D = 1024
S = 4096
NT = S // 128
NE = 32
CAP = 1024
NB = CAP // 128
ALPHA = 2.0 ** 0.25
EPS = 1e-5
ENGS = ("pe", "act", "dve", "pool", "sp")
EPOCH = 12000


class Prog:
    def __init__(self, nc):
        self.nc = nc
        self.ops = {e: [] for e in ENGS}
        self.cnt = {e: 0 for e in ENGS}
        self.res = {}
        self.dmacnt = {}
        self.waited = {e: {} for e in ENGS}
        self.semnames = []

    def _sem(self, name):
        if name not in self.semnames:
            self.semnames.append(name)
        return name

    def _tok_engine(self, eng):
        self.cnt[eng] += 1
        c = self.cnt[eng]
        ep = (c - 1) // EPOCH
        return (self._sem(f"E{eng}{ep}"), c - ep * EPOCH, eng)

    def op(self, eng, fn, reads=(), writes=(), dma=None):
        waits = []
        for k in reads:
            st = self.res.get(k)
            if st and st["w"] is not None:
                waits.append(st["w"])
        for k in writes:
            st = self.res.get(k)
            if st:
                if st["w"] is not None:
                    waits.append(st["w"])
                waits.extend(st["r"])
        if dma is not None:
            self.dmacnt[dma] = self.dmacnt.get(dma, 0) + 16
            tok = (self._sem(dma), self.dmacnt[dma], "dma")
        else:
            tok = self._tok_engine(eng)
        need = []
        for (s, v, e) in waits:
            if e == eng and dma is None and eng == "pe":
                continue
            if self.waited[eng].get(s, 0) >= v:
                continue
            self.waited[eng][s] = v
            need.append((s, v))
        for k in reads:
            st = self.res.setdefault(k, {"w": None, "r": []})
            st["r"].append(tok)
        for k in writes:
            self.res[k] = {"w": tok, "r": []}
        self.ops[eng].append((need, fn, (tok[0], 16 if dma is not None else 1)))
        return tok

    def barrier(self):
        toks = []
        for e in ENGS:
            if self.cnt[e] > 0:
                c = self.cnt[e]
                ep = (c - 1) // EPOCH
                toks.append((f"E{e}{ep}", c - ep * EPOCH))
        for s, v in self.dmacnt.items():
            toks.append((s, v))
        for e in ENGS:
            need = []
            for (s, v) in toks:
                if s.startswith(f"E{e}"):
                    continue
                if self.waited[e].get(s, 0) >= v:
                    continue
                self.waited[e][s] = v
                need.append((s, v))
            if need:
                self.ops[e].append((need, None, None))

    def emit(self):
        nc = self.nc
        with contextlib.ExitStack() as es:
            sems = {n: es.enter_context(nc.semaphore(n)) for n in self.semnames}
            block = es.enter_context(nc.Block())

            def run(engname, eng):
                for (need, fn, inc) in self.ops[engname]:
                    for (s, v) in need:
                        eng.wait_ge(sems[s], v)
                    if fn is not None:
                        fn(eng).then_inc(sems[inc[0]], inc[1])

            @block.sync
            def _(e):
                run("sp", e)

            @block.scalar
            def _(e):
                run("act", e)

            @block.vector
            def _(e):
                run("dve", e)

            @block.gpsimd
            def _(e):
                run("pool", e)

            @block.tensor
            def _(e):
                run("pe", e)


def interleave(P, builders):
    chains = []
    prev = P.__dict__.get("op")
    for b in builders:
        lst = []
        P.op = lambda *a, _l=lst, **k: _l.append((a, k))
        try:
            b()
        finally:
            if prev is None:
                del P.op
            else:
                P.op = prev
        chains.append(lst)
    n = max(len(l) for l in chains)
    for i in range(n):
        for l in chains:
            if i < len(l):
                P.op(*l[i][0], **l[i][1])


class Arena:
    def __init__(self, t, size):
        self.t = t
        self.size = size
        self.off = 0

    def mark(self):
        return self.off

    def reset(self, m):
        self.off = m

    def alloc(self, shape, dt):
        esz = {F32: 4, BF16: 2, I32: 4, U8: 1}[dt]
        n = int(np.prod(shape))
        nb = (n * esz + 31) // 32 * 32
        assert self.off + nb <= self.size, f"arena overflow {self.off + nb} > {self.size}"
        v = self.t[:, self.off:self.off + n * esz].bitcast(dt)
        self.off += nb
        if len(shape) == 2:
            v = v.rearrange("p (a b) -> p a b", a=shape[0])
        elif len(shape) == 3:
            v = v.rearrange("p (a b c) -> p a b c", a=shape[0], b=shape[1])
        return v


def build_program(dbg=None):
    dbg = dbg or {}
    stop = dbg.get("stop", "end")
    nc = bass.Bass("TRN2", target_bir_lowering=False)
    P = Prog(nc)
    outs_dbg = {}

    def dram_in(name, shape, dt=F32):
        return nc.dram_tensor(name, list(shape), dt, kind="ExternalInput").ap()

    def dram_scr(name, shape, dt, inject=False):
        if inject and name in dbg.get("inject", ()):
            kind = "ExternalInput"
        elif name in dbg.get("dump", ()):
            kind = "ExternalOutput"
        else:
            kind = "Internal"
        return nc.dram_tensor(name, list(shape), dt, kind=kind).ap()

    x_d = dram_in("x", [S, D])
    ccol_d = dram_in("ccol", [128, 8])
    w_ada_d = dram_in("w_ada", [D, 6 * D])
    b_ada_d = dram_in("b_ada", [1, 6 * D])
    w_in_d = dram_in("w_in", [D, 4608])
    smallv_d = dram_in("smallv", [128, 100])
    w_rg_d = dram_in("w_rg", [2, 8, 128, 128])
    w_rnn_out_d = dram_in("w_rnn_out", [D, D])
    w_glu_d = dram_in("w_glu", [512, 2 * D])
    w_out_d = dram_in("w_out", [D, D])
    lnrows_d = dram_in("lnrows", [4, D])
    w_router_d = dram_in("w_router", [D, NE])
    b_router_d = dram_in("b_router", [1, NE])
    w_gu_d = dram_in("w_gu", [NE, D, 2 * D])
    b_gu_d = dram_in("b_gu", [128, NE, 16])
    w_down_d = dram_in("w_down", [NE, D, D])
    b_down_d = dram_in("b_down", [NE, D])
    s5lam_d = dram_in("s5lam", [128, 3, 16])
    s5b_d = dram_in("s5b", [128, 2, 16, 16])
    s5c_d = dram_in("s5c", [128, 2, 16, 16])
    s5d_d = dram_in("s5d", [128, 4])
    cst_d = dram_in("cst", [128, 640])
    out_d = nc.dram_tensor("out", [S, D], F32, kind="ExternalOutput").ap()

    ys_d = dram_scr("ys", [4, 128, S], BF16, inject=True)
    x1_d = dram_scr("x1", [S, D], F32, inject=True)
    h2_d = dram_scr("h2", [S + 128, D], BF16, inject=True)
    lg_d = dram_scr("lg", [128, NT, NE], F32, inject=True)
    tab_d = dram_scr("tab", [NE * CAP, 2], F32)
    yb_d = dram_scr("yb", [NE * CAP if "yb" in dbg.get("dump", ()) else 128, D], F32)
    acc_d = dram_scr("acc", [S + 128, D], F32)
    tabinit_d = dram_in("tabinit", [NE * CAP, 2])
    mod_d = dram_scr("modr", [1, 6 * D], F32)

    ARENA = 197 * 1024
    with contextlib.ExitStack() as es:
        arena_t = es.enter_context(nc.sbuf_tensor("arena", [128, ARENA], U8))
        pers_t = es.enter_context(nc.sbuf_tensor("pers", [128, 10 * 1024], U8))
        psum_t = es.enter_context(nc.psum_tensor("ps", [128, 8, 512], F32))
        A = Arena(arena_t, ARENA)
        PA = Arena(pers_t, 10 * 1024)

        cst = PA.alloc([640], F32)
        ident = cst[:, 0:128]
        smallv = PA.alloc([100], F32)
        modcol = PA.alloc([16], F32)
        cf = PA.alloc([8], F32)
        L = PA.alloc([NT, NE], F32)
        identb = PA.alloc([128], BF16)
        onesb = PA.alloc([128], BF16)
        ones_f = cst[:, 384:512]
        epsc = PA.alloc([1], F32)

        b_in_c = smallv[:, 0:36]
        conv_w_c = smallv[:, 36:68]
        conv_b_c = smallv[:, 68:76]
        b_rga_c = smallv[:, 76:84]
        b_rgx_c = smallv[:, 84:92]
        lam_c = smallv[:, 92:100]

        def PS(bank, lo=0, hi=512):
            return psum_t[:, bank, lo:hi]

        P.op("sp", lambda e: e.dma_start(out=cst, in_=cst_d), writes=["cst"], dma="d_cst")
        P.op("sp", lambda e: e.dma_start(out=smallv, in_=smallv_d), writes=["smallv"], dma="d_smallv")
        P.op("dve", lambda e: e.tensor_copy(identb, ident), reads=["cst"], writes=["identb"])
        P.op("dve", lambda e: e.tensor_copy(onesb, ones_f), reads=["cst"], writes=["onesb"])
        P.op("dve", lambda e: e.memset(epsc, EPS), writes=["epsc"])
        P.op("dve", lambda e: e.memset(L, 0.0), writes=["L"])

        mark0 = A.mark()
        K5 = None
        if "ys" not in dbg.get("inject", ()):
            K5 = s5_prepare(types.SimpleNamespace(**locals()))
        mA = A.mark()
        ccol = A.alloc([8], F32)
        cact = A.alloc([8], F32)
        modrow = A.alloc([6 * D], F32)
        wada = [A.alloc([8, 512], F32) for _ in range(2)]
        P.op("sp", lambda e: e.dma_start(out=ccol, in_=ccol_d), writes=["ccol"], dma="d_ccol")
        P.op("sp", lambda e: e.dma_start(out=modrow[0:1, :], in_=b_ada_d), writes=["modrow"], dma="d_bada")
        P.op("act", lambda e: e.activation(out=cact, in_=ccol, func=AF.Silu), reads=["ccol"], writes=["cact"])
        wada_v = w_ada_d.rearrange("(k p) n -> p k n", p=128)
        for j in range(12):
            buf = wada[j % 2]
            P.op("sp", lambda e, buf=buf, j=j: e.dma_start(out=buf, in_=wada_v[:, :, j * 512:(j + 1) * 512]),
                 writes=[f"wada{j % 2}"], dma=f"d_wada{j % 2}")
            bank = j % 2
            for k in range(8):
                P.op("pe", lambda e, buf=buf, k=k, bank=bank: e.matmul(PS(bank)[0:1, :], cact[:, k:k + 1], buf[:, k, :],
                                                                      start=(k == 0), stop=(k == 7)),
                     reads=[f"wada{j % 2}", "cact"], writes=[f"ps{bank}"])
            P.op("dve", lambda e, j=j, bank=bank: e.tensor_tensor(modrow[0:1, j * 512:(j + 1) * 512], PS(bank)[0:1, :],
                                                                 modrow[0:1, j * 512:(j + 1) * 512], op=ALU.add),
                 reads=[f"ps{bank}", "modrow"], writes=["modrow"])
        P.op("act", lambda e: e.activation(out=cf, in_=lam_c, func=AF.Exp, scale=-1.0), reads=["smallv"], writes=["cf"])
        P.op("act", lambda e: e.activation(out=cf, in_=cf, func=AF.Ln, bias=1.0), reads=["cf"], writes=["cf"])
        P.op("dve", lambda e: e.tensor_scalar(cf, cf, -8.0, None, op0=ALU.mult), reads=["cf"], writes=["cf"])

        P.op("sp", lambda e: e.dma_start(out=mod_d, in_=modrow[0:1, :]), reads=["modrow"], writes=["mod_d"], dma="d_modst")
        P.op("sp", lambda e: e.dma_start(out=modcol[:, 0:8], in_=mod_d[0:1, D:2 * D].rearrange("o (k p) -> p (o k)", p=128), allow_slow_non_contiguous=True),
             reads=["mod_d"], writes=["modcol"], dma="d_mc0")
        P.op("sp", lambda e: e.dma_start(out=modcol[:, 8:16], in_=mod_d[0:1, 0:D].rearrange("o (k p) -> p (o k)", p=128), allow_slow_non_contiguous=True),
             reads=["mod_d"], writes=["modcol2"], dma="d_mc1")
        P.op("dve", lambda e: e.tensor_scalar(modcol[:, 0:8], modcol[:, 0:8], 1.0, None, op0=ALU.add), reads=["modcol"], writes=["modcol"])
        P.barrier()
        A.reset(K5.mark if K5 is not None else mark0)

        if stop == "phaseA":
            return finish(nc, P, out_d, dbg)

        if "ys" not in dbg.get("inject", ()):
            s5_pass(types.SimpleNamespace(**locals()))
            P.barrier()
        A.reset(mark0)
        if stop == "s5":
            return finish(nc, P, out_d, dbg)

        if "x1" not in dbg.get("inject", ()):
            (mixer_pass2 if dbg.get('newmixer') else mixer_pass)(types.SimpleNamespace(**locals()))
            P.barrier()
            A.reset(mark0)
        if stop == "mixer":
            return finish(nc, P, out_d, dbg)

        moe_phase(types.SimpleNamespace(**locals()))
        return finish(nc, P, out_d, dbg)


def finish(nc, P, out_d, dbg):
    P.barrier()
    P.emit()
    return nc


def mixer_pass(c):
    P, A, psum_t = c.P, c.A, c.psum_t
    ident, smallv, modcol, cf, L = c.ident, c.smallv, c.modcol, c.cf, c.L
    TT = 256
    NST = S // TT
    NX = 2
    WIN = A.alloc([8, 4096], BF16)
    WRO = A.alloc([8, D], BF16)
    WGL = A.alloc([4, 2 * D], BF16)
    WOU = A.alloc([8, D], BF16)
    WRG = A.alloc([2, 8, 128], BF16)
    WR = A.alloc([8, NE], F32)
    LN1G = A.alloc([D], F32)
    LN1B = A.alloc([D], F32)
    P1 = A.alloc([D], F32)
    P2 = A.alloc([D], F32)
    BRB = A.alloc([NE], F32)
    XIN = [A.alloc([D], F32) for _ in range(NX)]
    hTs = [A.alloc([8, TT], BF16) for _ in range(2)]
    YSTs = [A.alloc([4, TT], BF16) for _ in range(2)]
    zAs = [A.alloc([8, TT], BF16) for _ in range(2)]
    XRES = A.alloc([D], F32)
    mT = A.alloc([8, TT], BF16)
    TS = []
    for i in range(2):
        TS.append(dict(xc=A.alloc([TT + 8], F32), xr=A.alloc([TT], F32), xrb=A.alloc([TT], BF16), thr=A.alloc([TT], F32),
                       thi=A.alloc([TT], F32), a2=A.alloc([TT], F32), ix=A.alloc([TT], F32)))
        TS[-1]["gy"] = TS[-1]["xc"][:, 0:TT]
        TS[-1]["hs"] = TS[-1]["thi"]
    MTS = [dict(t0=A.alloc([TT], F32), t1=A.alloc([TT], F32), tb=A.alloc([TT], F32)) for _ in range(2)]
    for M__ in MTS:
        M__['ta'] = M__['t0']
        M__['tbb'] = M__['tb']
    V = A.alloc([D], F32)
    H2 = A.alloc([D], F32)
    H2T = V.rearrange("p (k t) -> p k t", k=8)
    HALO = A.alloc([8, 4], F32)
    CARRY = A.alloc([8], F32)
    hbias = A.alloc([36], F32)
    hbrg = A.alloc([16], F32)
    cfh = A.alloc([8], F32)
    STATS = A.alloc([2, 6], F32)
    MV = A.alloc([2], F32)
    RSTD = A.alloc([1], F32)
    MHALF = A.alloc([1], F32)

    b_in_c = smallv[:, 0:36]
    conv_w_c = smallv[:, 36:68]
    conv_b_c = smallv[:, 68:76]

    w_in_v = c.w_in_d.rearrange("(k p) n -> p k n", p=128)
    for k in range(8):
        P.op("pool", lambda e, k=k: e.dma_start(out=WIN[:, k, 0:2048], in_=w_in_v[:, k, 0:2048]), writes=[f"WIN{k}a"], dma=f"d_win{k}a")
    P.op("pool", lambda e: e.dma_start(out=WRG, in_=c.w_rg_d.rearrange("a h i j -> i a h j")), writes=["WRG"], dma="d_wrg")
    P.op("pool", lambda e: e.dma_start(out=WRO, in_=c.w_rnn_out_d.rearrange("(k p) n -> p k n", p=128)), writes=["WRO"], dma="d_wro")
    P.op("pool", lambda e: e.dma_start(out=WGL, in_=c.w_glu_d.rearrange("(k p) n -> p k n", p=128)), writes=["WGL"], dma="d_wgl")
    for k in range(8):
        P.op("pool", lambda e, k=k: e.dma_start(out=WIN[:, k, 2048:4096], in_=w_in_v[:, k, 2560:4608]), writes=[f"WIN{k}b"], dma=f"d_win{k}b")
    P.op("sp", lambda e: e.dma_start(out=WR, in_=c.w_router_d.rearrange("(k p) n -> p k n", p=128)), writes=["WR"], dma="d_wr")
    P.op("sp", lambda e: e.dma_start(out=BRB, in_=c.b_router_d.partition_broadcast(128)), writes=["BRB"], dma="d_brb")
    P.op("sp", lambda e: e.dma_start(out=LN1G, in_=c.lnrows_d[0:1, :].partition_broadcast(128)), writes=["LN1G"], dma="d_ln1g")
    P.op("sp", lambda e: e.dma_start(out=LN1B, in_=c.lnrows_d[1:2, :].partition_broadcast(128)), writes=["LN1B"], dma="d_ln1b")
    P.op("sp", lambda e: e.dma_start(out=P1, in_=c.mod_d[0:1, 4 * D:5 * D].partition_broadcast(128)), reads=["mod_d"], writes=["P1"], dma="d_p1")
    P.op("sp", lambda e: e.dma_start(out=H2, in_=c.mod_d[0:1, 3 * D:4 * D].partition_broadcast(128)), reads=["mod_d"], writes=["H2"], dma="d_h2")
    P.op("sp", lambda e: e.dma_start(out=V, in_=c.mod_d[0:1, 2 * D:3 * D].partition_broadcast(128)), reads=["mod_d"], writes=["V"], dma="d_v")
    P.op("dve", lambda e: e.tensor_scalar(P1, P1, 1.0, None, op0=ALU.add), reads=["P1"], writes=["P1"])
    P.op("dve", lambda e: e.tensor_tensor(P2, LN1B, P1, op=ALU.mult), reads=["LN1B", "P1"], writes=["P2"])
    P.op("dve", lambda e: e.tensor_tensor(P2, P2, H2, op=ALU.add), reads=["P2", "H2"], writes=["P2"])
    P.op("dve", lambda e: e.tensor_tensor(P1, P1, LN1G, op=ALU.mult), reads=["P1", "LN1G"], writes=["P1"])
    P.op("dve", lambda e: e.tensor_scalar(V, V, 1.0, 0.5, op0=ALU.add, op1=ALU.mult), reads=["V"], writes=["V"])
    w_out_v = c.w_out_d.rearrange("(k p) n -> p k n", p=128)
    for k in range(8):
        sl = k % NX
        P.op("sp", lambda e, k=k, sl=sl: e.dma_start(out=XIN[sl], in_=w_out_v[:, k, :]), writes=[f"xin{sl}"], dma=f"d_xin{sl}")
        P.op("dve", lambda e, k=k, sl=sl: e.tensor_tensor(WOU[:, k, :], XIN[sl], V, op=ALU.mult), reads=[f"xin{sl}", "V"], writes=[f"WOU{k}"])
    P.op("dve", lambda e: e.tensor_scalar(hbias, b_in_c, 0.5, None, op0=ALU.mult), reads=["smallv"], writes=["hbias"])
    P.op("dve", lambda e: e.tensor_scalar(hbrg, smallv[:, 76:92], 0.5, None, op0=ALU.mult), reads=["smallv"], writes=["hbrg"])
    P.op("dve", lambda e: e.tensor_scalar(cfh, cf, 0.5, None, op0=ALU.mult), reads=["cf"], writes=["cfh"])
    P.op("dve", lambda e: e.memset(HALO, 0.0), writes=["halo"])
    P.op("dve", lambda e: e.memset(CARRY, 0.0), writes=["carry"])
    P.op("pool", lambda e: e.memset(MHALF, -0.5), writes=["mhalf"])

    hbn = [0]

    pools = {"A": [0, 1, 2, 3], "B4": [4, 5, 6, 7], "B2": [4, 5]}
    pcnt = {"A": 0, "B4": 0, "B2": 0}
    cur_pool = ["A"]

    def _nb():
        pl = cur_pool[0]
        i = pools[pl][pcnt[pl] % len(pools[pl])]
        pcnt[pl] += 1
        return i

    def hb():
        i = _nb()
        return psum_t[:, i, 0:256], f"bank{i}"

    def hb2():
        i = _nb()
        return psum_t[:, i, 0:256], psum_t[:, i, 256:512], f"bank{i}"

    def load_x(g):
        sl = g % NX
        P.op("sp", lambda e, g=g, sl=sl: e.dma_start(out=XIN[sl], in_=c.x_d[g * 128:(g + 1) * 128, :]), writes=[f"xin{sl}"], dma=f"d_xin{sl}")

    load_x(0)
    load_x(1)

    def sec_front(s):
        cur_pool[0] = "A"
        t0 = s * TT
        hT = hTs[s % 2]
        YST = YSTs[s % 2]
        hp_ = s % 2
        P.op("sp", lambda e, t0=t0, YST=YST: e.dma_start(out=YST, in_=c.ys_d[:, :, t0:t0 + TT].rearrange("q p t -> p q t")), writes=[f"yst{hp_}"], dma=f"d_yst{hp_}")
        for k in range(8):
            ps, key = hb()
            for tt in range(2):
                sl = (2 * s + tt) % NX
                P.op("pe", lambda e, ps=ps, tt=tt, sl=sl, k=k: e.transpose(ps[:, tt * 128:(tt + 1) * 128], XIN[sl][:, k * 128:(k + 1) * 128], ident),
                     reads=[f"xin{sl}", "cst"], writes=[key])
            P.op("act", lambda e, ps=ps, k=k, hT=hT: e.activation(out=hT[:, k, :], in_=ps, func=AF.Identity, scale=modcol[:, k:k + 1], bias=modcol[:, 8 + k:9 + k]),
                 reads=[key, "modcol", "modcol2"], writes=[f"hT{hp_}_{k}"])
        if 2 * s + 2 < NT:
            load_x(2 * s + 2)
        if 2 * s + 3 < NT:
            load_x(2 * s + 3)

    def sec_rg(s, hps):
        cur_pool[0] = "A"
        hT = hTs[s % 2]
        zA = zAs[s % 2]
        hp_ = s % 2
        for hp in hps:
            st = {}
            def front_body(j):
                h = 2 * hp + j
                T_ = TS[j]
                psx, kx = hb()
                for k in range(8):
                    P.op("pe", lambda e, psx=psx, k=k, h=h: e.matmul(psx, WIN[:, k, h * 128:(h + 1) * 128], hT[:, k, :], start=(k == 0), stop=(k == 7)),
                         reads=[f"WIN{k}a", f"hT{hp_}_{k}"], writes=[kx])
                P.op("pool", lambda e, T_=T_, h=h: e.tensor_copy(T_["xc"][:, 0:3], HALO[:, h, 0:3]), reads=["halo"], writes=[f"xc{j}"])
                P.op("act", lambda e, T_=T_, psx=psx, h=h: e.activation(out=T_["xc"][:, 3:3 + TT], in_=psx, func=AF.Identity, bias=b_in_c[:, h:h + 1]),
                     reads=[kx, "smallv"], writes=[f"xc{j}"])
                P.op("pool", lambda e, T_=T_, h=h: e.tensor_copy(HALO[:, h, 0:3], T_["xc"][:, TT:TT + 3]), reads=[f"xc{j}"], writes=["halo"])
                P.op("dve", lambda e, T_=T_, h=h: e.tensor_scalar(T_["xr"], T_["xc"][:, 0:TT], conv_w_c[:, 4 * h:4 * h + 1], conv_b_c[:, h:h + 1], op0=ALU.mult, op1=ALU.add),
                     reads=[f"xc{j}", "smallv"], writes=[f"xr{j}"])
                for q in range(1, 4):
                    P.op("dve", lambda e, T_=T_, h=h, q=q: e.scalar_tensor_tensor(T_["xr"], T_["xc"][:, q:q + TT], conv_w_c[:, 4 * h + q:4 * h + q + 1], T_["xr"], op0=ALU.mult, op1=ALU.add),
                         reads=[f"xc{j}", f"xr{j}"], writes=[f"xr{j}"])
                P.op("pool", lambda e, T_=T_: e.tensor_copy(T_["xrb"], T_["xr"]), reads=[f"xr{j}"], writes=[f"xrb{j}"])
                psr, psi, kr = hb2()
                ki = kr
                P.op("pe", lambda e, psr=psr, T_=T_, h=h: e.matmul(psr, WRG[:, 0, h, :], T_["xrb"], start=True, stop=True), reads=["WRG", f"xrb{j}"], writes=[kr])
                P.op("pe", lambda e, psi=psi, T_=T_, h=h: e.matmul(psi, WRG[:, 1, h, :], T_["xrb"], start=True, stop=True), reads=["WRG", f"xrb{j}"], writes=[ki])
                P.op("act", lambda e, T_=T_, psr=psr, h=h: e.activation(out=T_["thr"], in_=psr, func=AF.Tanh, scale=0.5, bias=hbrg[:, h:h + 1]),
                     reads=[kr, "hbrg"], writes=[f"thr{j}"])
                P.op("act", lambda e, T_=T_, psi=psi, h=h: e.activation(out=T_["thi"], in_=psi, func=AF.Tanh, scale=0.5, bias=hbrg[:, 8 + h:9 + h]),
                     reads=[ki, "hbrg"], writes=[f"thi{j}"])
                P.op("act", lambda e, T_=T_, h=h: e.activation(out=T_["thr"], in_=T_["thr"], func=AF.Exp, scale=cfh[:, h:h + 1], bias=cfh[:, h:h + 1]),
                     reads=[f"thr{j}", "cfh"], writes=[f"thr{j}"])
                P.op("pool", lambda e, T_=T_: e.tensor_tensor(T_["a2"], T_["thr"], T_["thr"], op=ALU.mult), reads=[f"thr{j}"], writes=[f"a2{j}"])
                P.op("dve", lambda e, T_=T_: e.scalar_tensor_tensor(T_["ix"], T_["thi"], 1.0, T_["xr"], op0=ALU.add, op1=ALU.mult),
                     reads=[f"thi{j}", f"xr{j}"], writes=[f"ix{j}"])
            interleave(P, [lambda j=j: front_body(j) for j in range(2)])
            for j in range(2):
                T_ = TS[j]
                P.op("act", lambda e, T_=T_: e.activation(out=T_["a2"], in_=T_["a2"], func=AF.Sqrt, scale=-1.0, bias=1.0), reads=[f"a2{j}"], writes=[f"a2{j}"])
            def back_body(j):
                h = 2 * hp + j
                T_ = TS[j]
                psy, ky = hb()
                for k in range(8):
                    P.op("pe", lambda e, psy=psy, k=k, h=h: e.matmul(psy, WIN[:, k, 1024 + h * 128:1024 + (h + 1) * 128], hT[:, k, :], start=(k == 0), stop=(k == 7)),
                         reads=[f"WIN{k}a", f"hT{hp_}_{k}"], writes=[ky])
                P.op("act", lambda e, T_=T_, psy=psy, h=h: e.activation(out=T_["gy"], in_=psy, func=AF.Gelu_apprx_tanh, bias=b_in_c[:, 8 + h:9 + h]),
                     reads=[ky, "smallv"], writes=[f"xc{j}"])
                P.op("dve", lambda e, T_=T_: e.scalar_tensor_tensor(T_["ix"], T_["a2"], 0.5, T_["ix"], op0=ALU.mult, op1=ALU.mult),
                     reads=[f"a2{j}", f"ix{j}"], writes=[f"ix{j}"])
                P.op("dve", lambda e, T_=T_, h=h: e.tensor_tensor_scan(T_["hs"], T_["thr"], T_["ix"], CARRY[:, h:h + 1], op0=ALU.mult, op1=ALU.add),
                     reads=[f"thr{j}", f"ix{j}", "carry"], writes=[f"thi{j}"])
                P.op("dve", lambda e, T_=T_, h=h: e.tensor_copy(CARRY[:, h:h + 1], T_["hs"][:, TT - 1:TT]), reads=[f"thi{j}"], writes=["carry"])
                P.op("pool", lambda e, T_=T_, h=h, zA=zA: e.tensor_tensor(zA[:, h, :], T_["gy"], T_["hs"], op=ALU.mult), reads=[f"xc{j}", f"thi{j}"], writes=[f"zA{hp_}_{h}"])
            interleave(P, [lambda j=j: back_body(j) for j in range(2)])

    def sec_merge(s, ccs):
        cur_pool[0] = "B4"
        hT = hTs[s % 2]
        zA = zAs[s % 2]
        YST = YSTs[s % 2]
        hp_ = s % 2
        def merge_body(cc):
            psA, kA = hb()
            for h in range(8):
                P.op("pe", lambda e, psA=psA, h=h, cc=cc, zA=zA: e.matmul(psA, WRO[:, h, cc * 128:(cc + 1) * 128], zA[:, h, :], start=(h == 0), stop=(h == 7)),
                     reads=["WRO", f"zA{hp_}_{h}"], writes=[kA])
            psB1, psB2, kB1 = hb2()
            kB2 = kB1
            for q in range(4):
                P.op("pe", lambda e, psB1=psB1, q=q, cc=cc, YST=YST: e.matmul(psB1, WGL[:, q, cc * 128:(cc + 1) * 128], YST[:, q, :], start=(q == 0), stop=(q == 3)),
                     reads=["WGL", f"yst{hp_}"], writes=[kB1])
            for q in range(4):
                P.op("pe", lambda e, psB2=psB2, q=q, cc=cc, YST=YST: e.matmul(psB2, WGL[:, q, D + cc * 128:D + (cc + 1) * 128], YST[:, q, :], start=(q == 0), stop=(q == 3)),
                     reads=["WGL", f"yst{hp_}"], writes=[kB2])
            psG0, psG1, kG0 = hb2()
            kG1 = kG0
            for k in range(8):
                P.op("pe", lambda e, psG0=psG0, k=k, cc=cc: e.matmul(psG0, WIN[:, k, 2048 + cc * 128:2048 + (cc + 1) * 128], hT[:, k, :], start=(k == 0), stop=(k == 7)),
                     reads=[f"WIN{k}b", f"hT{hp_}_{k}"], writes=[kG0])
            for k in range(8):
                P.op("pe", lambda e, psG1=psG1, k=k, cc=cc: e.matmul(psG1, WIN[:, k, 3072 + cc * 128:3072 + (cc + 1) * 128], hT[:, k, :], start=(k == 0), stop=(k == 7)),
                     reads=[f"WIN{k}b", f"hT{hp_}_{k}"], writes=[kG1])
            M_ = MTS[cc % 2]
            mk = cc % 2
            P.op("act", lambda e, psG0=psG0, cc=cc, M_=M_: e.activation(out=M_["t0"], in_=psG0, func=AF.Tanh, scale=0.5, bias=hbias[:, 20 + cc:21 + cc]), reads=[kG0, "hbias"], writes=[f"m_t0{mk}"])
            P.op("act", lambda e, psG1=psG1, cc=cc, M_=M_: e.activation(out=M_["t1"], in_=psG1, func=AF.Tanh, scale=0.5, bias=hbias[:, 28 + cc:29 + cc]), reads=[kG1, "hbias"], writes=[f"m_t1{mk}"])
            P.op("act", lambda e, psB2=psB2, M_=M_: e.activation(out=M_["tb"], in_=psB2, func=AF.Tanh, scale=0.5), reads=[kB2], writes=[f"m_tb{mk}"])
            P.op("dve", lambda e, psA=psA, M_=M_: e.scalar_tensor_tensor(M_["ta"], M_["t0"], 1.0, psA, op0=ALU.add, op1=ALU.mult), reads=[f"m_t0{mk}", kA], writes=[f"m_t0{mk}"])
            P.op("dve", lambda e, psB1=psB1, M_=M_: e.scalar_tensor_tensor(M_["tbb"], M_["tb"], 1.0, psB1, op0=ALU.add, op1=ALU.mult), reads=[f"m_tb{mk}", kB1], writes=[f"m_tb{mk}"])
            P.op("dve", lambda e, M_=M_: e.scalar_tensor_tensor(M_["tbb"], M_["t1"], 1.0, M_["tbb"], op0=ALU.add, op1=ALU.mult), reads=[f"m_t1{mk}", f"m_tb{mk}"], writes=[f"m_tb{mk}"])
            P.op("dve", lambda e, cc=cc, M_=M_: e.scalar_tensor_tensor(mT[:, cc, :], M_["tbb"], 0.5, M_["ta"], op0=ALU.mult, op1=ALU.add), reads=[f"m_tb{mk}", f"m_t0{mk}"], writes=[f"mT{cc}"])
        for cc in ccs:
            merge_body(cc)

    def sec_out(s, tts):
        cur_pool[0] = "B2"
        for tt in tts:
            g = 2 * s + tt
            sl = 0
            P.op("sp", lambda e, g=g: e.dma_start(out=XRES, in_=c.x_d[g * 128:(g + 1) * 128, :]), writes=["xres"], dma="d_xres")
            for hlf in range(2):
                for cc in range(8):
                    P.op("pe", lambda e, hlf=hlf, cc=cc, tt=tt: e.matmul(psum_t[:, 6 + hlf, :], mT[:, cc, tt * 128:(tt + 1) * 128], WOU[:, cc, hlf * 512:(hlf + 1) * 512],
                                                                       start=(cc == 0), stop=(cc == 7)),
                         reads=[f"mT{cc}", f"WOU{cc}"], writes=[f"bank{6 + hlf}"])
                P.op("dve", lambda e, hlf=hlf, sl=sl: e.scalar_tensor_tensor(V[:, hlf * 512:(hlf + 1) * 512], XRES[:, hlf * 512:(hlf + 1) * 512], ALPHA,
                                                                            psum_t[:, 6 + hlf, :], op0=ALU.mult, op1=ALU.add),
                     reads=["xres", f"bank{6 + hlf}"], writes=["V"])
                P.op("dve", lambda e, hlf=hlf: e.bn_stats(STATS[:, hlf, :], V[:, hlf * 512:(hlf + 1) * 512]), reads=["V"], writes=["stats"])
            P.op("dve", lambda e: e.bn_aggr(MV, STATS.rearrange("p a b -> p (a b)")), reads=["stats"], writes=["mv"])
            P.op("pool", lambda e: e.tensor_scalar(RSTD, MV[:, 1:2], EPS, None, op0=ALU.add), reads=["mv"], writes=["rstd"])
            P.op("pool", lambda e: e.tensor_tensor(RSTD, RSTD, MHALF, op=ALU.pow), reads=["rstd", "mhalf"], writes=["rstd"])
            P.op("dve", lambda e: e.tensor_scalar(V, V, MV[:, 0:1], RSTD, op0=ALU.subtract, op1=ALU.mult), reads=["V", "mv", "rstd"], writes=["V"])
            P.op("pool", lambda e, sl=sl: e.tensor_tensor(XRES, V, LN1G, op=ALU.mult), reads=["V", "LN1G"], writes=["xres"])
            P.op("pool", lambda e, sl=sl: e.tensor_tensor(XRES, XRES, LN1B, op=ALU.add), reads=["xres", "LN1B"], writes=["xres"])
            P.op("dve", lambda e: e.tensor_tensor(H2, V, P1, op=ALU.mult), reads=["V", "P1"], writes=["H2"])
            P.op("dve", lambda e: e.tensor_tensor(H2, H2, P2, op=ALU.add), reads=["H2", "P2"], writes=["H2"])
            P.op("sp", lambda e, g=g, sl=sl: e.dma_start(out=c.x1_d[g * 128:(g + 1) * 128, :], in_=XRES), reads=["xres"], writes=["x1_d"], dma="d_x1st")
            P.op("pool", lambda e, g=g: e.dma_start(out=c.h2_d[g * 128:(g + 1) * 128, :], in_=H2), reads=["H2"], writes=["h2_d"], dma="d_h2st")
            for kp in range(4):
                ps, key = hb()
                for j in range(2):
                    k = 2 * kp + j
                    P.op("pe", lambda e, ps=ps, j=j, k=k: e.transpose(ps[:, j * 128:(j + 1) * 128], H2[:, k * 128:(k + 1) * 128], ident), reads=["H2", "cst"], writes=[key])
                P.op("act", lambda e, ps=ps, kp=kp: e.activation(out=H2T[:, 2 * kp:2 * kp + 2, :], in_=ps.rearrange("p (a b) -> p a b", a=2), func=AF.Identity),
                     reads=[key], writes=["V"])
            psl, kl = hb()
            for k in range(8):
                P.op("pe", lambda e, psl=psl, k=k: e.matmul(psl[:, 0:NE], H2T[:, k, :], WR[:, k, :], start=(k == 0), stop=(k == 7)), reads=["V", "WR"], writes=[kl])
            P.op("dve", lambda e, psl=psl, g=g: e.tensor_tensor(L[:, g, :], psl[:, 0:NE], BRB, op=ALU.add), reads=[kl, "BRB"], writes=["L"])
    nst_ = c.dbg.get('nst', NST)
    sec_front(0)
    for s in range(nst_ + 1):
        for hp in range(4):
            bl_ = []
            if s < nst_:
                bl_.append(lambda s=s, hp=hp: sec_rg(s, [hp]))
            if s >= 1:
                bl_.append(lambda s=s, hp=hp: sec_merge(s - 1, [2 * hp, 2 * hp + 1]))
            interleave(P, bl_)
        bl_ = []
        if s >= 1:
            bl_.append(lambda s=s: sec_out(s - 1, [0, 1]))
        if s + 1 < nst_:
            bl_.append(lambda s=s: sec_front(s + 1))
        if bl_:
            interleave(P, bl_)
    P.op("sp", lambda e: e.dma_start(out=c.lg_d, in_=L), reads=["L"], writes=["lg_d"], dma="d_lgst")


def mixer_pass2(c):
    P, A, psum_t = c.P, c.A, c.psum_t
    ident, smallv, modcol, cf, L = c.ident, c.smallv, c.modcol, c.cf, c.L
    TT = 128
    NST = c.dbg.get("nst", S // TT)
    WIN = A.alloc([8, 4096], BF16)
    WRO = A.alloc([8, D], BF16)
    WGL = A.alloc([4, 2 * D], BF16)
    WOU = A.alloc([8, D], BF16)
    WRG = A.alloc([2, 8, 128], BF16)
    WR = A.alloc([8, NE], F32)
    LN1G = A.alloc([D], F32)
    LN1B = A.alloc([D], F32)
    P1 = A.alloc([D], F32)
    P2 = A.alloc([D], F32)
    BRB = A.alloc([NE], F32)
    XIN = [A.alloc([D], F32) for _ in range(3)]
    hTs = [A.alloc([8, TT], BF16) for _ in range(2)]
    YSTs = [A.alloc([4, TT], BF16)]
    zA = A.alloc([8, TT], BF16)
    mT = A.alloc([8, TT], BF16)
    XC = A.alloc([8, TT + 8], F32)
    XR = A.alloc([8, TT], F32)
    XRB = A.alloc([8, TT], BF16)
    THR = A.alloc([8, TT], F32)
    THI = A.alloc([8, TT], F32)
    A2 = A.alloc([8, TT], F32)
    IX = A.alloc([8, TT], F32)
    GY = XC[:, :, 0:TT]
    HS = THI
    MTS = [dict(t0=A.alloc([4, TT], F32), t1=A.alloc([4, TT], F32), tb=A.alloc([4, TT], F32)) for _ in range(1)]
    MTS[0]['ta'] = MTS[0]['t0']
    MTS[0]['tbb'] = MTS[0]['tb']
    V = A.alloc([D], F32)
    H2 = A.alloc([D], F32)
    H2T = V.rearrange("p (k t) -> p k t", k=8)
    HALO = A.alloc([8, 4], F32)
    CARRY = A.alloc([8], F32)
    hbias = A.alloc([36], F32)
    hbrg = A.alloc([16], F32)
    cfh = A.alloc([8], F32)
    cf1 = A.alloc([8], F32)
    STATS = A.alloc([2, 6], F32)
    MV = A.alloc([2], F32)
    RSTD = A.alloc([1], F32)
    MHALF = A.alloc([1], F32)
    b_in_c = smallv[:, 0:36]
    conv_w_c = smallv[:, 36:68]
    conv_b_c = smallv[:, 68:76]
    MUL, ADD = ALU.mult, ALU.add

    w_in_v = c.w_in_d.rearrange("(k p) n -> p k n", p=128)
    for k in range(8):
        P.op("pool", lambda e, k=k: e.dma_start(out=WIN[:, k, 0:2048], in_=w_in_v[:, k, 0:2048]), writes=[f"WIN{k}a"], dma=f"d_win{k}a")
    P.op("pool", lambda e: e.dma_start(out=WRG, in_=c.w_rg_d.rearrange("a h i j -> i a h j")), writes=["WRG"], dma="d_wrg")
    P.op("pool", lambda e: e.dma_start(out=WRO, in_=c.w_rnn_out_d.rearrange("(k p) n -> p k n", p=128)), writes=["WRO"], dma="d_wro")
    P.op("pool", lambda e: e.dma_start(out=WGL, in_=c.w_glu_d.rearrange("(k p) n -> p k n", p=128)), writes=["WGL"], dma="d_wgl")
    for k in range(8):
        P.op("pool", lambda e, k=k: e.dma_start(out=WIN[:, k, 2048:4096], in_=w_in_v[:, k, 2560:4608]), writes=[f"WIN{k}b"], dma=f"d_win{k}b")
    P.op("sp", lambda e: e.dma_start(out=WR, in_=c.w_router_d.rearrange("(k p) n -> p k n", p=128)), writes=["WR"], dma="d_wr")
    P.op("sp", lambda e: e.dma_start(out=BRB, in_=c.b_router_d.partition_broadcast(128)), writes=["BRB"], dma="d_brb")
    P.op("sp", lambda e: e.dma_start(out=LN1G, in_=c.lnrows_d[0:1, :].partition_broadcast(128)), writes=["LN1G"], dma="d_ln1g")
    P.op("sp", lambda e: e.dma_start(out=LN1B, in_=c.lnrows_d[1:2, :].partition_broadcast(128)), writes=["LN1B"], dma="d_ln1b")
    P.op("sp", lambda e: e.dma_start(out=P1, in_=c.mod_d[0:1, 4 * D:5 * D].partition_broadcast(128)), reads=["mod_d"], writes=["P1"], dma="d_p1")
    P.op("sp", lambda e: e.dma_start(out=H2, in_=c.mod_d[0:1, 3 * D:4 * D].partition_broadcast(128)), reads=["mod_d"], writes=["H2"], dma="d_h2")
    P.op("sp", lambda e: e.dma_start(out=V, in_=c.mod_d[0:1, 2 * D:3 * D].partition_broadcast(128)), reads=["mod_d"], writes=["V"], dma="d_v")
    P.op("dve", lambda e: e.tensor_scalar(P1, P1, 1.0, None, op0=ADD), reads=["P1"], writes=["P1"])
    P.op("dve", lambda e: e.tensor_tensor(P2, LN1B, P1, op=MUL), reads=["LN1B", "P1"], writes=["P2"])
    P.op("dve", lambda e: e.tensor_tensor(P2, P2, H2, op=ADD), reads=["P2", "H2"], writes=["P2"])
    P.op("dve", lambda e: e.tensor_tensor(P1, P1, LN1G, op=MUL), reads=["P1", "LN1G"], writes=["P1"])
    P.op("dve", lambda e: e.tensor_scalar(V, V, 1.0, 0.5, op0=ADD, op1=MUL), reads=["V"], writes=["V"])
    w_out_v = c.w_out_d.rearrange("(k p) n -> p k n", p=128)
    for k in range(8):
        sl = k % 3
        P.op("sp", lambda e, k=k, sl=sl: e.dma_start(out=XIN[sl], in_=w_out_v[:, k, :]), writes=[f"xin{sl}"], dma=f"d_xin{sl}")
        P.op("dve", lambda e, k=k, sl=sl: e.tensor_tensor(WOU[:, k, :], XIN[sl], V, op=MUL), reads=[f"xin{sl}", "V"], writes=[f"WOU{k}"])
    P.op("dve", lambda e: e.tensor_scalar(hbias, b_in_c, 0.5, None, op0=MUL), reads=["smallv"], writes=["hbias"])
    P.op("dve", lambda e: e.tensor_scalar(hbrg, smallv[:, 76:92], 0.5, None, op0=MUL), reads=["smallv"], writes=["hbrg"])
    P.op("dve", lambda e: e.tensor_scalar(cfh, cf, 0.5, None, op0=MUL), reads=["cf"], writes=["cfh"])
    P.op("dve", lambda e: e.tensor_copy(cf1, cf), reads=["cf"], writes=["cf1"])
    P.op("dve", lambda e: e.memset(HALO, 0.0), writes=["halo"])
    P.op("dve", lambda e: e.memset(CARRY, 0.0), writes=["carry"])
    P.op("pool", lambda e: e.memset(MHALF, -0.5), writes=["mhalf"])

    bn = [0]

    def nb():
        i = bn[0] % 8
        bn[0] += 1
        return psum_t[:, i, :], f"bank{i}"

    def load_x(g):
        sl = g % 3
        P.op("sp", lambda e, g=g, sl=sl: e.dma_start(out=XIN[sl], in_=c.x_d[g * 128:(g + 1) * 128, :]), writes=[f"xin{sl}"], dma=f"d_xin{sl}")

    def load_ys(g):
        P.op("sp", lambda e, g=g: e.dma_start(out=YSTs[0], in_=c.ys_d[:, :, g * TT:(g + 1) * TT].rearrange("q p t -> p q t")), writes=["yst0"], dma="d_yst0")

    load_x(0)
    load_x(1)
    load_ys(0)
    for s in range(NST):
        g = s
        sl = g % 3
        hT = hTs[s % 2]
        YST = YSTs[0]
        hp_ = s % 2
        if s + 2 < NT:
            load_x(s + 2)
        for kh in range(2):
            ps, key = nb()
            for kk in range(4):
                k = kh * 4 + kk
                P.op("pe", lambda e, ps=ps, kk=kk, k=k, sl=sl: e.transpose(ps[:, kk * 128:(kk + 1) * 128], XIN[sl][:, k * 128:(k + 1) * 128], ident), reads=[f"xin{sl}", "cst"], writes=[key])
            for kk in range(4):
                k = kh * 4 + kk
                P.op("act", lambda e, ps=ps, kk=kk, k=k, hT=hT: e.activation(out=hT[:, k, :], in_=ps[:, kk * 128:(kk + 1) * 128], func=AF.Identity, scale=modcol[:, k:k + 1], bias=modcol[:, 8 + k:9 + k]),
                     reads=[key, "modcol", "modcol2"], writes=[f"hT{hp_}"])
        hk = f"hT{hp_}"
        P.op("pool", lambda e: e.tensor_copy(XC[:, :, 0:3], HALO[:, :, 0:3]), reads=["halo"], writes=[f"XC{h_}" for h_ in range(8)])
        for hh in range(2):
            ps, key = nb()
            for hq in range(4):
                h = hh * 4 + hq
                for k in range(8):
                    P.op("pe", lambda e, ps=ps, hq=hq, h=h, k=k, hT=hT: e.matmul(ps[:, hq * 128:(hq + 1) * 128], WIN[:, k, h * 128:(h + 1) * 128], hT[:, k, :], start=(k == 0), stop=(k == 7)),
                         reads=[f"WIN{k}a", hk], writes=[key])
            for hq in range(4):
                h = hh * 4 + hq
                P.op("act", lambda e, ps=ps, hq=hq, h=h: e.activation(out=XC[:, h, 3:3 + TT], in_=ps[:, hq * 128:(hq + 1) * 128], func=AF.Identity, bias=b_in_c[:, h:h + 1]),
                     reads=[key, "smallv"], writes=[f"XC{h}"])
        P.op("pool", lambda e: e.tensor_copy(HALO[:, :, 0:3], XC[:, :, TT:TT + 3]), reads=[f"XC{h_}" for h_ in range(8)], writes=["halo"])
        for h in range(8):
            P.op("dve", lambda e, h=h: e.tensor_scalar(XR[:, h, :], XC[:, h, 0:TT], conv_w_c[:, 4 * h:4 * h + 1], conv_b_c[:, h:h + 1], op0=MUL, op1=ADD), reads=[f"XC{h}", "smallv"], writes=[f"XR{h}"])
        for q in range(1, 4):
            for h in range(8):
                P.op("dve", lambda e, h=h, q=q: e.scalar_tensor_tensor(XR[:, h, :], XC[:, h, q:q + TT], conv_w_c[:, 4 * h + q:4 * h + q + 1], XR[:, h, :], op0=MUL, op1=ADD),
                     reads=[f"XC{h}", f"XR{h}"], writes=[f"XR{h}"])
        for hh in range(2):
            P.op("dve", lambda e, hh=hh: e.tensor_copy(XRB[:, hh * 4:(hh + 1) * 4, :], XR[:, hh * 4:(hh + 1) * 4, :]), reads=[f"XR{h_}" for h_ in range(hh * 4, hh * 4 + 4)], writes=[f"XRB{hh}"])
        gbanks = []
        for a_ in range(2):
            for hh in range(2):
                ps, key = nb()
                gbanks.append((ps, key))
                for hq in range(4):
                    h = hh * 4 + hq
                    P.op("pe", lambda e, ps=ps, hq=hq, h=h, a_=a_: e.matmul(ps[:, hq * 128:(hq + 1) * 128], WRG[:, a_, h, :], XRB[:, h, :], start=True, stop=True), reads=["WRG", f"XRB{hh}"], writes=[key])
        for a_ in range(2):
            for hh in range(2):
                ps, key = gbanks[a_ * 2 + hh]
                for hq in range(4):
                    h = hh * 4 + hq
                    dst = THR if a_ == 0 else THI
                    P.op("act", lambda e, ps=ps, hq=hq, h=h, a_=a_, dst=dst: e.activation(out=dst[:, h, :], in_=ps[:, hq * 128:(hq + 1) * 128], func=AF.Tanh, scale=0.5, bias=hbrg[:, a_ * 8 + h:a_ * 8 + h + 1]),
                         reads=[key, "hbrg"], writes=[(f"THR{h}" if a_ == 0 else f"THI{h}")])
        for h in range(8):
            P.op("act", lambda e, h=h: e.activation(out=A2[:, h, :], in_=THR[:, h, :], func=AF.Exp, scale=cf1[:, h:h + 1], bias=cf1[:, h:h + 1]), reads=[f"THR{h}", "cf1"], writes=[f"A2{h}"])
        for h in range(8):
            P.op("act", lambda e, h=h: e.activation(out=THR[:, h, :], in_=THR[:, h, :], func=AF.Exp, scale=cfh[:, h:h + 1], bias=cfh[:, h:h + 1]), reads=[f"THR{h}", "cfh"], writes=[f"THR{h}"])
        def hv(t, hh):
            return t[:, hh * 4:(hh + 1) * 4, :].rearrange("p a b -> p (a b)")
        for hh in range(2):
            P.op("dve", lambda e, hh=hh: e.scalar_tensor_tensor(hv(IX, hh), hv(THI, hh), 1.0, hv(XR, hh), op0=ADD, op1=MUL),
                 reads=[f"THI{h_}" for h_ in range(hh * 4, hh * 4 + 4)] + [f"XR{h_}" for h_ in range(hh * 4, hh * 4 + 4)], writes=[f"IX{hh}"])
        for hh in range(2):
            P.op("act", lambda e, hh=hh: e.activation(out=hv(A2, hh), in_=hv(A2, hh), func=AF.Sqrt, scale=-1.0, bias=1.0), reads=[f"A2{h_}" for h_ in range(hh * 4, hh * 4 + 4)], writes=[f"A2s{hh}"])
        for hh in range(2):
            ps, key = nb()
            for hq in range(4):
                h = hh * 4 + hq
                for k in range(8):
                    P.op("pe", lambda e, ps=ps, hq=hq, h=h, k=k, hT=hT: e.matmul(ps[:, hq * 128:(hq + 1) * 128], WIN[:, k, 1024 + h * 128:1024 + (h + 1) * 128], hT[:, k, :], start=(k == 0), stop=(k == 7)),
                         reads=[f"WIN{k}a", hk], writes=[key])
            for hq in range(4):
                h = hh * 4 + hq
                P.op("act", lambda e, ps=ps, hq=hq, h=h: e.activation(out=GY[:, h, :], in_=ps[:, hq * 128:(hq + 1) * 128], func=AF.Gelu_apprx_tanh, bias=b_in_c[:, 8 + h:9 + h]),
                     reads=[key, "smallv"], writes=[f"XC{h}"])
        for hh in range(2):
            P.op("dve", lambda e, hh=hh: e.scalar_tensor_tensor(hv(IX, hh), hv(A2, hh), 0.5, hv(IX, hh), op0=MUL, op1=MUL), reads=[f"A2s{hh}", f"IX{hh}"], writes=[f"IX{hh}"])
        for h in range(8):
            P.op("dve", lambda e, h=h: e.tensor_tensor_scan(HS[:, h, :], THR[:, h, :], IX[:, h, :], CARRY[:, h:h + 1], op0=MUL, op1=ADD), reads=[f"THR{h}", f"IX{h // 4}", "carry", f"THI{h}"], writes=[f"THI{h}"])
        P.op("dve", lambda e: e.tensor_copy(CARRY, HS[:, :, TT - 1]), reads=[f"THI{h_}" for h_ in range(8)], writes=["carry"])
        for hh in range(2):
            P.op("dve", lambda e, hh=hh: e.tensor_tensor(hv(zA, hh), GY[:, hh * 4:(hh + 1) * 4, :], HS[:, hh * 4:(hh + 1) * 4, :], op=MUL),
                 reads=[f"XC{h_}" for h_ in range(hh * 4, hh * 4 + 4)] + [f"THI{h_}" for h_ in range(hh * 4, hh * 4 + 4)], writes=[f"zA{hh}"])
        for ch in range(2):
            M_ = MTS[0]
            psA, kA = nb()
            for cq in range(4):
                cc = ch * 4 + cq
                for h in range(8):
                    P.op("pe", lambda e, psA=psA, cq=cq, cc=cc, h=h: e.matmul(psA[:, cq * 128:(cq + 1) * 128], WRO[:, h, cc * 128:(cc + 1) * 128], zA[:, h, :], start=(h == 0), stop=(h == 7)),
                         reads=["WRO", f"zA{h // 4}"], writes=[kA])
            psB1, kB1 = nb()
            for cq in range(4):
                cc = ch * 4 + cq
                for q in range(4):
                    P.op("pe", lambda e, psB1=psB1, cq=cq, cc=cc, q=q, YST=YST: e.matmul(psB1[:, cq * 128:(cq + 1) * 128], WGL[:, q, cc * 128:(cc + 1) * 128], YST[:, q, :], start=(q == 0), stop=(q == 3)),
                         reads=["WGL", "yst0"], writes=[kB1])
            psB2, kB2 = nb()
            for cq in range(4):
                cc = ch * 4 + cq
                for q in range(4):
                    P.op("pe", lambda e, psB2=psB2, cq=cq, cc=cc, q=q, YST=YST: e.matmul(psB2[:, cq * 128:(cq + 1) * 128], WGL[:, q, D + cc * 128:D + (cc + 1) * 128], YST[:, q, :], start=(q == 0), stop=(q == 3)),
                         reads=["WGL", "yst0"], writes=[kB2])
            psG = []
            for gi_ in range(2):
                psg, kg = nb()
                psG.append((psg, kg))
                for cq in range(4):
                    cc = ch * 4 + cq
                    for k in range(8):
                        P.op("pe", lambda e, psg=psg, cq=cq, cc=cc, k=k, gi_=gi_, hT=hT: e.matmul(psg[:, cq * 128:(cq + 1) * 128], WIN[:, k, 2048 + gi_ * 1024 + cc * 128:2048 + gi_ * 1024 + (cc + 1) * 128], hT[:, k, :],
                                                                                                 start=(k == 0), stop=(k == 7)),
                             reads=[f"WIN{k}b", hk], writes=[kg])
            for gi_ in range(2):
                psg, kg = psG[gi_]
                dst = M_["t0"] if gi_ == 0 else M_["t1"]
                for cq in range(4):
                    cc = ch * 4 + cq
                    P.op("act", lambda e, psg=psg, cq=cq, cc=cc, gi_=gi_, dst=dst: e.activation(out=dst[:, cq, :], in_=psg[:, cq * 128:(cq + 1) * 128], func=AF.Tanh, scale=0.5, bias=hbias[:, 20 + gi_ * 8 + cc:21 + gi_ * 8 + cc]),
                         reads=[kg, "hbias"], writes=[f"m_t{gi_}"])
            f2 = lambda t: t.rearrange("p a b -> p (a b)")
            P.op("act", lambda e, psB2=psB2, M_=M_: e.activation(out=f2(M_["tb"]), in_=psB2, func=AF.Tanh, scale=0.5), reads=[kB2], writes=[f"m_tb"])
            P.op("dve", lambda e, psA=psA, M_=M_: e.scalar_tensor_tensor(f2(M_["ta"]), f2(M_["t0"]), 1.0, psA, op0=ADD, op1=MUL), reads=[f"m_t0", kA], writes=[f"m_t0"])
            P.op("dve", lambda e, psB1=psB1, M_=M_: e.scalar_tensor_tensor(f2(M_["tbb"]), f2(M_["tb"]), 1.0, psB1, op0=ADD, op1=MUL), reads=[f"m_tb", kB1], writes=[f"m_tb"])
            P.op("dve", lambda e, M_=M_: e.scalar_tensor_tensor(f2(M_["tbb"]), f2(M_["t1"]), 1.0, f2(M_["tbb"]), op0=ADD, op1=MUL), reads=[f"m_t1", f"m_tb"], writes=[f"m_tb"])
            P.op("dve", lambda e, M_=M_, ch=ch: e.scalar_tensor_tensor(f2(mT[:, ch * 4:(ch + 1) * 4, :]), f2(M_["tbb"]), 0.5, f2(M_["ta"]), op0=MUL, op1=ADD), reads=[f"m_tb", f"m_t0"], writes=[f"mT{ch}"])
        if s + 1 < NT:
            load_ys(s + 1)
        obk = []
        for hlf in range(2):
            ps, key = nb()
            obk.append((ps, key))
            for cc in range(8):
                P.op("pe", lambda e, ps=ps, hlf=hlf, cc=cc: e.matmul(ps, mT[:, cc, :], WOU[:, cc, hlf * 512:(hlf + 1) * 512], start=(cc == 0), stop=(cc == 7)),
                     reads=[f"mT{cc // 4}", f"WOU{cc}"], writes=[key])
        for hlf in range(2):
            ps, key = obk[hlf]
            P.op("dve", lambda e, ps=ps, hlf=hlf, sl=sl: e.scalar_tensor_tensor(V[:, hlf * 512:(hlf + 1) * 512], XIN[sl][:, hlf * 512:(hlf + 1) * 512], ALPHA, ps, op0=MUL, op1=ADD),
                 reads=[f"xin{sl}", key], writes=["V"])
            P.op("dve", lambda e, hlf=hlf: e.bn_stats(STATS[:, hlf, :], V[:, hlf * 512:(hlf + 1) * 512]), reads=["V"], writes=["stats"])
        P.op("dve", lambda e: e.bn_aggr(MV, STATS.rearrange("p a b -> p (a b)")), reads=["stats"], writes=["mv"])
        P.op("pool", lambda e: e.tensor_scalar(RSTD, MV[:, 1:2], EPS, None, op0=ADD), reads=["mv"], writes=["rstd"])
        P.op("pool", lambda e: e.tensor_tensor(RSTD, RSTD, MHALF, op=ALU.pow), reads=["rstd", "mhalf"], writes=["rstd"])
        P.op("dve", lambda e: e.tensor_scalar(V, V, MV[:, 0:1], RSTD, op0=ALU.subtract, op1=MUL), reads=["V", "mv", "rstd"], writes=["V"])
        P.op("pool", lambda e, sl=sl: e.tensor_tensor(XIN[sl], V, LN1G, op=MUL), reads=["V", "LN1G"], writes=[f"xin{sl}"])
        P.op("pool", lambda e, sl=sl: e.tensor_tensor(XIN[sl], XIN[sl], LN1B, op=ADD), reads=[f"xin{sl}", "LN1B"], writes=[f"xin{sl}"])
        P.op("dve", lambda e: e.tensor_tensor(H2, V, P1, op=MUL), reads=["V", "P1"], writes=["H2"])
        P.op("dve", lambda e: e.tensor_tensor(H2, H2, P2, op=ADD), reads=["H2", "P2"], writes=["H2"])
        P.op("sp", lambda e, g=g, sl=sl: e.dma_start(out=c.x1_d[g * 128:(g + 1) * 128, :], in_=XIN[sl]), reads=[f"xin{sl}"], writes=["x1_d"], dma=f"d_x1st{sl}")
        P.op("pool", lambda e, g=g: e.dma_start(out=c.h2_d[g * 128:(g + 1) * 128, :], in_=H2), reads=["H2"], writes=["h2_d"], dma="d_h2st")
        for kh in range(2):
            ps, key = nb()
            for kk in range(4):
                k = kh * 4 + kk
                P.op("pe", lambda e, ps=ps, kk=kk, k=k: e.transpose(ps[:, kk * 128:(kk + 1) * 128], H2[:, k * 128:(k + 1) * 128], ident), reads=["H2", "cst"], writes=[key])
            P.op("act", lambda e, ps=ps, kh=kh: e.activation(out=H2T[:, kh * 4:(kh + 1) * 4, :], in_=ps.rearrange("p (a b) -> p a b", a=4), func=AF.Identity), reads=[key], writes=["V"])
        psl, kl = nb()
        for k in range(8):
            P.op("pe", lambda e, psl=psl, k=k: e.matmul(psl[:, 0:NE], H2T[:, k, :], WR[:, k, :], start=(k == 0), stop=(k == 7)), reads=["V", "WR"], writes=[kl])
        P.op("dve", lambda e, psl=psl, g=g: e.tensor_tensor(L[:, g, :], psl[:, 0:NE], BRB, op=ADD), reads=[kl, "BRB"], writes=["L"])
    P.op("sp", lambda e: e.dma_start(out=c.lg_d, in_=L), reads=["L"], writes=["lg_d"], dma="d_lgst")


def s5_prepare(c):
    import math
    P, A, psum_t, cst, ident = c.P, c.A, c.psum_t, c.cst, c.ident
    NJ = 64
    half = cst[:, 512:514]
    mask16 = cst[:, 256:384]
    K_ = types.SimpleNamespace()
    K_.WINU = A.alloc([8, 512], BF16)
    K_.WinT = A.alloc([2, 4, 1024], BF16)
    K_.KTA = A.alloc([4, 1024], BF16)
    K_.WinT3 = A.alloc([2, 4, 1024], BF16)
    K_.COUT = A.alloc([2, 16, 320], BF16)
    K_.COS = A.alloc([16, NJ], F32)
    K_.SIN = A.alloc([16, NJ], F32)
    K_.RHO = A.alloc([16, NJ], F32)
    K_.PW = A.alloc([9, 2, 16], F32)
    K_.CAR = A.alloc([2, 16], F32)
    K_.mark = A.mark()
    LAM = A.alloc([3, 16], F32)
    Bt = A.alloc([2, 16, 16], F32)
    Ct = A.alloc([2, 16, 16], F32)
    DCOL = A.alloc([4], F32)
    ED = A.alloc([8, 2, 512], F32)
    CX = A.alloc([2, 512], F32)
    BB = A.alloc([2, 16, 16], F32)
    W3 = [A.alloc([16, 16], F32) for _ in range(6)]
    s = [A.alloc([16], F32) for _ in range(16)]
    TJ = [A.alloc([16, 32], F32) for _ in range(4)]
    KT0 = A.alloc([128], F32)
    PRE = ["s5pre"]

    def dve(fn):
        P.op("dve", fn, reads=PRE, writes=PRE)

    def act(fn):
        P.op("act", fn, reads=PRE, writes=PRE)

    def tt(o, a, b, op):
        dve(lambda e: e.tensor_tensor(o, a, b, op=op))

    def ts(o, a, s1, op0, s2=None, op1=None):
        if op1 is None:
            dve(lambda e: e.tensor_scalar(o, a, s1, None, op0=op0))
        else:
            dve(lambda e: e.tensor_scalar(o, a, s1, s2, op0=op0, op1=op1))

    MUL, ADD, SUB = ALU.mult, ALU.add, ALU.subtract

    def cmul(ore, oim, are, aim, bre, bim, t1, t2):
        tt(t1, aim, bim, MUL)
        tt(t2, are, bre, MUL)
        tt(ore, t2, t1, SUB)
        tt(t1, are, bim, MUL)
        tt(t2, aim, bre, MUL)
        tt(oim, t2, t1, ADD)

    P.op("sp", lambda e: e.dma_start(out=LAM, in_=c.s5lam_d), writes=PRE, dma="d_s5lam")
    P.op("sp", lambda e: e.dma_start(out=Bt, in_=c.s5b_d), writes=["s5Bt"], dma="d_s5b")
    P.op("sp", lambda e: e.dma_start(out=Ct, in_=c.s5c_d), writes=["s5Ct"], dma="d_s5c")
    P.op("sp", lambda e: e.dma_start(out=DCOL, in_=c.s5d_d), writes=["s5D"], dma="d_s5d")
    P.op("pool", lambda e: e.dma_start(out=K_.WINU, in_=c.w_in_d.rearrange("(k p) n -> p k n", p=128)[:, :, 2048:2560]), writes=["WINU"], dma="d_winu")
    lr, li, ldt = LAM[:, 0, :], LAM[:, 1, :], LAM[:, 2, :]
    dt, rl, th, mag, rho8, sn, cs, t1, t2, t3, are, aim, qre, qim, den, am1 = s
    act(lambda e: e.activation(out=dt, in_=ldt, func=AF.Exp))
    tt(rl, lr, dt, MUL)
    tt(th, li, dt, MUL)
    act(lambda e: e.activation(out=mag, in_=rl, func=AF.Exp))
    act(lambda e: e.activation(out=rho8, in_=rl, func=AF.Exp, scale=8.0))
    ts(t1, th, 1.0 / 32, MUL)
    ts(t2, th, 1.0 / 32, MUL, math.pi / 2, ADD)
    act(lambda e: e.activation(out=sn, in_=t1, func=AF.Sin))
    act(lambda e: e.activation(out=cs, in_=t2, func=AF.Sin))
    for _ in range(5):
        tt(t1, cs, cs, MUL)
        tt(t2, sn, sn, MUL)
        tt(t3, cs, sn, MUL)
        tt(cs, t1, t2, SUB)
        ts(sn, t3, 2.0, MUL)
    tt(are, mag, cs, MUL)
    tt(aim, mag, sn, MUL)
    tt(t1, lr, lr, MUL)
    tt(t2, li, li, MUL)
    tt(den, t1, t2, ADD)
    dve(lambda e: e.reciprocal(den, den))
    ts(am1, are, -1.0, ADD)
    tt(t1, am1, lr, MUL)
    tt(t2, aim, li, MUL)
    tt(t1, t1, t2, ADD)
    tt(qre, t1, den, MUL)
    tt(t1, aim, lr, MUL)
    tt(t2, am1, li, MUL)
    tt(t1, t1, t2, SUB)
    tt(qim, t1, den, MUL)
    PRE.extend(["s5Bt", "s5Ct", "s5D"])

    def bc(a):
        return a.unsqueeze(2).to_broadcast([128, 16, 16])

    cmul(BB[:, 0], BB[:, 1], bc(qre), bc(qim), Bt[:, 0], Bt[:, 1], W3[0], W3[1])
    PW = K_.PW
    dve(lambda e: e.memset(PW[:, 0, 0, :], 1.0))
    dve(lambda e: e.memset(PW[:, 0, 1, :], 0.0))
    for k in range(1, 9):
        cmul(PW[:, k, 0, :], PW[:, k, 1, :], PW[:, k - 1, 0, :], PW[:, k - 1, 1, :], are, aim, t1, t2)
    for d in range(8):
        cmul(W3[2], W3[3], bc(PW[:, d, 0, :]), bc(PW[:, d, 1, :]), BB[:, 0], BB[:, 1], W3[0], W3[1])
        for part in range(2):
            ev = ED[:, d, part, :].rearrange("p (m g c) -> p m g c", m=16, g=2)
            for g in range(2):
                ts(ev[:, :, g, :], W3[2 + part], half[:, g:g + 1], MUL)
    for part in range(2):
        cv = CX[:, part, :].rearrange("p (m g c) -> p m g c", m=16, g=2)
        for g in range(2):
            ts(cv[:, :, g, :], Ct[:, part], half[:, g:g + 1], MUL, (1.0 if part == 0 else -1.0), MUL)
    n = 0
    for part in range(2):
        for q in range(4):
            for dh in range(2):
                bank = n % 2
                n += 1
                for dd in range(4):
                    d = dh * 4 + dd
                    P.op("pe", lambda e, bank=bank, dd=dd, d=d, part=part, q=q: e.transpose(psum_t[:, bank, dd * 128:(dd + 1) * 128], ED[:, d, part, q * 128:(q + 1) * 128], ident),
                         reads=PRE + ["cst"], writes=[f"bank{bank}"])
                P.op("act", lambda e, bank=bank, part=part, q=q, dh=dh: e.activation(out=K_.WinT[:, part, q, dh * 512:(dh + 1) * 512], in_=psum_t[:, bank, :], func=AF.Identity),
                     reads=[f"bank{bank}"], writes=["WinT"])
    P.op("dve", lambda e: e.tensor_copy(K_.WinT3[64:128], K_.WinT[64:128]), reads=["WinT"], writes=["WinT3"])
    P.op("dve", lambda e: e.memset(K_.WinT3[64:96], 0.0), reads=["WinT3"], writes=["WinT3"])
    for q in range(4):
        for d in range(8):
            bank = 2 + (n % 2)
            n += 1
            P.op("pe", lambda e, bank=bank, d=d, q=q: e.matmul(psum_t[:, bank, 0:128], ED[:, d, 0, q * 128:(q + 1) * 128], CX[:, 0, q * 128:(q + 1) * 128], start=True, stop=False),
                 reads=PRE, writes=[f"bank{bank}"])
            P.op("pe", lambda e, bank=bank, d=d, q=q: e.matmul(psum_t[:, bank, 0:128], ED[:, d, 1, q * 128:(q + 1) * 128], CX[:, 1, q * 128:(q + 1) * 128], start=False, stop=True),
                 reads=PRE, writes=[f"bank{bank}"])
            if d == 0:
                P.op("dve", lambda e, bank=bank: e.tensor_tensor(KT0, psum_t[:, bank, 0:128], mask16, op=MUL), reads=[f"bank{bank}", "cst"], writes=["KT0"])
                P.op("dve", lambda e, q=q: e.scalar_tensor_tensor(K_.KTA[:, q, 0:128], ident, DCOL[:, q:q + 1], KT0, op0=MUL, op1=ADD), reads=["KT0", "cst"] + PRE, writes=["KTA"])
            else:
                P.op("dve", lambda e, bank=bank, q=q, d=d: e.tensor_tensor(K_.KTA[:, q, d * 128:(d + 1) * 128], psum_t[:, bank, 0:128], mask16, op=MUL),
                     reads=[f"bank{bank}", "cst"], writes=["KTA"])
    for mp in range(8):
        pr, pi_ = bc(PW[:, mp + 1, 0, :]), bc(PW[:, mp + 1, 1, :])
        tt(W3[0], Ct[:, 0], pr, MUL)
        tt(W3[1], Ct[:, 1], pi_, MUL)
        tt(W3[2], W3[0], W3[1], SUB)
        tt(W3[0], Ct[:, 0], pi_, MUL)
        tt(W3[1], Ct[:, 1], pr, MUL)
        tt(W3[3], W3[0], W3[1], ADD)
        for part in range(2):
            ov = K_.COUT[:, part, :, :].rearrange("p m (a f) -> p m a f", a=8)
            for g in range(2):
                ts(ov[:, :, mp, g * 16:(g + 1) * 16], W3[2 + part], half[:, g:g + 1], MUL, (1.0 if part == 0 else -1.0), MUL)
    e1c, e1s = t1, t2
    dve(lambda e: e.reciprocal(t3, rho8))
    tt(e1c, PW[:, 8, 0, :], t3, MUL)
    tt(e1s, PW[:, 8, 1, :], t3, MUL)
    COS, SIN, RHO = K_.COS, K_.SIN, K_.RHO
    dve(lambda e: e.memset(COS[:, :, 0:1], 1.0))
    dve(lambda e: e.memset(SIN[:, :, 0:1], 0.0))
    dve(lambda e: e.tensor_copy(COS[:, :, 1:2], e1c.unsqueeze(2)))
    dve(lambda e: e.tensor_copy(SIN[:, :, 1:2], e1s.unsqueeze(2)))
    pc, ps_ = den, am1
    dve(lambda e: e.tensor_copy(pc, e1c))
    dve(lambda e: e.tensor_copy(ps_, e1s))
    nn = 2
    while nn < NJ:
        tt(qre, pc, pc, MUL)
        tt(qim, ps_, ps_, MUL)
        tt(t3, pc, ps_, MUL)
        tt(pc, qre, qim, SUB)
        ts(ps_, t3, 2.0, MUL)
        bcn = lambda a, nn=nn: a.unsqueeze(2).to_broadcast([128, 16, nn])
        a1, a2, a3, a4 = (TJ[i][:, :, 0:nn] for i in range(4))
        tt(a1, COS[:, :, 0:nn], bcn(pc), MUL)
        tt(a2, SIN[:, :, 0:nn], bcn(ps_), MUL)
        tt(a3, SIN[:, :, 0:nn], bcn(pc), MUL)
        tt(a4, COS[:, :, 0:nn], bcn(ps_), MUL)
        tt(COS[:, :, nn:2 * nn], a1, a2, SUB)
        tt(SIN[:, :, nn:2 * nn], a3, a4, ADD)
        nn *= 2
    dve(lambda e: e.tensor_copy(RHO, rho8.unsqueeze(2).to_broadcast([128, 16, NJ])))
    dve(lambda e: e.memset(RHO[:, :, 0:1], 0.0))
    dve(lambda e: e.memset(K_.CAR, 0.0))
    return K_


def s5_pass(c):
    P, A, psum_t, K_ = c.P, c.A, c.psum_t, c.K5
    ident, identb, smallv, modcol = c.ident, c.identb, c.smallv, c.modcol
    TT, NJ = 512, 64
    NST = c.dbg.get("nst5", S // TT)
    b_in_c = smallv[:, 0:36]
    XIN = [A.alloc([D], F32) for _ in range(4)]
    hTs = [A.alloc([8, TT], BF16) for _ in range(2)]
    XQs = [A.alloc([4, TT], BF16) for _ in range(2)]
    Tm = [A.alloc([16, NJ], F32) for _ in range(6)]
    ST = A.alloc([2, 16, NJ], BF16)
    YSC = A.alloc([1024], BF16)
    YSF = A.alloc([4, TT], BF16)
    CT = A.alloc([2, 16], F32)
    COS, SIN, RHO, PW, CAR = K_.COS, K_.SIN, K_.RHO, K_.PW, K_.CAR
    MUL, ADD, SUB = ALU.mult, ALU.add, ALU.subtract
    psTb = psum_t[:, 1, :].bitcast(BF16)

    def v4(t):
        return t.rearrange("p (q ml) j -> p q ml j", q=4)

    def f2(t):
        return t.rearrange("p m j -> p (m j)")

    def load_x(g):
        sl = g % 4
        P.op("sp", lambda e, g=g, sl=sl: e.dma_start(out=XIN[sl], in_=c.x_d[g * 128:(g + 1) * 128, :]), writes=[f"xin{sl}"], dma=f"d_xin{sl}")

    for g in range(4):
        load_x(g)
    for s in range(NST):
        t0 = s * TT
        hT = hTs[s % 2]
        XQ = XQs[s % 2]
        sp_ = s % 2
        for k in range(8):
            for tt_ in range(4):
                sl = (4 * s + tt_) % 4
                P.op("pe", lambda e, tt_=tt_, sl=sl, k=k: e.transpose(psum_t[:, 0, tt_ * 128:(tt_ + 1) * 128], XIN[sl][:, k * 128:(k + 1) * 128], ident),
                     reads=[f"xin{sl}", "cst"], writes=["bank0"])
            P.op("act", lambda e, k=k, hT=hT: e.activation(out=hT[:, k, :], in_=psum_t[:, 0, :], func=AF.Identity, scale=modcol[:, k:k + 1], bias=modcol[:, 8 + k:9 + k]),
                 reads=["bank0", "modcol", "modcol2"], writes=[f"hT{sp_}_{k}"])
        if s + 1 < S // TT:
            for tt_ in range(4):
                load_x(4 * (s + 1) + tt_)
        for q in range(4):
            for k in range(8):
                P.op("pe", lambda e, q=q, k=k, hT=hT: e.matmul(psum_t[:, 1, :], K_.WINU[:, k, q * 128:(q + 1) * 128], hT[:, k, :], start=(k == 0), stop=(k == 7)),
                     reads=["WINU", f"hT{sp_}_{k}"], writes=["bank1"])
            P.op("act", lambda e, q=q, XQ=XQ: e.activation(out=XQ[:, q, :], in_=psum_t[:, 1, :], func=AF.Identity, bias=b_in_c[:, 16 + q:17 + q]), reads=["bank1", "smallv"], writes=[f"XQ{sp_}_{q}"])
        for ml in range(4):
            for part in range(2):
                for q in range(4):
                    reg = (part * 4 + q) * 64
                    for k in range(8):
                        if ml < 3:
                            P.op("pe", lambda e, ml=ml, part=part, q=q, k=k, reg=reg, XQ=XQ: e.matmul(psum_t[:, 2 + ml, reg:reg + 64], K_.WinT[32 * ml:32 * ml + 32, part, q, (7 - k) * 128:(8 - k) * 128],
                                                                                              XQ[32 * ml:32 * ml + 32, q, k:TT:8], start=(k == 0), stop=(k == 7)),
                                 reads=["WinT", f"XQ{sp_}_{q}"], writes=[f"bank{2 + ml}"])
                        else:
                            P.op("pe", lambda e, ml=ml, part=part, q=q, k=k, reg=reg, XQ=XQ: e.matmul(psum_t[:, 2 + ml, reg:reg + 64], K_.WinT3[64:128, part, q, (7 - k) * 128:(8 - k) * 128],
                                                                                              XQ[64:128, q, k:TT:8], start=(k == 0), stop=(k == 7)),
                                 reads=["WinT3", f"XQ{sp_}_{q}"], writes=[f"bank{2 + ml}"])
        vbanks = [f"bank{2 + ml}" for ml in range(4)]
        Vre = psum_t[:, 2:6, 0:256].rearrange("p ml (q j) -> p q ml j", q=4)
        Vim = psum_t[:, 2:6, 256:512].rearrange("p ml (q j) -> p q ml j", q=4)
        T0, T1, T2, T3, T4, T5 = Tm
        P.op("dve", lambda e: e.tensor_tensor(v4(T0), v4(COS), Vre, op=MUL), reads=vbanks + ["s5pre"], writes=["T0"])
        P.op("dve", lambda e: e.tensor_tensor(v4(T1), v4(SIN), Vim, op=MUL), reads=vbanks + ["s5pre"], writes=["T1"])
        P.op("dve", lambda e: e.tensor_tensor(v4(T2), v4(COS), Vim, op=MUL), reads=vbanks + ["s5pre"], writes=["T2"])
        P.op("dve", lambda e: e.tensor_tensor(v4(T3), v4(SIN), Vre, op=MUL), reads=vbanks + ["s5pre"], writes=["T3"])
        P.op("pool", lambda e: e.tensor_tensor(T0, T0, T1, op=ADD), reads=["T0", "T1"], writes=["T0"])
        P.op("pool", lambda e: e.tensor_tensor(T2, T2, T3, op=SUB), reads=["T2", "T3"], writes=["T2"])
        P.op("dve", lambda e: e.tensor_tensor(CT[:, 0, :], PW[:, 8, 0, :], CAR[:, 0, :], op=MUL), reads=["car", "s5pre"], writes=["CT"])
        P.op("dve", lambda e: e.tensor_tensor(CT[:, 1, :], PW[:, 8, 1, :], CAR[:, 1, :], op=MUL), reads=["car", "s5pre"], writes=["CT"])
        P.op("dve", lambda e: e.tensor_tensor(CT[:, 0, :], CT[:, 0, :], CT[:, 1, :], op=SUB), reads=["CT"], writes=["CT"])
        P.op("dve", lambda e: e.tensor_tensor(T0[:, :, 0], T0[:, :, 0], CT[:, 0, :], op=ADD), reads=["CT", "T0"], writes=["T0"])
        P.op("dve", lambda e: e.tensor_tensor(CT[:, 0, :], PW[:, 8, 0, :], CAR[:, 1, :], op=MUL), reads=["car", "s5pre", "T0"], writes=["CT"])
        P.op("dve", lambda e: e.tensor_tensor(CT[:, 1, :], PW[:, 8, 1, :], CAR[:, 0, :], op=MUL), reads=["car", "s5pre"], writes=["CT"])
        P.op("dve", lambda e: e.tensor_tensor(CT[:, 0, :], CT[:, 0, :], CT[:, 1, :], op=ADD), reads=["CT"], writes=["CT"])
        P.op("dve", lambda e: e.tensor_tensor(T2[:, :, 0], T2[:, :, 0], CT[:, 0, :], op=ADD), reads=["CT", "T2"], writes=["T2"])
        P.op("pool", lambda e: e.tensor_copy(ST[:, :, :, 0], CAR), reads=["car"], writes=["ST"])
        P.op("dve", lambda e: e.tensor_tensor_scan(f2(T1), f2(RHO), f2(T0), 0.0, op0=MUL, op1=ADD), reads=["T0", "s5pre"], writes=["T1"])
        P.op("dve", lambda e: e.tensor_tensor_scan(f2(T3), f2(RHO), f2(T2), 0.0, op0=MUL, op1=ADD), reads=["T2", "s5pre"], writes=["T3"])
        P.op("pool", lambda e: e.tensor_tensor(T0, COS, T1, op=MUL), reads=["T1", "s5pre"], writes=["T0"])
        P.op("pool", lambda e: e.tensor_tensor(T2, SIN, T3, op=MUL), reads=["T3", "s5pre"], writes=["T2"])
        P.op("dve", lambda e: e.tensor_tensor(T4, SIN, T1, op=MUL), reads=["T1", "s5pre"], writes=["T4"])
        P.op("dve", lambda e: e.tensor_tensor(T5, COS, T3, op=MUL), reads=["T3", "s5pre"], writes=["T5"])
        P.op("pool", lambda e: e.tensor_tensor(T0, T0, T2, op=SUB), reads=["T0", "T2"], writes=["T0"])
        P.op("dve", lambda e: e.tensor_tensor(T4, T4, T5, op=ADD), reads=["T4", "T5"], writes=["T4"])
        P.op("pool", lambda e: e.tensor_copy(ST[:, 0, :, 1:NJ], T0[:, :, 0:NJ - 1]), reads=["T0"], writes=["ST"])
        P.op("pool", lambda e: e.tensor_copy(ST[:, 1, :, 1:NJ], T4[:, :, 0:NJ - 1]), reads=["T4"], writes=["ST"])
        P.op("dve", lambda e: e.tensor_copy(CAR[:, 0, :], T0[:, :, NJ - 1]), reads=["T0"], writes=["car"])
        P.op("dve", lambda e: e.tensor_copy(CAR[:, 1, :], T4[:, :, NJ - 1]), reads=["T4"], writes=["car"])
        for q in range(4):
            for k in range(8):
                lhs = XQ[:, q, k:TT:8]
                if k < 4:
                    P.op("pe", lambda e, lhs=lhs, k=k, q=q: e.matmul(psum_t[0:NJ, 6, k * 128:512], lhs, K_.KTA[:, q, 0:(4 - k) * 128], start=(k == 0), stop=False),
                         reads=[f"XQ{q}", "KTA"], writes=["bank6"])
                    P.op("pe", lambda e, lhs=lhs, k=k, q=q: e.matmul(psum_t[0:NJ, 7, :], lhs, K_.KTA[:, q, (4 - k) * 128:(8 - k) * 128], start=(k == 0), stop=False),
                         reads=[f"XQ{q}", "KTA"], writes=["bank7"])
                else:
                    P.op("pe", lambda e, lhs=lhs, k=k, q=q: e.matmul(psum_t[0:NJ, 7, (k - 4) * 128:512], lhs, K_.KTA[:, q, 0:(8 - k) * 128], start=False, stop=False),
                         reads=[f"XQ{q}", "KTA"], writes=["bank7"])
            for ml in range(4):
                m = 4 * q + ml
                for part in range(2):
                    last = (ml == 3 and part == 1)
                    for mp in range(8):
                        bb, ma = mp // 4, mp % 4
                        P.op("pe", lambda e, m=m, ml=ml, part=part, bb=bb, ma=ma, mp=mp, last=last: e.matmul(
                            psum_t[0:NJ, 6 + bb, ma * 128 + 32 * ml:ma * 128 + 32 * ml + 32], ST[:, part, m, :],
                            K_.COUT[:, part, m, mp * 40:mp * 40 + 32], start=False, stop=(last and ma == 3)),
                            reads=["ST", "COUT"], writes=[f"bank{6 + bb}"])
            P.op("act", lambda e: e.activation(out=YSC[0:NJ, :], in_=psum_t[0:NJ, 6:8, :].rearrange("p a b -> p (a b)"), func=AF.Gelu_apprx_tanh), reads=["bank6", "bank7"], writes=["YSC"])
            for mp in range(8):
                P.op("pe", lambda e, mp=mp: e.transpose(psTb[:, mp * 64:(mp + 1) * 64], YSC[0:NJ, mp * 128:(mp + 1) * 128], identb[0:NJ, 0:NJ]), reads=["YSC", "identb"], writes=["bank1"])
            P.op("dve", lambda e, q=q: e.tensor_copy(YSF[:, q, :].rearrange("p (j m) -> p m j", m=8), psTb[:, 0:512].rearrange("p (m j) -> p m j", m=8)), reads=["bank1"], writes=["YSF"])
        P.op("sp", lambda e, t0=t0: e.dma_start(out=c.ys_d[:, :, t0:t0 + TT].rearrange("q p t -> p q t"), in_=YSF), reads=["YSF"], writes=["ys_d"], dma="d_ysst")


def moe_phase(c):
    P, A, psum_t, dbg = c.P, c.A, c.psum_t, c.dbg
    cst, L, identb, onesb, ident = c.cst, c.L, c.identb, c.onesb, c.ident
    NEX = dbg.get("ne", NE)
    NBK = dbg.get("nbk", NB)
    NTC = dbg.get("ntc", NT)
    NBLK = NE * NB
    ecap1 = cst[:, 520:552]
    tokc = cst[:, 552:584]
    utri = cst[:, 128:256]
    mR = A.mark()
    if "lg" in dbg.get("inject", ()):
        P.op("sp", lambda e: e.dma_start(out=L, in_=c.lg_d), writes=["L"], dma="d_lgin")
    TB = [A.alloc([256], F32) for _ in range(2)]
    IDXF = A.alloc([NBLK], F32)
    IDX = A.alloc([NBLK], I32)
    WSL = A.alloc([NBLK], F32)
    mT_ = A.mark()
    def t3():
        return A.alloc([NT, NE], F32)
    M, E_, POS, TOT, OFF, SIDM, TMP = t3(), t3(), t3(), t3(), t3(), t3(), t3()
    Mb = A.alloc([NT * NE], BF16)
    utb = A.alloc([128], BF16)
    m8 = A.alloc([NT, 8], F32)
    s8 = A.alloc([NT, 8], F32)
    den = A.alloc([NT], F32)
    w4 = A.alloc([NT, 4], F32)
    SC = A.alloc([NT, 4, 2], F32)
    SIDI = A.alloc([NT, 4], I32)
    ZT = A.alloc([8448], F32)
    P.op("pool", lambda e: e.memset(ZT, 0.0), writes=["ZT"])
    P.op("sp", lambda e: e.dma_start(out=c.tab_d, in_=c.tabinit_d), writes=["tab_d"], dma="d_tabz")
    accv = c.acc_d.rearrange("(p r) d -> p (r d)", p=128)
    for j in range(4):
        P.op("sp", lambda e, j=j: e.dma_start(out=accv[:, j * 8448:(j + 1) * 8448], in_=ZT), reads=["ZT"], writes=[f"acc_z{j}"], dma=f"d_accz{j}")
    P.op("sp", lambda e: e.dma_start(out=c.h2_d[S:S + 128, :], in_=ZT[:, 0:512].bitcast(BF16)), reads=["ZT"], writes=["h2_pad"], dma="d_h2pad")
    P.op("dve", lambda e: e.tensor_copy(utb, utri), reads=["cst"], writes=["utb"])
    for i in range(NT):
        P.op("dve", lambda e, i=i: e.max(out=m8[:, i, :], in_=L[:, i, :]), reads=["L"], writes=["m8"])
    P.op("dve", lambda e: e.tensor_tensor(M, L, m8[:, :, 3:4].to_broadcast([128, NT, NE]), op=ALU.is_ge), reads=["L", "m8"], writes=["M"])
    P.op("dve", lambda e: e.tensor_tensor(E_, L, m8[:, :, 0:1].to_broadcast([128, NT, NE]), op=ALU.subtract), reads=["L", "m8"], writes=["E"])
    P.op("act", lambda e: e.activation(out=E_, in_=E_, func=AF.Exp), reads=["E"], writes=["E"])
    P.op("dve", lambda e: e.tensor_tensor(E_, E_, M, op=ALU.mult), reads=["E", "M"], writes=["E"])
    P.op("dve", lambda e: e.tensor_reduce(out=den, in_=E_, axis=AX.X, op=ALU.add), reads=["E"], writes=["den"])
    P.op("dve", lambda e: e.reciprocal(den, den), reads=["den"], writes=["den"])
    P.op("dve", lambda e: e.tensor_tensor(E_, E_, den.unsqueeze(2).to_broadcast([128, NT, NE]), op=ALU.mult), reads=["E", "den"], writes=["E"])
    P.op("dve", lambda e: e.tensor_copy(Mb, M.rearrange("p a b -> p (a b)")), reads=["M"], writes=["Mb"])
    for hlf in range(2):
        P.op("pe", lambda e, hlf=hlf: e.matmul(psum_t[:, hlf, :], utb, Mb[:, hlf * 512:(hlf + 1) * 512], start=True, stop=True), reads=["utb", "Mb"], writes=[f"bank{hlf}"])
        P.op("pe", lambda e, hlf=hlf: e.matmul(psum_t[:, 2 + hlf, :], onesb, Mb[:, hlf * 512:(hlf + 1) * 512], start=True, stop=True), reads=["onesb", "Mb"], writes=[f"bank{2 + hlf}"])
        P.op("dve", lambda e, hlf=hlf: e.tensor_copy(POS.rearrange("p a b -> p (a b)")[:, hlf * 512:(hlf + 1) * 512], psum_t[:, hlf, :]), reads=[f"bank{hlf}"], writes=["POS"])
        P.op("act", lambda e, hlf=hlf: e.activation(out=TOT.rearrange("p a b -> p (a b)")[:, hlf * 512:(hlf + 1) * 512], in_=psum_t[:, 2 + hlf, :], func=AF.Identity), reads=[f"bank{2 + hlf}"], writes=["TOT"])
    P.op("dve", lambda e: e.memset(OFF[:, 0, :], 0.0), writes=["OFF"])
    for i in range(1, NT):
        P.op("dve", lambda e, i=i: e.tensor_tensor(OFF[:, i, :], OFF[:, i - 1, :], TOT[:, i - 1, :], op=ALU.add), reads=["OFF", "TOT"], writes=["OFF"])
    P.op("dve", lambda e: e.tensor_tensor(POS, POS, OFF, op=ALU.add), reads=["POS", "OFF"], writes=["POS"])
    P.op("dve", lambda e: e.tensor_scalar(TMP, POS, float(CAP), None, op0=ALU.is_lt), reads=["POS"], writes=["TMP"])
    P.op("dve", lambda e: e.tensor_tensor(TMP, TMP, M, op=ALU.mult), reads=["TMP", "M"], writes=["TMP"])
    P.op("dve", lambda e: e.tensor_tensor(SIDM, POS, ecap1.unsqueeze(1).to_broadcast([128, NT, NE]), op=ALU.add), reads=["POS", "cst"], writes=["SIDM"])
    P.op("dve", lambda e: e.tensor_tensor(SIDM, SIDM, TMP, op=ALU.mult), reads=["SIDM", "TMP"], writes=["SIDM"])
    P.op("dve", lambda e: e.tensor_scalar(SIDM, SIDM, -1.0, None, op0=ALU.add), reads=["SIDM"], writes=["SIDM"])
    for i in range(NT):
        P.op("dve", lambda e, i=i: e.max(out=s8[:, i, :], in_=SIDM[:, i, :]), reads=["SIDM"], writes=["s8"])
    for k in range(4):
        P.op("dve", lambda e, k=k: e.tensor_tensor(TMP, SIDM, s8[:, :, k:k + 1].to_broadcast([128, NT, NE]), op=ALU.is_equal), reads=["SIDM", "s8"], writes=["TMP"])
        P.op("dve", lambda e: e.tensor_tensor(TMP, TMP, E_, op=ALU.mult), reads=["TMP", "E"], writes=["TMP"])
        P.op("dve", lambda e, k=k: e.tensor_reduce(out=w4[:, :, k], in_=TMP, axis=AX.X, op=ALU.add), reads=["TMP"], writes=["w4"])
    for k in range(4):
        P.op("dve", lambda e, k=k: e.tensor_copy(SC[:, :, k, 0], tokc), reads=["cst"], writes=["SC"])
    P.op("dve", lambda e: e.tensor_copy(SC[:, :, :, 1], w4), reads=["w4"], writes=["SC"])
    P.op("dve", lambda e: e.tensor_copy(SIDI, s8[:, :, 0:4]), reads=["s8"], writes=["SIDI"])
    regc = {}

    def breg(e):
        if "r" not in regc:
            regc["r"] = e.to_reg(NE * CAP - 1)
        return regc["r"]

    for i in range(NT):
        for k in range(4):
            P.op("pool", lambda e, i=i, k=k: e.indirect_dma_start(out=c.tab_d, out_offset=bass.IndirectOffsetOnAxis(ap=SIDI[:, i, k:k + 1], axis=0),
                                                                  in_=SC[:, i, k, :], in_offset=None, bounds_check=breg(e), oob_is_err=False),
                 reads=["SC", "SIDI", "tab_d"], dma="d_scat")
    P.barrier()
    if dbg.get("mstop") == "route":
        return
    tabv = c.tab_d.rearrange("(b s) two -> b (s two)", s=128)
    for hb_ in range(2):
        P.op("sp", lambda e, hb_=hb_: e.dma_start(out=TB[hb_], in_=tabv[hb_ * 128:(hb_ + 1) * 128, :]), writes=[f"TB{hb_}"], dma=f"d_tb{hb_}")
        tv = TB[hb_].rearrange("p (s two) -> p s two", two=2)
        P.op("pe", lambda e, hb_=hb_, tv=tv: e.transpose(psum_t[:, hb_, 0:128], tv[:, :, 0], ident), reads=[f"TB{hb_}", "cst"], writes=[f"bank{hb_}"])
        P.op("pe", lambda e, hb_=hb_, tv=tv: e.transpose(psum_t[:, hb_, 128:256], tv[:, :, 1], ident), reads=[f"TB{hb_}", "cst"], writes=[f"bank{hb_}"])
        P.op("dve", lambda e, hb_=hb_: e.tensor_copy(IDXF[:, hb_ * 128:(hb_ + 1) * 128], psum_t[:, hb_, 0:128]), reads=[f"bank{hb_}"], writes=["IDXF"])
        P.op("dve", lambda e, hb_=hb_: e.tensor_copy(WSL[:, hb_ * 128:(hb_ + 1) * 128], psum_t[:, hb_, 128:256]), reads=[f"bank{hb_}"], writes=["WSL"])
    P.op("dve", lambda e: e.tensor_copy(IDX, IDXF), reads=["IDXF"], writes=["IDX"])
    A.reset(mT_)
    mE = A.mark()
    if dbg.get("mstop") == "table":
        return
    WGU = [A.alloc([8, 2 * D], BF16) for _ in range(2)]
    WDN = [A.alloc([8, D], BF16) for _ in range(2)]
    BGU = A.alloc([NE, 16], F32)
    BDB = [A.alloc([D], F32) for _ in range(2)]
    NXG = 8
    XG = [A.alloc([D], BF16) for _ in range(NXG)]
    XT = [A.alloc([8, 512], BF16) for _ in range(2)]
    Gt = [A.alloc([512], F32) for _ in range(2)]
    Ut = [A.alloc([512], F32) for _ in range(2)]
    SG = [A.alloc([512], F32) for _ in range(2)]
    ACT_ = [A.alloc([8, 512], BF16) for _ in range(2)]
    TY = [A.alloc([D], F32) for _ in range(2)]
    YS = [A.alloc([D], F32) for _ in range(4)]
    P.op("sp", lambda e: e.dma_start(out=BGU, in_=c.b_gu_d), writes=["BGU"], dma="d_bgu")
    P.op("dve", lambda e: e.tensor_scalar(BGU[:, :, 8:16], BGU[:, :, 8:16], 1.0, None, op0=ALU.add), reads=["BGU"], writes=["BGU"])

    def load_w(e_):
        bf = e_ % 2
        gv = c.w_gu_d[e_].rearrange("(k p) n -> p k n", p=128)
        for hk in range(2):
            P.op("pool", lambda e, bf=bf, hk=hk, gv=gv: e.dma_start(out=WGU[bf][:, hk * 4:(hk + 1) * 4, :], in_=gv[:, hk * 4:(hk + 1) * 4, :]),
                 writes=[f"WGU{bf}_{hk}"], dma=f"d_wgu{bf}_{hk}")
        P.op("pool", lambda e, bf=bf, e_=e_: e.dma_start(out=WDN[bf], in_=c.w_down_d[e_].rearrange("(k p) n -> p k n", p=128)), writes=[f"WDN{bf}"], dma=f"d_wdn{bf}")
        P.op("sp", lambda e, bf=bf, e_=e_: e.dma_start(out=BDB[bf], in_=c.b_down_d[e_:e_ + 1, :].partition_broadcast(128)), writes=[f"BDB{bf}"], dma=f"d_bdb{bf}")

    NGRP = NBK // 4
    groups = [(e_, gq) for e_ in range(NEX) for gq in range(NGRP)]
    psTs = [psum_t[:, 0, :].bitcast(BF16), psum_t[:, 7, :].bitcast(BF16)]
    ycnt = [0]
    tcnt = [0]
    deferred = []

    def gather(gi, defer=False):
        e_, gq = groups[gi]
        for bl in range(4):
            blk = e_ * NB + gq * 4 + bl
            sl = (gi * 4 + bl) % NXG

            def rec(sl=sl, blk=blk):
                P.op("pool", lambda e, sl=sl, blk=blk: e.indirect_dma_start(out=XG[sl], out_offset=None, in_=c.h2_d,
                                                                            in_offset=bass.IndirectOffsetOnAxis(ap=IDX[:, blk:blk + 1], axis=0)),
                     reads=["IDX", "h2_pad"], writes=[f"XG{sl}"], dma=f"d_xg{sl}")
            if defer:
                deferred.append(rec)
            else:
                rec()

    def front(gi):
        gb = gi % 2
        for bl in range(4):
            sl = (gi * 4 + bl) % NXG
            ti = tcnt[0] % 2
            tcnt[0] += 1
            psT = psTs[ti]
            tkey = "bank0" if ti == 0 else "bank7"
            for k in range(8):
                P.op("pe", lambda e, k=k, sl=sl, psT=psT: e.transpose(psT[:, k * 128:(k + 1) * 128], XG[sl][:, k * 128:(k + 1) * 128], identb), reads=[f"XG{sl}", "identb"], writes=[tkey])
            P.op("act", lambda e, gb=gb, bl=bl, psT=psT: e.activation(out=XT[gb][:, :, bl * 128:(bl + 1) * 128], in_=psT.rearrange("p (k s) -> p k s", k=8), func=AF.Identity),
                 reads=[tkey], writes=[f"XT{gb}"])

    def gu(gi):
        e_, gq = groups[gi]
        gb = gi % 2
        bf = e_ % 2
        for j in range(8):
            tb = j % 2
            bG, bU = 1 + 2 * tb, 2 + 2 * tb
            for k in range(8):
                P.op("pe", lambda e, bG=bG, j=j, k=k, bf=bf, gb=gb: e.matmul(psum_t[:, bG, :], WGU[bf][:, k, j * 128:(j + 1) * 128], XT[gb][:, k, :], start=(k == 0), stop=(k == 7)),
                     reads=[f"WGU{bf}_{k // 4}", f"XT{gb}"], writes=[f"bank{bG}"])
            for k in range(8):
                P.op("pe", lambda e, bU=bU, j=j, k=k, bf=bf, gb=gb: e.matmul(psum_t[:, bU, :], WGU[bf][:, k, (8 + j) * 128:(9 + j) * 128], XT[gb][:, k, :], start=(k == 0), stop=(k == 7)),
                     reads=[f"WGU{bf}_{k // 4}", f"XT{gb}"], writes=[f"bank{bU}"])
            P.op("dve", lambda e, bG=bG, j=j, e_=e_, tb=tb: e.tensor_scalar(Gt[tb], psum_t[:, bG, :], BGU[:, e_, j:j + 1], 7.0, op0=ALU.add, op1=ALU.min),
                 reads=[f"bank{bG}", "BGU"], writes=[f"G{tb}"])
            P.op("dve", lambda e, bU=bU, j=j, e_=e_, tb=tb: e.tensor_scalar(Ut[tb], psum_t[:, bU, :], BGU[:, e_, 8 + j:9 + j], 8.0, op0=ALU.add, op1=ALU.min),
                 reads=[f"bank{bU}", "BGU"], writes=[f"U{tb}"])
            P.op("act", lambda e, tb=tb: e.activation(out=SG[tb], in_=Gt[tb], func=AF.Silu, scale=1.702), reads=[f"G{tb}"], writes=[f"SG{tb}"])
            P.op("dve", lambda e, tb=tb, gb=gb, j=j: e.scalar_tensor_tensor(ACT_[gb][:, j, :], Ut[tb], -6.0, SG[tb], op0=ALU.max, op1=ALU.mult),
                 reads=[f"SG{tb}", f"U{tb}"], writes=[f"ACT{gb}_{j}"])
            if deferred:
                deferred.pop(0)()

    def down(gi):
        e_, gq = groups[gi]
        gb = gi % 2
        bf = e_ % 2
        for bl in range(4):
            blk = e_ * NB + gq * 4 + bl
            pb = ycnt[0] % 2
            ycnt[0] += 1
            yb_ = bl
            for hlf in range(2):
                bank = 5 + hlf
                for fc in range(8):
                    P.op("pe", lambda e, bank=bank, hlf=hlf, fc=fc, gb=gb, bf=bf, bl=bl: e.matmul(psum_t[:, bank, :], ACT_[gb][:, fc, bl * 128:(bl + 1) * 128], WDN[bf][:, fc, hlf * 512:(hlf + 1) * 512],
                                                                                                 start=(fc == 0), stop=(fc == 7)),
                         reads=[f"ACT{gb}_{fc}", f"WDN{bf}"], writes=[f"bank{bank}"])
                P.op("dve", lambda e, bank=bank, hlf=hlf, bf=bf, pb=pb: e.scalar_tensor_tensor(TY[pb][:, hlf * 512:(hlf + 1) * 512], psum_t[:, bank, :], 1.0 / 1.702, BDB[bf][:, hlf * 512:(hlf + 1) * 512], op0=ALU.mult, op1=ALU.add),
                     reads=[f"bank{bank}", f"BDB{bf}"], writes=[f"TY{pb}"])
            P.op("act", lambda e, pb=pb, yb_=yb_, blk=blk: e.activation(out=YS[yb_], in_=TY[pb], func=AF.Copy, scale=WSL[:, blk:blk + 1]), reads=[f"TY{pb}", "WSL"], writes=[f"YS{yb_}"])
            if "yb" in dbg.get("dump", ()):
                P.op("sp", lambda e, yb_=yb_, blk=blk: e.dma_start(out=c.yb_d[blk * 128:(blk + 1) * 128, :], in_=YS[yb_]), reads=[f"YS{yb_}"], writes=["yb_d"], dma=f"d_ybst{yb_}")

            def rec(yb_=yb_, blk=blk, first=(gq == 0 and bl == 0), e_=e_):
                P.op("pool", lambda e, yb_=yb_, blk=blk: e.indirect_dma_start(out=c.acc_d, out_offset=bass.IndirectOffsetOnAxis(ap=IDX[:, blk:blk + 1], axis=0),
                                                                            in_=YS[yb_], in_offset=None, compute_op=ALU.add),
                     reads=[f"YS{yb_}", "IDX"] + ([f"acc_{(e_ - 1) * NB + q_}" for q_ in range(NB)] + [f"acc_z{q_}" for q_ in range(4)] if first else []),
                     writes=[f"acc_{blk}"], dma=f"d_acc{yb_}")
            deferred.append(rec)

    load_w(0)
    gather(0)
    front(0)
    if len(groups) > 1:
        gather(1)
    for gi in range(len(groups)):
        e_, gq = groups[gi]
        gu(gi)
        while deferred:
            deferred.pop(0)()
        if gi + 1 < len(groups):
            front(gi + 1)
        if gq == 0 and e_ + 1 < NEX:
            load_w(e_ + 1)
        down(gi)
        if gi + 2 < len(groups):
            gather(gi + 2, defer=True)
    while deferred:
        deferred.pop(0)()
    P.barrier()
    if dbg.get("mstop") == "experts":
        return
    A.reset(mR)
    G2B = A.alloc([D], F32)
    LN2G = A.alloc([D], F32)
    LN2B = A.alloc([D], F32)
    GA = [A.alloc([D], F32) for _ in range(4)]
    X1T = [A.alloc([D], F32) for _ in range(4)]
    VV = [A.alloc([D], F32) for _ in range(4)]
    STATS = A.alloc([4, 2, 6], F32)
    MV = A.alloc([4, 2], F32)
    RSTD = A.alloc([4, 1], F32)
    MHALF = A.alloc([1], F32)
    P.op("pool", lambda e: e.memset(MHALF, -0.5), writes=["mhalf"])
    P.op("sp", lambda e: e.dma_start(out=G2B, in_=c.mod_d[0:1, 5 * D:6 * D].partition_broadcast(128)), writes=["G2B"], dma="d_g2b")
    P.op("sp", lambda e: e.dma_start(out=LN2G, in_=c.lnrows_d[2:3, :].partition_broadcast(128)), writes=["LN2G"], dma="d_ln2g")
    P.op("sp", lambda e: e.dma_start(out=LN2B, in_=c.lnrows_d[3:4, :].partition_broadcast(128)), writes=["LN2B"], dma="d_ln2b")
    P.op("dve", lambda e: e.tensor_scalar(G2B, G2B, 1.0, None, op0=ALU.add), reads=["G2B"], writes=["G2B"])
    def comb(i):
        pb = i % 4
        P.op("sp", lambda e, i=i, pb=pb: e.dma_start(out=GA[pb], in_=c.acc_d[i * 128:(i + 1) * 128, :]), writes=[f"GA{pb}"], dma=f"d_xg{pb}")
        P.op("sp", lambda e, i=i, pb=pb: e.dma_start(out=X1T[pb], in_=c.x1_d[i * 128:(i + 1) * 128, :]), writes=[f"X1T{pb}"], dma=f"d_xg{4 + pb}")
        g, v = GA[pb], VV[pb]
        P.op("pool", lambda e, g=g: e.tensor_tensor(g, g, G2B, op=ALU.mult), reads=[f"GA{pb}", "G2B"], writes=[f"GA{pb}"])
        P.op("dve", lambda e, g=g, v=v, pb=pb: e.scalar_tensor_tensor(v, X1T[pb], ALPHA, g, op0=ALU.mult, op1=ALU.add), reads=[f"X1T{pb}", f"GA{pb}"], writes=[f"VV{pb}"])
        for hlf in range(2):
            P.op("dve", lambda e, v=v, hlf=hlf, pb=pb: e.bn_stats(STATS[:, pb, hlf, :], v[:, hlf * 512:(hlf + 1) * 512]), reads=[f"VV{pb}"], writes=[f"stats{pb}"])
        P.op("dve", lambda e, pb=pb: e.bn_aggr(MV[:, pb, :], STATS[:, pb, :, :].rearrange("p a b -> p (a b)")), reads=[f"stats{pb}"], writes=[f"mv{pb}"])
        P.op("pool", lambda e, pb=pb: e.tensor_scalar(RSTD[:, pb, :], MV[:, pb, 1:2], EPS, None, op0=ALU.add), reads=[f"mv{pb}"], writes=[f"rstd{pb}"])
        P.op("pool", lambda e, pb=pb: e.tensor_tensor(RSTD[:, pb, :], RSTD[:, pb, :], MHALF, op=ALU.pow), reads=[f"rstd{pb}", "mhalf"], writes=[f"rstd{pb}"])
        P.op("dve", lambda e, v=v, pb=pb: e.tensor_scalar(v, v, MV[:, pb, 0:1], RSTD[:, pb, :], op0=ALU.subtract, op1=ALU.mult), reads=[f"VV{pb}", f"mv{pb}", f"rstd{pb}"], writes=[f"VV{pb}"])
        P.op("dve", lambda e, v=v: e.tensor_tensor(v, v, LN2G, op=ALU.mult), reads=[f"VV{pb}", "LN2G"], writes=[f"VV{pb}"])
        P.op("pool", lambda e, v=v: e.tensor_tensor(v, v, LN2B, op=ALU.add), reads=[f"VV{pb}", "LN2B"], writes=[f"VV{pb}"])
        P.op("sp", lambda e, i=i, v=v: e.dma_start(out=c.out_d[i * 128:(i + 1) * 128, :], in_=v), reads=[f"VV{pb}"], writes=["out_d"], dma=f"d_acc{pb}")

    for i in range(0, NTC, 4):
        interleave(P, [lambda j=j: comb(j) for j in range(i, min(i + 4, NTC))])


def _consts():
    cst = np.zeros((128, 640), np.float32)
    cst[:, 0:128] = np.eye(128, dtype=np.float32)
    cst[:, 128:256] = np.triu(np.ones((128, 128), np.float32), 1)
    idx = np.arange(128)
    cst[:, 256:384] = (idx[:, None] // 16 == idx[None, :] // 16).astype(np.float32)
    cst[:, 384:512] = 1.0
    cst[:, 512] = (idx < 64).astype(np.float32)
    cst[:, 513] = (idx >= 64).astype(np.float32)
    cst[:, 514] = idx.astype(np.float32)
    cst[:, 520:552] = (np.arange(32) * CAP + 1)[None, :].astype(np.float32)
    cst[:, 552:584] = (idx[:, None] + 128 * np.arange(32)[None, :]).astype(np.float32)
    return cst


def prep_core_inputs(inp, b):
    f = lambda a: np.ascontiguousarray(np.asarray(a, dtype=np.float32))
    m = {}
    m["x"] = f(inp["x"][b])
    m["ccol"] = f(inp["c"][b].reshape(8, 128).T)
    m["w_ada"] = f(inp["w_ada"][0])
    m["b_ada"] = f(inp["b_ada"][0][None, :])
    m["w_in"] = f(inp["w_in"][0])
    sv = np.concatenate([
        inp["b_in"][0].reshape(36, 128),
        inp["conv_w"][0].reshape(4, 8, 128).transpose(1, 0, 2).reshape(32, 128),
        inp["conv_b"][0].reshape(8, 128), inp["b_rg_a"][0].reshape(8, 128), inp["b_rg_x"][0].reshape(8, 128),
        inp["lru_lambda"][0].reshape(8, 128)], axis=0)
    m["smallv"] = f(sv.T)
    m["w_rg"] = f(np.stack([inp["w_rg_a"][0], inp["w_rg_x"][0]], axis=0))
    m["w_rnn_out"] = f(inp["w_rnn_out"][0])
    m["w_glu"] = f(inp["w_glu"][0])
    m["w_out"] = f(inp["w_out"][0])
    m["lnrows"] = f(np.stack([inp["ln1_g"][0], inp["ln1_b"][0], inp["ln2_g"][0], inp["ln2_b"][0]], axis=0))
    m["w_router"] = f(inp["w_router"][0])
    m["b_router"] = f(inp["b_router"][0][None, :])
    m["w_gu"] = f(inp["w_gu"][0])
    m["b_gu"] = f(np.asarray(inp["b_gu"][0]).reshape(32, 16, 128).transpose(2, 0, 1))
    m["w_down"] = f(inp["w_down"][0])
    m["b_down"] = f(inp["b_down"][0])
    lane = lambda a: np.asarray(a).reshape(16, 128).T
    ldt = np.repeat(np.asarray(inp["s5_log_dt"][0]), 64).reshape(16, 128).T
    m["s5lam"] = f(np.stack([lane(inp["s5_lambda_re"][0]), lane(inp["s5_lambda_im"][0]), ldt], axis=1))
    lb = lambda a: np.asarray(a).reshape(16, 128, 16).transpose(1, 0, 2)
    m["s5b"] = f(np.stack([lb(inp["s5_b_re"][0]), lb(inp["s5_b_im"][0])], axis=1))
    lc = lambda a: np.asarray(a).reshape(16, 2, 16, 64).transpose(1, 3, 0, 2).reshape(128, 16, 16)
    m["s5c"] = f(np.stack([lc(inp["s5_c_re"][0]), lc(inp["s5_c_im"][0])], axis=1))
    m["s5d"] = f(np.asarray(inp["s5_d"][0]).reshape(4, 128).T)
    m["cst"] = _consts()
    ti = np.zeros((NE * CAP, 2), np.float32)
    ti[:, 0] = S + (np.arange(NE * CAP) % 128)
    m["tabinit"] = ti
    return m


_NC_CACHE = {}


def kernel(**inputs):
    if "nc" not in _NC_CACHE:
        _NC_CACHE["nc"] = build_program()
    nc = _NC_CACHE["nc"]
    in_maps = [prep_core_inputs(inputs, b) for b in range(8)]
    res = run_bass_kernel_spmd(nc, in_maps, core_ids=list(range(8)))
    return np.stack([np.asarray(r["out"], dtype=np.float32) for r in res.results], axis=0)
```
